# Optimizing a Trainium2 kernel written in Bass

```python
import math
import jax, jax.numpy as jnp
from jax import lax
import numpy as np

D_MODEL = 1024
BATCH = 4
SEQ = 4096
DEPTH = 1
DEC_BATCH = 32
DEC_SEQ = 8
PAST_LEN = 8192
PAGE_SIZE = 128

MIX_WIDTH = D_MODEL
ATTN_WIDTH = MIX_WIDTH // 2
LRU_WIDTH = MIX_WIDTH - ATTN_WIDTH
HEAD_DIM = 64
N_HEADS = ATTN_WIDTH // HEAD_DIM
LRU_BLOCKS = 8
LRU_BLOCK_DIM = LRU_WIDTH // LRU_BLOCKS
CONV_W = 4
LRU_C = 8.0
MOBA_BLOCK = 256
MOBA_TOPK = 3
Q_CHUNK = 64
ROPE_THETA = 10000.0
N_EXPERTS = 64
TOP_K = 8
N_GROUPS = 8
TOPK_GROUPS = 4
D_EXPERT = D_MODEL // 4
D_SHARED = D_EXPERT
ROUTED_SCALE = 2.5
EPS = 1e-6
IN_COLS = 3 * ATTN_WIDTH + 2 * LRU_WIDTH

kernel_name = 'moba_rglru_moe_hybrid_step'

F32 = jnp.float32


def _rmsnorm(x, g):
    xf = x.astype(F32)
    y = xf * lax.rsqrt(jnp.mean(xf * xf, axis=-1, keepdims=True) + EPS)
    return (y * g.astype(F32)).astype(x.dtype)


def _rope(x, pos):
    half = HEAD_DIM // 2
    inv_freq = ROPE_THETA ** (-jnp.arange(half, dtype=F32) / half)
    ang = pos.astype(F32)[:, None] * inv_freq[None, :]
    cos = jnp.cos(ang)[None, :, None, :]
    sin = jnp.sin(ang)[None, :, None, :]
    xf = x.astype(F32)
    x1, x2 = xf[..., :half], xf[..., half:]
    out = jnp.concatenate([x1 * cos - x2 * sin, x2 * cos + x1 * sin], axis=-1)
    return out.astype(x.dtype)


def _moba_chunk(q, qpos, k_blk, v_blk, k_mean):
    B, C, H, D = q.shape
    NB = k_blk.shape[2]
    n_sel = min(MOBA_TOPK, NB)
    qf = q.astype(F32)
    gate = jnp.einsum('bchd,bhnd->bchn', qf, k_mean)
    cur = qpos // MOBA_BLOCK
    past = jnp.arange(NB)[None, :] < cur[:, None]
    gate = jnp.where(past[None, :, None, :], gate, -jnp.inf)
    _, top_idx = lax.top_k(gate, n_sel)
    own = jnp.broadcast_to(cur[None, :, None, None], (B, C, H, 1)).astype(top_idx.dtype)
    idx = jnp.concatenate([top_idx, own], axis=-1)
    bi = jnp.arange(B)[:, None, None, None]
    hi = jnp.arange(H)[None, None, :, None]
    kg = k_blk[bi, hi, idx].astype(F32)
    vg = v_blk[bi, hi, idx].astype(F32)
    s = jnp.einsum('bchd,bchnkd->bchnk', qf, kg) * (HEAD_DIM ** -0.5)
    key_pos = idx[..., None] * MOBA_BLOCK + jnp.arange(MOBA_BLOCK)
    sel_ok = jnp.concatenate([jnp.arange(n_sel)[None, :] < cur[:, None],
                              jnp.ones((C, 1), dtype=bool)], axis=-1)
    valid = sel_ok[None, :, None, :, None] & (key_pos <= qpos[None, :, None, None, None])
    s = jnp.where(valid, s, -jnp.inf)
    p = jax.nn.softmax(s.reshape(B, C, H, -1), axis=-1).reshape(s.shape)
    o = jnp.einsum('bchnk,bchnkd->bchd', p, vg)
    return o.astype(q.dtype)


def _moba_attend(q, k_all, v_all, qpos):
    B, S, H, D = q.shape
    T = k_all.shape[1]
    NB = -(-T // MOBA_BLOCK)
    pad = NB * MOBA_BLOCK - T
    widths = ((0, 0), (0, pad), (0, 0), (0, 0))
    k_blk = jnp.pad(k_all, widths).reshape(B, NB, MOBA_BLOCK, H, D).transpose(0, 3, 1, 2, 4)
    v_blk = jnp.pad(v_all, widths).reshape(B, NB, MOBA_BLOCK, H, D).transpose(0, 3, 1, 2, 4)
    k_mean = jnp.mean(k_blk.astype(F32), axis=3)
    chunk = Q_CHUNK if S % Q_CHUNK == 0 else S
    n_chunks = S // chunk
    qc = q.reshape(B, n_chunks, chunk, H, D).transpose(1, 0, 2, 3, 4)
    pc = qpos.reshape(n_chunks, chunk)
    out = lax.map(lambda qp: _moba_chunk(qp[0], qp[1], k_blk, v_blk, k_mean), (qc, pc))
    return out.transpose(1, 0, 2, 3, 4).reshape(B, S, H * D)


def _lru_combine(e1, e2):
    a1, b1 = e1
    a2, b2 = e2
    return a1 * a2, a2 * b1 + b2


def _rg_lru(xb, h0, conv_buf, start_pos, conv_w, conv_b, wa, ba, wx, bx, lam):
    B, S, W = xb.shape
    xp = jnp.concatenate([conv_buf.astype(xb.dtype), xb], axis=1)
    conv = conv_b + conv_w[0] * xp[:, 0:S]
    for j in range(1, CONV_W):
        conv = conv + conv_w[j] * xp[:, j:j + S]
    new_buf = xp[:, S:]
    cf = conv.astype(F32)
    xc = cf.reshape(B, S, LRU_BLOCKS, LRU_BLOCK_DIM)
    r = jax.nn.sigmoid(jnp.einsum('bsnd,nde->bsne', xc, wa.astype(F32)).reshape(B, S, W) + ba.astype(F32))
    i = jax.nn.sigmoid(jnp.einsum('bsnd,nde->bsne', xc, wx.astype(F32)).reshape(B, S, W) + bx.astype(F32))
    log_a = -LRU_C * r * jax.nn.softplus(-lam.astype(F32))
    pos = start_pos + jnp.arange(S, dtype=jnp.int32)
    reset = (pos == 0)[None, :, None]
    a = jnp.where(reset, 0.0, jnp.exp(log_a))
    mult = jnp.where(reset, 1.0, jnp.sqrt(-jnp.expm1(2.0 * log_a)))
    b = mult * i * cf
    a_cum, b_cum = lax.associative_scan(_lru_combine, (a, b), axis=1)
    h = a_cum * h0.astype(F32)[:, None, :] + b_cum
    return h.astype(xb.dtype), h[:, -1].astype(h0.dtype), new_buf.astype(conv_buf.dtype)


def _swiglu(x, w_gu, w_down):
    g, u = jnp.split(jnp.dot(x, w_gu), 2, axis=-1)
    return jnp.dot(jax.nn.silu(g) * u, w_down)


def _moe(x, router_w, router_bias, w_gu, w_down, ws_gu, ws_down):
    T = x.shape[0]
    scores = jax.nn.sigmoid(jnp.dot(x.astype(F32), router_w.astype(F32)))
    biased = scores + router_bias.astype(F32)
    grp = biased.reshape(T, N_GROUPS, N_EXPERTS // N_GROUPS)
    gscore = jnp.sum(lax.top_k(grp, 2)[0], axis=-1)
    _, gidx = lax.top_k(gscore, TOPK_GROUPS)
    gmask = jnp.sum(jax.nn.one_hot(gidx, N_GROUPS, dtype=F32), axis=-2) > 0
    emask = jnp.repeat(gmask, N_EXPERTS // N_GROUPS, axis=-1)
    _, eidx = lax.top_k(jnp.where(emask, biased, -jnp.inf), TOP_K)
    w = jnp.take_along_axis(scores, eidx, axis=-1)
    w = ROUTED_SCALE * w / jnp.sum(w, axis=-1, keepdims=True)
    gates = jnp.einsum('tk,tke->te', w, jax.nn.one_hot(eidx, N_EXPERTS, dtype=F32)).astype(x.dtype)
    out = _swiglu(x, ws_gu, ws_down)
    for e in range(N_EXPERTS):
        out = out + gates[:, e:e + 1] * _swiglu(x, w_gu[e], w_down[e])
    return out


def _layer(x, c, past_k, past_v, h0, conv0, start_pos,
           ada_w, ada_b, norm_mix_g, w_in, conv_w, conv_b, gate_a_w, gate_a_b,
           gate_x_w, gate_x_b, lru_lambda, attn_out_g, lru_out_g, w_out, norm_ffn_g,
           router_w, router_bias, exp_w_gu, exp_w_down, shared_w_gu, shared_w_down):
    B, S, _ = x.shape
    mod = jnp.dot(jax.nn.silu(c), ada_w) + ada_b
    shift_m, scale_m, gate_m, shift_f, scale_f, gate_f = [m[:, None, :] for m in jnp.split(mod, 6, axis=-1)]
    h = _rmsnorm(x, norm_mix_g) * (1.0 + scale_m) + shift_m
    proj = jnp.dot(h, w_in)
    q, k, v, xl, gl = jnp.split(proj, [ATTN_WIDTH, 2 * ATTN_WIDTH, 3 * ATTN_WIDTH,
                                       3 * ATTN_WIDTH + LRU_WIDTH], axis=-1)
    pos = start_pos + jnp.arange(S, dtype=jnp.int32)
    q = _rope(q.reshape(B, S, N_HEADS, HEAD_DIM), pos)
    k = _rope(k.reshape(B, S, N_HEADS, HEAD_DIM), pos)
    v = v.reshape(B, S, N_HEADS, HEAD_DIM)
    k_all = jnp.concatenate([past_k.astype(k.dtype), k], axis=1)
    v_all = jnp.concatenate([past_v.astype(v.dtype), v], axis=1)
    attn = _moba_attend(q, k_all, v_all, pos)
    lru, h_last, new_buf = _rg_lru(xl, h0, conv0, start_pos, conv_w, conv_b,
                                   gate_a_w, gate_a_b, gate_x_w, gate_x_b, lru_lambda)
    lru = lru * jax.nn.gelu(gl)
    mixed = jnp.dot(jnp.concatenate([_rmsnorm(attn, attn_out_g), _rmsnorm(lru, lru_out_g)], axis=-1), w_out)
    x = x + gate_m * mixed
    h2 = _rmsnorm(x, norm_ffn_g) * (1.0 + scale_f) + shift_f
    f = _moe(h2.reshape(B * S, D_MODEL), router_w, router_bias, exp_w_gu, exp_w_down,
             shared_w_gu, shared_w_down).reshape(B, S, D_MODEL)
    x = x + gate_f * f
    return x, k, v, h_last, new_buf


def setup_inputs(seed: int = 0) -> dict:
    key = jax.random.key(seed)
    ks = jax.random.split(key, 40)
    n_pages = PAST_LEN // PAGE_SIZE
    n_pool = (DEC_BATCH * n_pages * 5) // 4
    nrm = jax.random.normal
    page_table = jax.random.permutation(ks[9], n_pool)[:DEC_BATCH * n_pages]
    page_table = page_table.reshape(DEC_BATCH, n_pages).astype(jnp.int32)
    a_base = jax.random.uniform(ks[18], (DEPTH, LRU_WIDTH), F32, minval=0.9, maxval=0.999)
    return {
        'x_prompt': nrm(ks[0], (BATCH, SEQ, D_MODEL), F32),
        'x_sample': nrm(ks[1], (DEC_BATCH, DEC_SEQ, D_MODEL), F32),
        'c_prompt': nrm(ks[2], (BATCH, D_MODEL), F32),
        'c_sample': nrm(ks[3], (DEC_BATCH, D_MODEL), F32),
        'cache_k': nrm(ks[4], (DEPTH, n_pool, PAGE_SIZE, N_HEADS, HEAD_DIM), F32),
        'cache_v': nrm(ks[5], (DEPTH, n_pool, PAGE_SIZE, N_HEADS, HEAD_DIM), F32),
        'state_h': 0.5 * nrm(ks[6], (DEPTH, DEC_BATCH, LRU_WIDTH), F32),
        'state_conv': nrm(ks[7], (DEPTH, DEC_BATCH, CONV_W - 1, LRU_WIDTH), F32),
        'page_table': page_table,
        'ada_w': 0.2 * D_MODEL ** -0.5 * nrm(ks[10], (DEPTH, D_MODEL, 6 * D_MODEL), F32),
        'ada_b': 0.02 * nrm(ks[11], (DEPTH, 6 * D_MODEL), F32),
        'norm_mix_g': 1.0 + 0.02 * nrm(ks[12], (DEPTH, D_MODEL), F32),
        'w_in': D_MODEL ** -0.5 * nrm(ks[13], (DEPTH, D_MODEL, IN_COLS), F32),
        'conv_w': CONV_W ** -0.5 * nrm(ks[14], (DEPTH, CONV_W, LRU_WIDTH), F32),
        'conv_b': 0.02 * nrm(ks[15], (DEPTH, LRU_WIDTH), F32),
        'gate_a_w': LRU_BLOCK_DIM ** -0.5 * nrm(ks[16], (DEPTH, LRU_BLOCKS, LRU_BLOCK_DIM, LRU_BLOCK_DIM), F32),
        'gate_a_b': 0.02 * nrm(ks[17], (DEPTH, LRU_WIDTH), F32),
        'gate_x_w': LRU_BLOCK_DIM ** -0.5 * nrm(ks[19], (DEPTH, LRU_BLOCKS, LRU_BLOCK_DIM, LRU_BLOCK_DIM), F32),
        'gate_x_b': 0.02 * nrm(ks[20], (DEPTH, LRU_WIDTH), F32),
        'lru_lambda': jnp.log(a_base) - jnp.log1p(-a_base),
        'attn_out_g': 1.0 + 0.02 * nrm(ks[21], (DEPTH, ATTN_WIDTH), F32),
        'lru_out_g': 1.0 + 0.02 * nrm(ks[22], (DEPTH, LRU_WIDTH), F32),
        'w_out': MIX_WIDTH ** -0.5 * nrm(ks[23], (DEPTH, MIX_WIDTH, D_MODEL), F32),
        'norm_ffn_g': 1.0 + 0.02 * nrm(ks[24], (DEPTH, D_MODEL), F32),
        'router_w': D_MODEL ** -0.5 * nrm(ks[25], (DEPTH, D_MODEL, N_EXPERTS), F32),
        'router_bias': 0.01 * nrm(ks[26], (DEPTH, N_EXPERTS), F32),
        'exp_w_gu': D_MODEL ** -0.5 * nrm(ks[27], (DEPTH, N_EXPERTS, D_MODEL, 2 * D_EXPERT), F32),
        'exp_w_down': D_EXPERT ** -0.5 * nrm(ks[28], (DEPTH, N_EXPERTS, D_EXPERT, D_MODEL), F32),
        'shared_w_gu': D_MODEL ** -0.5 * nrm(ks[29], (DEPTH, D_MODEL, 2 * D_SHARED), F32),
        'shared_w_down': D_SHARED ** -0.5 * nrm(ks[30], (DEPTH, D_SHARED, D_MODEL), F32),
        'final_g': 1.0 + 0.02 * nrm(ks[31], (D_MODEL,), F32),
    }


def reference(x_prompt, x_sample, c_prompt, c_sample, cache_k, cache_v, state_h, state_conv,
              page_table, ada_w, ada_b, norm_mix_g, w_in, conv_w, conv_b, gate_a_w, gate_a_b,
              gate_x_w, gate_x_b, lru_lambda, attn_out_g, lru_out_g, w_out, norm_ffn_g,
              router_w, router_bias, exp_w_gu, exp_w_down, shared_w_gu, shared_w_down, final_g):
    n_pages = page_table.shape[1]
    past_len = n_pages * PAGE_SIZE
    n_dec = x_sample.shape[0]
    n_prm = x_prompt.shape[0]
    yp, ys = x_prompt, x_sample
    kp_l, vp_l, hp_l, cp_l, ks_l, vs_l, hs_l, cs_l = [], [], [], [], [], [], [], []
    for l in range(DEPTH):
        w = (ada_w[l], ada_b[l], norm_mix_g[l], w_in[l], conv_w[l], conv_b[l], gate_a_w[l],
             gate_a_b[l], gate_x_w[l], gate_x_b[l], lru_lambda[l], attn_out_g[l], lru_out_g[l],
             w_out[l], norm_ffn_g[l], router_w[l], router_bias[l], exp_w_gu[l], exp_w_down[l],
             shared_w_gu[l], shared_w_down[l])
        no_kv = jnp.zeros((n_prm, 0, N_HEADS, HEAD_DIM), yp.dtype)
        h0 = jnp.zeros((n_prm, LRU_WIDTH), yp.dtype)
        c0 = jnp.zeros((n_prm, CONV_W - 1, LRU_WIDTH), yp.dtype)
        yp, k1, v1, h1, c1 = _layer(yp, c_prompt, no_kv, no_kv, h0, c0, 0, *w)
        past_k = cache_k[l][page_table].reshape(n_dec, past_len, N_HEADS, HEAD_DIM)
        past_v = cache_v[l][page_table].reshape(n_dec, past_len, N_HEADS, HEAD_DIM)
        ys, k2, v2, h2, c2 = _layer(ys, c_sample, past_k, past_v, state_h[l], state_conv[l], past_len, *w)
        kp_l.append(k1); vp_l.append(v1); hp_l.append(h1); cp_l.append(c1)
        ks_l.append(k2); vs_l.append(v2); hs_l.append(h2); cs_l.append(c2)
    y_prompt = _rmsnorm(yp, final_g)
    y_sample = _rmsnorm(ys, final_g)
    k_prompt = jnp.stack(kp_l)
    v_prompt = jnp.stack(vp_l)
    h_prompt = jnp.stack(hp_l)
    conv_prompt = jnp.stack(cp_l)
    k_sample = jnp.stack(ks_l)
    v_sample = jnp.stack(vs_l)
    h_sample = jnp.stack(hs_l)
    conv_sample = jnp.stack(cs_l)
    return (y_prompt, y_sample, k_prompt, v_prompt, h_prompt, conv_prompt,
            k_sample, v_sample, h_sample, conv_sample)
```

```python
import contextlib
import numpy as np
import concourse.bass as bass
import concourse.mybir as mybir
from concourse.bass_utils import run_bass_kernel_spmd

F32 = mybir.dt.float32
BF16 = mybir.dt.bfloat16
I32 = mybir.dt.int32
U32 = mybir.dt.uint32
ALU = mybir.AluOpType
AF = mybir.ActivationFunctionType
AX = mybir.AxisListType

SEM_LIMIT = 30000
P = 128
D = 1024
NH = 8
HD = 64
NCT = 16
NOT_ = 16
NTL = NCT + NOT_
NBL = 16
NEG = -30000.0
EPS = 1e-6
NE = 64
TOKS = NOT_ * P + 32


class Res:
    __slots__ = ("name", "w", "r", "excl")

    def __init__(self, name="", excl=False):
        self.name = name
        self.w = None
        self.r = {}
        self.excl = excl


class Eng:
    def __init__(self, tr, key, handle, step):
        self.tr = tr
        self.key = key
        self.h = handle
        self.step = step
        self.sems = []
        self.cnt = 0
        self.seen = {}
        self.new_epoch()

    def new_epoch(self):
        s = self.tr.nc.semaphore(f"s_{self.key}_{len(self.sems)}")
        self.sems.append(s.__enter__())
        self.tr._sem_guards.append(s)
        self.cnt = 0

    @property
    def epoch(self):
        return len(self.sems) - 1


class Tracker:
    def __init__(self, nc, ndma=8):
        self.nc = nc
        self._sem_guards = []
        self.e = {}
        for key, h in (("pe", nc.tensor), ("act", nc.scalar), ("dve", nc.vector),
                       ("pool", nc.gpsimd), ("sp", nc.sync)):
            self.e[key] = Eng(self, key, h, 1)
        self.dq = {}
        for q in ("sp", "act", "pool"):
            ring = []
            for i in range(ndma):
                eng = Eng(self, f"dq_{q}{i}", None, 16)
                ring.append(eng)
                self.e[eng.key] = eng
            self.dq[q] = [ring, 0]

    def _wait(self, eng, dep):
        k, ep, n = dep
        if eng.seen.get((k, ep), 0) >= n:
            return
        eng.seen[(k, ep)] = n
        eng.h.wait_ge(self.e[k].sems[ep], n)

    def _deps(self, reads, writes):
        deps = set()
        for res in reads:
            if res.w is not None:
                deps.add(res.w)
            if res.excl:
                for k, (ep, n) in res.r.items():
                    deps.add((k, ep, n))
        for res in writes:
            if res.w is not None:
                deps.add(res.w)
            for k, (ep, n) in res.r.items():
                deps.add((k, ep, n))
        return deps

    def op(self, engkey, fn, reads=(), writes=()):
        eng = self.e[engkey]
        for dep in sorted(self._deps(reads, writes)):
            if dep[0] == engkey and engkey == "pe":
                continue
            self._wait(eng, dep)
        if eng.cnt + 1 > SEM_LIMIT:
            eng.new_epoch()
        inst = fn(eng.h)
        eng.cnt += 1
        inst.then_inc(eng.sems[eng.epoch], 1)
        pos = (eng.epoch, eng.cnt)
        for res in writes:
            res.w = (engkey, pos[0], pos[1])
            res.r = {}
        for res in reads:
            res.r[engkey] = pos
        return inst

    def dma(self, q, out, in_, reads=(), writes=(), **kw):
        issuer = self.e[q]
        ring, idx = self.dq[q]
        deng = ring[idx % len(ring)]
        self.dq[q][1] = idx + 1
        if deng.cnt > 0:
            self._wait(issuer, (deng.key, deng.epoch, deng.cnt))
        for dep in sorted(self._deps(reads, writes)):
            self._wait(issuer, dep)
        if deng.cnt + 16 > SEM_LIMIT:
            deng.new_epoch()
        inst = issuer.h.dma_start(out=out, in_=in_, **kw)
        deng.cnt += 16
        inst.then_inc(deng.sems[deng.epoch], 16)
        pos = (deng.epoch, deng.cnt)
        for res in writes:
            res.w = (deng.key, pos[0], pos[1])
            res.r = {}
        for res in reads:
            res.r[deng.key] = pos
        return inst

    def gather(self, out, in_, idx, reads=(), writes=()):
        issuer = self.e["pool"]
        ring, i = self.dq["pool"]
        deng = ring[i % len(ring)]
        self.dq["pool"][1] = i + 1
        if deng.cnt > 0:
            self._wait(issuer, (deng.key, deng.epoch, deng.cnt))
        for dep in sorted(self._deps(reads, writes)):
            self._wait(issuer, dep)
        if deng.cnt + 16 > SEM_LIMIT:
            deng.new_epoch()
        inst = issuer.h.indirect_dma_start(out=out, out_offset=None, in_=in_,
                                           in_offset=bass.IndirectOffsetOnAxis(ap=idx, axis=0))
        deng.cnt += 16
        inst.then_inc(deng.sems[deng.epoch], 16)
        pos = (deng.epoch, deng.cnt)
        for res in writes:
            res.w = (deng.key, pos[0], pos[1])
            res.r = {}
        for res in reads:
            res.r[deng.key] = pos
        return inst

    def barrier(self):
        targets = [(k, e.epoch, e.cnt) for k, e in self.e.items() if e.cnt > 0]
        for key in ("pe", "act", "dve", "pool", "sp"):
            eng = self.e[key]
            for dep in targets:
                if dep[0] == key:
                    continue
                self._wait(eng, dep)

    def finish(self, outs):
        eng = self.e["sp"]
        for res in outs:
            if res.w is not None:
                self._wait(eng, res.w)
            for k, (ep, n) in res.r.items():
                self._wait(eng, (k, ep, n))

    def close(self):
        for g in reversed(self._sem_guards):
            g.__exit__(None, None, None)


def build(stages=("all",), npool=2560):
    nc = bass.Bass("TRN2", target_bir_lowering=False)
    tr = Tracker(nc)
    ALL = "all" in stages
    DO_ATT = ALL or "ATT" in stages
    DO_MOE = ALL or "MOE" in stages
    DO_SMP = ALL or "S" in stages

    def din(name, shape, dt=F32):
        return nc.dram_tensor(name, list(shape), dt, kind="ExternalInput").ap()

    def dout(name, shape, dt=F32):
        return nc.dram_tensor(name, list(shape), dt, kind="ExternalOutput").ap()

    def dscr(name, shape, dt=F32):
        return nc.dram_tensor(name, list(shape), dt, kind="Internal").ap()

    xp = din("xp", [NTL * P, D])
    xs = din("xs", [32, D])
    c5 = din("c5", [P, 8, 5])
    ada_w = din("ada_w", [D, 6 * D])
    ada_b = din("ada_b", [6 * D])
    nmg = din("norm_mix_g", [D])
    nfg = din("norm_ffn_g", [D])
    fing = din("final_g", [D])
    w_in = din("w_in", [D, 2560])
    w_out = din("w_out", [D, D])
    rope = din("rope", [NTL * P, 64])
    ropes = din("ropes", [8, 64])
    lruv = din("lruv", [P, 4, 12])
    wab = din("wab", [P, 4, P])
    wxb = din("wxb", [P, 4, P])
    aog = din("attn_out_g", [512])
    blkc = din("blkc", [8 * 3 * 16])
    flags = din("flags", [P, 2])
    trim = din("trim", [P, P])
    ohm = din("ohm", [16, 16 * P])
    router_w = din("router_w", [D, NE])
    router_b = din("router_bias", [NE])
    if DO_MOE:
        w_gu = din("w_gu", [NE + 1, D, 512])
        w_dn = din("w_dn", [NE + 1, 256, D])
    if DO_SMP:
        cache_k = din("cache_k", [npool * P, 512])
        cache_v = din("cache_v", [npool * P, 512])
    ptab = din("ptab", [4, 64], I32)
    sth = din("sth", [P, 4, 4])
    stc = din("stc", [P, 4, 4, 3])
    ohs = din("ohs", [32, 32 * P])
    trs = din("trs", [32, 256])
    selq = din("selq", [P, 64])
    dmask = din("dmask", [64, 512])
    par = din("par", [64, 2])
    iot = din("iot", [P, 1])

    o_y = dout("o_y", [NOT_ * P, D])
    o_k = dout("o_k", [NOT_ * P, 512])
    o_v = dout("o_v", [NOT_ * P, 512])
    o_h = dout("o_h", [512])
    o_c = dout("o_c", [3, 512])
    o_ys = dout("o_ys", [32, D])
    o_ks = dout("o_ks", [32, 512])
    o_vs = dout("o_vs", [32, 512])
    o_hs = dout("o_hs", [4, 512])
    o_cs = dout("o_cs", [12, 512])

    modd = dscr("modd", [5, 6 * D])
    xmid = dscr("xmid", [TOKS, D])
    h2d = dscr("h2d", [P, 8, TOKS], BF16)

    out_res = []
    r_xmid = [Res() for _ in range(NOT_ + 1)]
    r_h2d = [Res() for _ in range(NOT_ + 1)]

    with contextlib.ExitStack() as top:
        def sb(es, name, shape, dt=F32):
            return es.enter_context(nc.sbuf_tensor(name, list(shape), dt))

        def ps(es, name, shape, dt=F32):
            return es.enter_context(nc.psum_tensor(name, list(shape), dt))

        pbT = ps(top, "pbT", [P, 1024], BF16); r_pbT = Res(excl=True)
        pb = [ps(top, f"pb{i}", [P, 512], F32) for i in range(7)]
        r_pb = [Res(excl=True) for _ in range(7)]

        idb = sb(top, "idb", [P, P], BF16); r_idb = Res()
        idf = sb(top, "idf", [P, P], F32); r_idf = Res()
        ones_f = sb(top, "ones_f", [P, P], F32); r_ones = Res()
        GT = sb(top, "GT", [P, NOT_ + 1, NE], F32); r_GT = [Res() for _ in range(NOT_ + 1)]

        tr.op("pool", lambda e: e.memset(idb[:], 0.0), writes=[r_idb])
        tr.op("pool", lambda e: e.affine_select(out=idb[:], in_=idb[:], pattern=[[-1, P]],
                                                 compare_op=ALU.not_equal, fill=1.0, base=0,
                                                 channel_multiplier=1), reads=[r_idb], writes=[r_idb])
        tr.op("pool", lambda e: e.memset(idf[:], 0.0), writes=[r_idf])
        tr.op("pool", lambda e: e.affine_select(out=idf[:], in_=idf[:], pattern=[[-1, P]],
                                                 compare_op=ALU.not_equal, fill=1.0, base=0,
                                                 channel_multiplier=1), reads=[r_idf], writes=[r_idf])
        tr.op("pool", lambda e: e.memset(ones_f[:], 1.0), writes=[r_ones])

        r_modd = Res()
        with contextlib.ExitStack() as es:
            c5t = sb(es, "c5t", [P, 8, 5]); r_c5 = Res()
            sct = sb(es, "sct", [P, 8, 5]); r_sc = Res()
            adab = sb(es, "adab", [5, 6 * D]); r_adab = Res()
            modt = sb(es, "modt", [5, 6 * D]); r_modt = Res()
            aw = [sb(es, f"aw{i}", [P, 8, 512]) for i in range(2)]
            r_aw = [Res(), Res()]
            tr.dma("sp", c5t[:], c5[:, :, :], writes=[r_c5])
            tr.dma("sp", adab[:], ada_b.partition_broadcast(5), writes=[r_adab])
            tr.op("act", lambda e: e.activation(out=sct[:], in_=c5t[:], func=AF.Silu),
                  reads=[r_c5], writes=[r_sc])
            awv = ada_w.rearrange("(kc p) n -> p kc n", p=P)
            for cc in range(12):
                a = aw[cc % 2]; ra = r_aw[cc % 2]
                tr.dma("sp", a[:], awv[:, :, cc * 512:(cc + 1) * 512], writes=[ra])
                bank = pb[cc % 2]; rb = r_pb[cc % 2]
                for kc in range(8):
                    tr.op("pe", lambda e, kc=kc, a=a, bank=bank: e.matmul(
                        bank[0:5, :], lhsT=sct[:, kc, :], rhs=a[:, kc, :],
                        start=(kc == 0), stop=(kc == 7)), reads=[r_sc, ra], writes=[rb])
                tr.op("dve", lambda e, bank=bank, cc=cc: e.tensor_tensor(
                    out=modt[:, cc * 512:(cc + 1) * 512], in0=bank[0:5, :],
                    in1=adab[:, cc * 512:(cc + 1) * 512], op=ALU.add),
                    reads=[rb, r_adab], writes=[r_modt])
            tr.dma("sp", modd[:, :], modt[:], reads=[r_modt], writes=[r_modd])
        tr.barrier()

        def load_bc(specs, rows, scratch):
            gtmp, r_g = scratch if scratch is not None else (None, None)
            hi_all = max(r[2] for r in rows)
            for (tile, res, col, g) in specs:
                for (mr, lo, hi) in rows:
                    tr.dma("sp", tile[lo:hi, :],
                           modd[mr, col * D:(col + 1) * D].partition_broadcast(hi - lo),
                           reads=[r_modd], writes=[res])
                if g is not None:
                    tr.dma("sp", gtmp[0:hi_all, :], g.partition_broadcast(hi_all), writes=[r_g])
                    tr.op("dve", lambda e, tile=tile: e.scalar_tensor_tensor(
                        out=tile[0:hi_all, :], in0=tile[0:hi_all, :], scalar=1.0,
                        in1=gtmp[0:hi_all, :], op0=ALU.add, op1=ALU.mult),
                        reads=[res, r_g], writes=[res])

        SROWS = [(1 + s, 8 * s, 8 * s + 8) for s in range(4)]
        NS = 32
        QTs = sb(top, "QTs", [P, 4, NS], BF16); r_QTs = Res()
        KTn = sb(top, "KTn", [P, 4, NS], BF16); r_KTn = Res()
        VNb = sb(top, "VNb", [P, 512], BF16); r_VNb = Res()
        YNs = sb(top, "YNs", [P, 4, NS], BF16); r_YNs = Res()
        tr.op("pool", lambda e: e.memset(VNb[:], 0.0), writes=[r_VNb])

        with contextlib.ExitStack() as es0:
            KT = sb(es0, "KT", [P, 4, NTL * P], BF16); r_KT = [Res() for _ in range(NTL)]
            VA = sb(es0, "VA", [P, NTL, NH, 65], BF16); r_VA = [Res() for _ in range(NTL)]
            tr.op("pool", lambda e: e.memset(VA[:, :, :, 64:65], 1.0), writes=r_VA)
            QTA = sb(es0, "QTA", [P, 4, NOT_ * P], BF16); r_QTA = [Res() for _ in range(NOT_)]
            YNA = sb(es0, "YNA", [P, 4, NOT_ * P], BF16); r_YNA = [Res() for _ in range(4)]
            KM = sb(es0, "KM", [P, 4, 16], BF16); r_KM = Res()

            with contextlib.ExitStack() as es:
                bS1 = sb(es, "bS1", [P, D]); r_bS1 = Res()
                bG1 = sb(es, "bG1", [P, D]); r_bG1 = Res()
                WIN = sb(es, "WIN", [P, 8, 2560], BF16); r_WIN = Res()
                winv = w_in.rearrange("(kc p) n -> p kc n", p=P)
                for cc in range(5):
                    tr.dma("pool", WIN[:, :, cc * 512:(cc + 1) * 512], winv[:, :, cc * 512:(cc + 1) * 512],
                           writes=[r_WIN])
                xt = [sb(es, f"xt{i}", [P, D]) for i in range(2)]; r_xt = [Res() for _ in range(2)]
                hb = sb(es, "hb", [P, D], BF16); r_hb = Res()
                HT = sb(es, "HT", [P, 8, 512], BF16); r_HT = [Res() for _ in range(4)]
                rp = [sb(es, f"rp{i}", [P, 64]) for i in range(2)]; r_rp = [Res(), Res()]
                krb = sb(es, "krb", [P, 512], BF16); r_krb = Res()
                rt_full = sb(es, "rt", [P, NH, 32]); r_rt = Res()
                st = sb(es, "st", [P, 8]); r_st = Res()
                LT = sb(es, "LT", [P, 9, 512]); r_LT = [Res() for _ in range(9)]
                G = [LT[:, i, :] for i in range(9)]
                cv, rr, ii, a2, aa, bb, hh, gsb, uu = G
                r_cv, r_rr, r_ii, r_a2, r_aa, r_bb, r_hh, r_gsb, r_uu = r_LT
                qs, ks, vs, kr = G[0], G[1], G[2], G[3]
                r_qs, r_ks, r_vs, r_kr = r_LT[0], r_LT[1], r_LT[2], r_LT[3]
                qr, r_qr = G[4], r_LT[4]
                tmpf = LT[:, 5:7, :].rearrange("p a b -> p (a b)"); r_tmpf2 = [r_LT[5], r_LT[6]]
                XPc = sb(es, "XPc", [P, 515]); r_XPc = Res()
                XH = sb(es, "XH", [P, 4, 3]); r_XH = [Res() for _ in range(4)]
                HS = sb(es, "HS", [P, 4]); r_HS = [Res() for _ in range(4)]
                YY = sb(es, "YY", [P, 4, 512], BF16); r_YY = [Res() for _ in range(4)]
                YQ = sb(es, "YQ", [P, 512]); r_YQ = Res()
                tcol = sb(es, "tcol", [P, 4]); r_tcol = Res()
                LV = sb(es, "LV", [P, 4, 12]); r_LV = Res()
                WAB = sb(es, "WAB", [P, 4, P]); r_WAB = Res()
                WXB = sb(es, "WXB", [P, 4, P]); r_WXB = Res()
                LC1 = sb(es, "LC1", [P, 4]); r_LC1 = Res()
                KMf = sb(es, "KMf", [P, 4, 16]); r_KMf = Res()
                FL = sb(es, "FL", [P, 2]); r_FL = Res()
                tr.dma("sp", FL[:], flags[:, :], writes=[r_FL])
                tr.dma("sp", LV[:], lruv[:, :, :], writes=[r_LV])
                tr.dma("sp", WAB[:], wab[:, :, :], writes=[r_WAB])
                tr.dma("sp", WXB[:], wxb[:, :, :], writes=[r_WXB])
                tr.op("act", lambda e: e.activation(out=LC1[:], in_=LV[:, :, 7], func=AF.Exp, scale=-1.0),
                      reads=[r_LV], writes=[r_LC1])
                tr.op("act", lambda e: e.activation(out=LC1[:], in_=LC1[:], func=AF.Ln, bias=1.0),
                      reads=[r_LC1], writes=[r_LC1])
                tr.op("dve", lambda e: e.tensor_scalar(out=LC1[:], in0=LC1[:], scalar1=-8.0, scalar2=None,
                                                       op0=ALU.mult), reads=[r_LC1], writes=[r_LC1])
                tr.op("pool", lambda e: e.memset(KMf[:], 0.0), writes=[r_KMf])
                tr.op("pool", lambda e: e.memset(HS[:], 0.0), writes=r_HS)
                tr.op("pool", lambda e: e.memset(XH[:], 0.0), writes=r_XH)
                load_bc([(bS1, r_bS1, 0, None), (bG1, r_bG1, 1, nmg)], [(0, 0, P)], (xt[1], r_xt[1]))

                def rope_apply(src, r_src, dst, r_dst, rpt, rrp, n=P):
                    s3 = src[0:n, :].rearrange("p (h d) -> p h d", h=NH)
                    d3 = dst[0:n, :].rearrange("p (h d) -> p h d", h=NH)
                    cosb = rpt[0:n, 0:32].unsqueeze(1).broadcast_to([n, NH, 32])
                    sinb = rpt[0:n, 32:64].unsqueeze(1).broadcast_to([n, NH, 32])
                    rt = rt_full[0:n]
                    x1 = s3[:, :, 0:32]; x2 = s3[:, :, 32:64]
                    tr.op("pool", lambda e: e.tensor_tensor(out=d3[:, :, 0:32], in0=x1, in1=cosb, op=ALU.mult),
                          reads=[r_src, rrp], writes=[r_dst])
                    tr.op("pool", lambda e: e.tensor_tensor(out=rt, in0=x2, in1=sinb, op=ALU.mult),
                          reads=[r_src, rrp], writes=[r_rt])
                    tr.op("pool", lambda e: e.tensor_tensor(out=d3[:, :, 0:32], in0=d3[:, :, 0:32], in1=rt,
                                                            op=ALU.subtract),
                          reads=[r_dst, r_rt], writes=[r_dst])
                    tr.op("pool", lambda e: e.tensor_tensor(out=d3[:, :, 32:64], in0=x2, in1=cosb, op=ALU.mult),
                          reads=[r_src, rrp], writes=[r_dst])
                    tr.op("pool", lambda e: e.tensor_tensor(out=rt, in0=x1, in1=sinb, op=ALU.mult),
                          reads=[r_src, rrp], writes=[r_rt])
                    tr.op("pool", lambda e: e.tensor_tensor(out=d3[:, :, 32:64], in0=d3[:, :, 32:64], in1=rt,
                                                            op=ALU.add),
                          reads=[r_dst, r_rt], writes=[r_dst])

                def norm_to_HT(x, rx, n, j0):
                    R = slice(0, n)
                    tr.op("act", lambda e: e.activation(out=hb[R, :], in_=x[R, :], func=AF.Square,
                                                        accum_out=st[R, 0:1]),
                          reads=[rx], writes=[r_hb, r_st])
                    tr.op("act", lambda e: e.activation(out=st[R, 1:2], in_=st[R, 0:1], func=AF.Sqrt,
                                                        scale=1.0 / D, bias=EPS), reads=[r_st], writes=[r_st])
                    tr.op("dve", lambda e: e.reciprocal(out=st[R, 2:3], in_=st[R, 1:2]),
                          reads=[r_st], writes=[r_st])
                    tr.op("dve", lambda e: e.scalar_tensor_tensor(
                        out=tmpf[R, :], in0=x[R, :], scalar=st[R, 2:3], in1=bG1[R, :],
                        op0=ALU.mult, op1=ALU.mult), reads=[rx, r_st, r_bG1], writes=r_tmpf2)
                    tr.op("pool", lambda e: e.tensor_tensor(out=hb[R, :], in0=tmpf[R, :], in1=bS1[R, :], op=ALU.add),
                          reads=r_tmpf2 + [r_bS1], writes=[r_hb])
                    for kc in range(8):
                        tr.op("pe", lambda e, kc=kc: e.transpose(out=pbT[:, kc * P:kc * P + n],
                                                                 in_=hb[R, kc * P:(kc + 1) * P],
                                                                 identity=idb[R, R]),
                              reads=[r_hb, r_idb], writes=[r_pbT])
                    tr.op("act", lambda e: e.activation(
                        out=HT[:, :, j0:j0 + n], in_=pbT[:].rearrange("p (k c) -> p k c", k=8)[:, :, 0:n],
                        func=AF.Copy), reads=[r_pbT], writes=[r_HT[j0 // P]])

                def proj_tok(n, j0, which):
                    for (bi_, c0) in which:
                        for kc in range(8):
                            tr.op("pe", lambda e, kc=kc, bi_=bi_, c0=c0: e.matmul(
                                pb[bi_][0:n, :], lhsT=HT[:, kc, j0:j0 + n], rhs=WIN[:, kc, c0:c0 + 512],
                                start=(kc == 0), stop=(kc == 7)),
                                reads=[r_HT[j0 // P], r_WIN], writes=[r_pb[bi_]])

                def transpose4(srcb, r_srcb, n, dst, r_dst):
                    for pr in range(4):
                        tr.op("pe", lambda e, pr=pr: e.transpose(out=pbT[:, pr * P:pr * P + n],
                                                                 in_=srcb[0:n, pr * P:(pr + 1) * P],
                                                                 identity=idb[0:n, 0:n]),
                              reads=[r_srcb, r_idb], writes=[r_pbT])
                    tr.op("dve", lambda e: e.tensor_copy(
                        out=dst, in_=pbT[:, 0:512].rearrange("p (k c) -> p k c", k=4)[:, :, 0:n]),
                        reads=[r_pbT], writes=[r_dst])

                def stage_a(t):
                    own = t >= NCT
                    x = xt[t % 2]; rx = r_xt[t % 2]
                    tr.dma("sp", x[:], xp[t * P:(t + 1) * P, :], writes=[rx])
                    rpt = rp[t % 2]; rrp = r_rp[t % 2]
                    tr.dma("sp", rpt[:], rope[t * P:(t + 1) * P, :], writes=[rrp])
                    j = t % 4
                    norm_to_HT(x, rx, P, j * P)
                    proj_tok(P, j * P, [(1, 512), (2, 1024)] + ([(0, 0)] if (own and DO_ATT) else []))
                    tr.op("act", lambda e: e.activation(out=ks, in_=pb[1][:], func=AF.Copy),
                          reads=[r_pb[1]], writes=[r_ks])
                    tr.op("act", lambda e: e.activation(out=vs, in_=pb[2][:], func=AF.Copy),
                          reads=[r_pb[2]], writes=[r_vs])
                    tr.op("pool", lambda e: e.tensor_copy(
                        out=VA[:, t, :, 0:64], in_=vs.rearrange("p (h d) -> p h d", h=NH)),
                        reads=[r_vs], writes=[r_VA[t]])
                    rope_apply(ks, r_ks, kr, r_kr, rpt, rrp)
                    tr.op("pool", lambda e: e.tensor_copy(out=krb[:], in_=kr), reads=[r_kr], writes=[r_krb])
                    transpose4(krb, r_krb, P, KT[:, :, t * P:(t + 1) * P], r_KT[t])
                    if own:
                        to = t - NCT
                        r1 = Res(); r2 = Res()
                        tr.dma("sp", o_k[to * P:(to + 1) * P, :], kr, reads=[r_kr], writes=[r1])
                        tr.dma("sp", o_v[to * P:(to + 1) * P, :], vs, reads=[r_vs], writes=[r2])
                        out_res.extend([r1, r2])
                        if DO_ATT:
                            tr.op("act", lambda e: e.activation(out=qs, in_=pb[0][:], func=AF.Copy, scale=0.125),
                                  reads=[r_pb[0]], writes=[r_qs])
                            rope_apply(qs, r_qs, qr, r_qr, rpt, rrp)
                            tr.op("pool", lambda e: e.tensor_copy(out=krb[:], in_=qr), reads=[r_qr],
                                  writes=[r_krb])
                            transpose4(krb, r_krb, P, QTA[:, :, to * P:(to + 1) * P], r_QTA[to])

                def kmean(blk):
                    tr.op("dve", lambda e: e.tensor_reduce(
                        out=KMf[:, :, blk], in_=KT[:, :, blk * 256:(blk + 1) * 256], axis=AX.X, op=ALU.add),
                        reads=[r_KT[2 * blk], r_KT[2 * blk + 1]], writes=[r_KMf])
                    tr.op("dve", lambda e: e.tensor_scalar(out=KM[:, :, blk], in0=KMf[:, :, blk],
                                                           scalar1=1.0 / 256.0, scalar2=None, op0=ALU.mult),
                          reads=[r_KMf], writes=[r_KM])

                def lru_core(ci, W, nseg, resets, hist_src, init_fn, own, ydst):
                    L = W // nseg
                    xv = XPc[:, 0:nseg * (L + 3)].rearrange("p (s c) -> p s c", s=nseg)

                    def v3(ap2):
                        return ap2[:, 0:W].rearrange("p (s c) -> p s c", s=nseg)
                    tr.op("dve", lambda e: e.tensor_scalar(
                        out=v3(cv), in0=xv[:, :, 3:3 + L], scalar1=LV[:, ci, 3:4], scalar2=LV[:, ci, 4:5],
                        op0=ALU.mult, op1=ALU.add), reads=[r_XPc, r_LV], writes=[r_cv])
                    for jj in range(3):
                        tr.op("dve", lambda e, jj=jj: e.scalar_tensor_tensor(
                            out=v3(cv), in0=xv[:, :, jj:jj + L], scalar=LV[:, ci, jj:jj + 1], in1=v3(cv),
                            op0=ALU.mult, op1=ALU.add), reads=[r_XPc, r_LV, r_cv], writes=[r_cv])
                    tr.op("pe", lambda e: e.matmul(pb[5][:, 0:W], lhsT=WAB[:, ci, :], rhs=cv[:, 0:W],
                                                   start=True, stop=True),
                          reads=[r_WAB, r_cv], writes=[r_pb[5]])
                    tr.op("pe", lambda e: e.matmul(pb[6][:, 0:W], lhsT=WXB[:, ci, :], rhs=cv[:, 0:W],
                                                   start=True, stop=True),
                          reads=[r_WXB, r_cv], writes=[r_pb[6]])
                    tr.op("act", lambda e: e.activation(out=rr[:, 0:W], in_=pb[5][:, 0:W], func=AF.Sigmoid,
                                                        bias=LV[:, ci, 5:6]),
                          reads=[r_pb[5], r_LV], writes=[r_rr])
                    tr.op("act", lambda e: e.activation(out=ii[:, 0:W], in_=pb[6][:, 0:W], func=AF.Sigmoid,
                                                        bias=LV[:, ci, 6:7]),
                          reads=[r_pb[6], r_LV], writes=[r_ii])
                    tr.op("act", lambda e: e.activation(out=aa[:, 0:W], in_=rr[:, 0:W], func=AF.Exp,
                                                        scale=LC1[:, ci:ci + 1]),
                          reads=[r_rr, r_LC1], writes=[r_aa])
                    tr.op("pool", lambda e: e.tensor_tensor(out=a2[:, 0:W], in0=aa[:, 0:W], in1=aa[:, 0:W],
                                                            op=ALU.mult), reads=[r_aa], writes=[r_a2])
                    tr.op("act", lambda e: e.activation(out=a2[:, 0:W], in_=a2[:, 0:W], func=AF.Sqrt,
                                                        scale=-1.0, bias=1.0), reads=[r_a2], writes=[r_a2])
                    tr.op("pool", lambda e: e.tensor_tensor(out=bb[:, 0:W], in0=a2[:, 0:W], in1=ii[:, 0:W],
                                                            op=ALU.mult), reads=[r_a2, r_ii], writes=[r_bb])
                    tr.op("pool", lambda e: e.tensor_tensor(out=bb[:, 0:W], in0=bb[:, 0:W], in1=cv[:, 0:W],
                                                            op=ALU.mult), reads=[r_bb, r_cv], writes=[r_bb])
                    if resets is not None:
                        tr.op("dve", lambda e: e.tensor_tensor(
                            out=tcol[:, ci:ci + 1], in0=ii[:, 0:1], in1=cv[:, 0:1], op=ALU.mult),
                            reads=[r_ii, r_cv], writes=[r_tcol])
                        if resets == "hard":
                            tr.op("dve", lambda e: e.memset(aa[:, 0:1], 0.0), reads=[r_aa], writes=[r_aa])
                            tr.op("dve", lambda e: e.tensor_copy(out=bb[:, 0:1], in_=tcol[:, ci:ci + 1]),
                                  reads=[r_tcol, r_bb], writes=[r_bb])
                        else:
                            tr.op("dve", lambda e: e.tensor_scalar(
                                out=aa[:, 0:1], in0=aa[:, 0:1], scalar1=FL[:, 0:1], scalar2=None, op0=ALU.mult),
                                reads=[r_aa, r_FL], writes=[r_aa])
                            tr.op("dve", lambda e: e.tensor_scalar(
                                out=tcol[:, ci:ci + 1], in0=tcol[:, ci:ci + 1], scalar1=FL[:, 1:2], scalar2=None,
                                op0=ALU.mult), reads=[r_tcol, r_FL], writes=[r_tcol])
                            tr.op("dve", lambda e: e.scalar_tensor_tensor(
                                out=bb[:, 0:1], in0=bb[:, 0:1], scalar=FL[:, 0:1], in1=tcol[:, ci:ci + 1],
                                op0=ALU.mult, op1=ALU.add), reads=[r_bb, r_FL, r_tcol], writes=[r_bb])
                    for s in range(nseg):
                        init_ap, init_res = init_fn(s)
                        tr.op("dve", lambda e, s=s, init_ap=init_ap: e.tensor_tensor_scan(
                            out=hh[:, s * L:(s + 1) * L], data0=aa[:, s * L:(s + 1) * L],
                            data1=bb[:, s * L:(s + 1) * L], initial=init_ap, op0=ALU.mult, op1=ALU.add),
                            reads=[r_aa, r_bb] + init_res, writes=[r_hh])
                    if own:
                        for kc in range(8):
                            tr.op("pe", lambda e, kc=kc: e.matmul(
                                pb[4][:, 0:W], lhsT=WIN[:, kc, 2048 + ci * P:2048 + (ci + 1) * P],
                                rhs=HT[:, kc, 0:W], start=(kc == 0), stop=(kc == 7)),
                                reads=r_HT + [r_WIN], writes=[r_pb[4]])
                        tr.op("act", lambda e: e.activation(out=gsb[:, 0:W], in_=pb[4][:, 0:W], func=AF.Copy),
                              reads=[r_pb[4]], writes=[r_gsb])
                        tr.op("pool", lambda e: e.tensor_tensor(out=uu[:, 0:W], in0=gsb[:, 0:W], in1=gsb[:, 0:W],
                                                                op=ALU.mult), reads=[r_gsb], writes=[r_uu])
                        tr.op("pool", lambda e: e.tensor_scalar(out=uu[:, 0:W], in0=uu[:, 0:W], scalar1=0.044715,
                                                                scalar2=1.0, op0=ALU.mult, op1=ALU.add),
                              reads=[r_uu], writes=[r_uu])
                        tr.op("pool", lambda e: e.tensor_tensor(out=uu[:, 0:W], in0=uu[:, 0:W], in1=gsb[:, 0:W],
                                                                op=ALU.mult), reads=[r_uu, r_gsb], writes=[r_uu])
                        tr.op("act", lambda e: e.activation(out=uu[:, 0:W], in_=uu[:, 0:W], func=AF.Sigmoid,
                                                            scale=1.5957691216057308),
                              reads=[r_uu], writes=[r_uu])
                        tr.op("pool", lambda e: e.tensor_tensor(out=uu[:, 0:W], in0=uu[:, 0:W], in1=gsb[:, 0:W],
                                                                op=ALU.mult), reads=[r_uu, r_gsb], writes=[r_uu])
                        tr.op("dve", lambda e: e.tensor_tensor(out=YY[:, ci, 0:W], in0=hh[:, 0:W], in1=uu[:, 0:W],
                                                               op=ALU.mult),
                              reads=[r_hh, r_uu], writes=[r_YY[ci]])

                def lru_norm(W, ydst, r_ydst):
                    for ci in range(4):
                        tr.op("act", lambda e, ci=ci: e.activation(out=YQ[:, 0:W], in_=YY[:, ci, 0:W],
                                                                   func=AF.Square),
                              reads=[r_YY[ci]], writes=[r_YQ])
                        tr.op("pe", lambda e, ci=ci: e.matmul(pb[5][:, 0:W], lhsT=ones_f[:], rhs=YQ[:, 0:W],
                                                              start=(ci == 0), stop=(ci == 3)),
                              reads=[r_ones, r_YQ], writes=[r_pb[5]])
                    tr.op("act", lambda e: e.activation(out=gsb[:, 0:W], in_=pb[5][:, 0:W], func=AF.Sqrt,
                                                        scale=1.0 / 512.0, bias=EPS),
                          reads=[r_pb[5]], writes=[r_gsb])
                    tr.op("dve", lambda e: e.reciprocal(out=gsb[:, 0:W], in_=gsb[:, 0:W]), reads=[r_gsb],
                          writes=[r_gsb])
                    for ci in range(4):
                        tr.op("dve", lambda e, ci=ci: e.scalar_tensor_tensor(
                            out=ydst(ci), in0=YY[:, ci, 0:W], scalar=LV[:, ci, 8:9], in1=gsb[:, 0:W],
                            op0=ALU.mult, op1=ALU.mult), reads=[r_YY[ci], r_LV, r_gsb], writes=[r_ydst])

                def lru_group(g):
                    own = g >= 4
                    for ci in range(4):
                        tr.op("pool", lambda e, ci=ci: e.tensor_copy(out=XPc[:, 0:3], in_=XH[:, ci, :]),
                              reads=[r_XH[ci]], writes=[r_XPc])
                        if g == 4:
                            tr.op("dve", lambda e: e.tensor_scalar(
                                out=XPc[:, 0:3], in0=XPc[:, 0:3], scalar1=FL[:, 0:1], scalar2=None,
                                op0=ALU.mult), reads=[r_XPc, r_FL], writes=[r_XPc])
                        for kc in range(8):
                            tr.op("pe", lambda e, kc=kc, ci=ci: e.matmul(
                                pb[3][:], lhsT=WIN[:, kc, 1536 + ci * P:1536 + (ci + 1) * P], rhs=HT[:, kc, :],
                                start=(kc == 0), stop=(kc == 7)), reads=r_HT + [r_WIN], writes=[r_pb[3]])
                        tr.op("act", lambda e: e.activation(out=XPc[:, 3:515], in_=pb[3][:], func=AF.Copy),
                              reads=[r_pb[3]], writes=[r_XPc])
                        tr.op("pool", lambda e, ci=ci: e.tensor_copy(out=XH[:, ci, :], in_=XPc[:, 512:515]),
                              reads=[r_XPc], writes=[r_XH[ci]])
                        resets = "hard" if g == 0 else ("soft" if g == 4 else None)
                        lru_core(ci, 512, 1, resets, None,
                                 lambda s, ci=ci: (HS[:, ci:ci + 1], [r_HS[ci]]), own, None)
                        tr.op("dve", lambda e, ci=ci: e.tensor_copy(out=HS[:, ci:ci + 1], in_=hh[:, 511:512]),
                              reads=[r_hh], writes=[r_HS[ci]])
                    if own:
                        c0 = (g - 4) * 512
                        lru_norm(512, lambda ci: YNA[:, ci, c0:c0 + 512], r_YNA[g - 4])

                for g in range(8):
                    for j in range(4):
                        t = 4 * g + j
                        stage_a(t)
                        if t % 2 == 1:
                            kmean(t // 2)
                    lru_group(g)
                tr.op("pe", lambda e: e.transpose(out=pb[0][0:4, 0:P], in_=HS[:, 0:4], identity=idf[:]),
                      reads=r_HS + [r_idf], writes=[r_pb[0]])
                for ci in range(4):
                    tr.op("pe", lambda e, ci=ci: e.transpose(out=pb[1][0:3, ci * P:(ci + 1) * P],
                                                             in_=XH[:, ci, :], identity=idf[:]),
                          reads=[r_XH[ci], r_idf], writes=[r_pb[1]])
                tr.op("dve", lambda e: e.tensor_copy(out=qs[0:4, 0:P], in_=pb[0][0:4, 0:P]),
                      reads=[r_pb[0]], writes=[r_qs])
                tr.op("dve", lambda e: e.tensor_copy(out=ks[0:3, :], in_=pb[1][0:3, :]),
                      reads=[r_pb[1]], writes=[r_ks])
                r1 = Res(); r2 = Res()
                tr.dma("sp", o_h.rearrange("(c p) -> c p", p=P), qs[0:4, 0:P], reads=[r_qs], writes=[r1])
                tr.dma("sp", o_c[:, :], ks[0:3, :], reads=[r_ks], writes=[r2])
                out_res.extend([r1, r2])

                load_bc([(bS1, r_bS1, 0, None), (bG1, r_bG1, 1, nmg)], SROWS, (xt[1], r_xt[1]))
                xs_t = xt[0]; rxs = r_xt[0]
                tr.dma("sp", xs_t[0:NS, :], xs[:, :], writes=[rxs])
                rps = rp[0]; rrps = r_rp[0]
                for s in range(4):
                    tr.dma("sp", rps[8 * s:8 * s + 8, :], ropes[:, :], writes=[rrps])
                norm_to_HT(xs_t, rxs, NS, 0)
                proj_tok(NS, 0, [(1, 512), (2, 1024), (0, 0)])
                tr.op("act", lambda e: e.activation(out=ks[0:NS, :], in_=pb[1][0:NS, :], func=AF.Copy),
                      reads=[r_pb[1]], writes=[r_ks])
                tr.op("act", lambda e: e.activation(out=vs[0:NS, :], in_=pb[2][0:NS, :], func=AF.Copy),
                      reads=[r_pb[2]], writes=[r_vs])
                tr.op("act", lambda e: e.activation(out=qs[0:NS, :], in_=pb[0][0:NS, :], func=AF.Copy, scale=0.125),
                      reads=[r_pb[0]], writes=[r_qs])
                tr.op("pool", lambda e: e.tensor_copy(out=VNb[0:NS, :], in_=vs[0:NS, :]),
                      reads=[r_vs, r_VNb], writes=[r_VNb])
                rope_apply(ks, r_ks, kr, r_kr, rps, rrps, n=NS)
                r1 = Res(); r2 = Res()
                tr.dma("sp", o_ks[:, :], kr[0:NS, :], reads=[r_kr], writes=[r1])
                tr.dma("sp", o_vs[:, :], vs[0:NS, :], reads=[r_vs], writes=[r2])
                out_res.extend([r1, r2])
                tr.op("pool", lambda e: e.tensor_copy(out=krb[0:NS, :], in_=kr[0:NS, :]), reads=[r_kr],
                      writes=[r_krb])
                transpose4(krb, r_krb, NS, KTn[:, :, :], r_KTn)
                rope_apply(qs, r_qs, qr, r_qr, rps, rrps, n=NS)
                tr.op("pool", lambda e: e.tensor_copy(out=krb[0:NS, :], in_=qr[0:NS, :]), reads=[r_qr],
                      writes=[r_krb])
                transpose4(krb, r_krb, NS, QTs[:, :, :], r_QTs)
                STH = sb(es, "STH", [P, 4, 4]); r_STH = Res()
                STC = sb(es, "STC", [P, 4, 4, 3]); r_STC = Res()
                HSs = sb(es, "HSs", [P, 4, 4]); r_HSs = Res()
                CSs = sb(es, "CSs", [P, 4, 12]); r_CSs = Res()
                tr.dma("sp", STH[:], sth[:, :, :], writes=[r_STH])
                tr.dma("sp", STC[:], stc[:, :, :, :], writes=[r_STC])
                for ci in range(4):
                    xv = XPc[:, 0:44].rearrange("p (s c) -> p s c", s=4)
                    tr.op("pool", lambda e, ci=ci: e.tensor_copy(out=xv[:, :, 0:3], in_=STC[:, ci, :, :]),
                          reads=[r_STC], writes=[r_XPc])
                    for kc in range(8):
                        tr.op("pe", lambda e, kc=kc, ci=ci: e.matmul(
                            pb[3][:, 0:NS], lhsT=WIN[:, kc, 1536 + ci * P:1536 + (ci + 1) * P], rhs=HT[:, kc, 0:NS],
                            start=(kc == 0), stop=(kc == 7)), reads=r_HT + [r_WIN], writes=[r_pb[3]])
                    tr.op("act", lambda e: e.activation(
                        out=xv[:, :, 3:11], in_=pb[3][:, 0:NS].rearrange("p (s c) -> p s c", s=4), func=AF.Copy),
                        reads=[r_pb[3]], writes=[r_XPc])
                    tr.op("pool", lambda e, ci=ci: e.tensor_copy(
                        out=CSs[:, ci, :].rearrange("p (s c) -> p s c", s=4), in_=xv[:, :, 8:11]),
                        reads=[r_XPc], writes=[r_CSs])
                    lru_core(ci, NS, 4, None, None,
                             lambda s, ci=ci: (STH[:, ci, s:s + 1], [r_STH]), True, None)
                    tr.op("dve", lambda e, ci=ci: e.tensor_copy(
                        out=HSs[:, ci, :].unsqueeze(2), in_=hh[:, 0:NS].rearrange("p (s c) -> p s c", s=4)[:, :, 7:8]),
                        reads=[r_hh], writes=[r_HSs])
                lru_norm(NS, lambda ci: YNs[:, ci, :], r_YNs)
                for ci in range(4):
                    tr.op("pe", lambda e, ci=ci: e.transpose(out=pb[0][0:4, ci * P:(ci + 1) * P],
                                                             in_=HSs[:, ci, :], identity=idf[:]),
                          reads=[r_HSs, r_idf], writes=[r_pb[0]])
                    tr.op("pe", lambda e, ci=ci: e.transpose(out=pb[1][0:12, ci * P:(ci + 1) * P],
                                                             in_=CSs[:, ci, :], identity=idf[:]),
                          reads=[r_CSs, r_idf], writes=[r_pb[1]])
                tr.op("dve", lambda e: e.tensor_copy(out=qs[0:4, :], in_=pb[0][0:4, :]),
                      reads=[r_pb[0]], writes=[r_qs])
                tr.op("dve", lambda e: e.tensor_copy(out=ks[0:12, :], in_=pb[1][0:12, :]),
                      reads=[r_pb[1]], writes=[r_ks])
                r1 = Res(); r2 = Res()
                tr.dma("sp", o_hs[:, :], qs[0:4, :], reads=[r_qs], writes=[r1])
                tr.dma("sp", o_cs[:, :], ks[0:12, :], reads=[r_ks], writes=[r2])
                out_res.extend([r1, r2])
            tr.barrier()
            def p2_common(es, sfx, rows):
                bGM = sb(es, "bGM" + sfx, [P, D]); r_bGM = Res()
                bS2 = sb(es, "bS2" + sfx, [P, D]); r_bS2 = Res()
                bG2 = sb(es, "bG2" + sfx, [P, D]); r_bG2 = Res()
                xr = sb(es, "xr" + sfx, [P, D]); r_xr = Res()
                xm = sb(es, "xm" + sfx, [P, D]); r_xm = Res()
                tmpf = sb(es, "tmpf2" + sfx, [P, D]); r_tmpf = Res()
                hb2 = sb(es, "hb2" + sfx, [P, D], BF16); r_hb2 = Res()
                h2s = sb(es, "h2s" + sfx, [P, 8, P], BF16); r_h2s = Res()
                WOUT = sb(es, "WOUT" + sfx, [P, 8, D], BF16); r_WOUT = Res()
                woutv = w_out.rearrange("(kc p) n -> p kc n", p=P)
                for cc in range(2):
                    tr.dma("pool", WOUT[:, :, cc * 512:(cc + 1) * 512], woutv[:, :, cc * 512:(cc + 1) * 512],
                           writes=[r_WOUT])
                RWB = sb(es, "RWB" + sfx, [P, 8, NE], BF16); r_RWB = Res()
                tr.dma("pool", RWB[:], router_w.rearrange("(kc p) n -> p kc n", p=P), writes=[r_RWB])
                RB = sb(es, "RB" + sfx, [P, NE]); r_RB = Res()
                tr.dma("sp", RB[:], router_b.partition_broadcast(P), writes=[r_RB])
                AOG = sb(es, "AOG" + sfx, [P, 512]); r_AOG = Res()
                tr.dma("sp", AOG[:], aog.partition_broadcast(P), writes=[r_AOG])
                AT = sb(es, "AT" + sfx, [P, 512]); r_AT = Res()
                atb = sb(es, "atb" + sfx, [P, 512], BF16); r_atb = Res()
                sc = sb(es, "sc" + sfx, [P, NE]); r_sc2 = Res()
                bi = sb(es, "bi" + sfx, [P, NE]); r_bi = Res()
                m8g = sb(es, "m8g" + sfx, [P, 8, 8]); r_m8g = Res()
                gsr = sb(es, "gsr" + sfx, [P, 8]); r_gsr = Res()
                gm = sb(es, "gm" + sfx, [P, 8]); r_gm = Res()
                msk = sb(es, "msk" + sfx, [P, NE]); r_msk = Res()
                em = sb(es, "em" + sfx, [P, NE]); r_em = Res()
                wv = sb(es, "wv" + sfx, [P, NE]); r_wv = Res()
                st2 = sb(es, "st2" + sfx, [P, 8]); r_st2 = Res()
                load_bc([(bGM, r_bGM, 2, None), (bS2, r_bS2, 3, None), (bG2, r_bG2, 4, nfg)], rows, (xr, r_xr))

                def attn_finish(o3, rc2, n, rds, dst, r_dst):
                    R = slice(0, n)
                    tr.op("dve", lambda e: e.tensor_tensor(
                        out=AT[R, :].rearrange("p (h d) -> p h d", h=NH), in0=o3,
                        in1=rc2.unsqueeze(2).broadcast_to([n, NH, 64]), op=ALU.mult),
                        reads=rds, writes=[r_AT])
                    tr.op("act", lambda e: e.activation(out=hb2[R, 0:512], in_=AT[R, :], func=AF.Square,
                                                        accum_out=st2[R, 0:1]),
                          reads=[r_AT], writes=[r_hb2, r_st2])
                    tr.op("act", lambda e: e.activation(out=st2[R, 1:2], in_=st2[R, 0:1], func=AF.Sqrt,
                                                        scale=1.0 / 512.0, bias=EPS),
                          reads=[r_st2], writes=[r_st2])
                    tr.op("dve", lambda e: e.reciprocal(out=st2[R, 2:3], in_=st2[R, 1:2]),
                          reads=[r_st2], writes=[r_st2])
                    tr.op("dve", lambda e: e.scalar_tensor_tensor(
                        out=atb[R, :], in0=AT[R, :], scalar=st2[R, 2:3], in1=AOG[R, :], op0=ALU.mult,
                        op1=ALU.mult), reads=[r_AT, r_st2, r_AOG], writes=[r_atb])
                    for pr in range(4):
                        tr.op("pe", lambda e, pr=pr: e.transpose(out=pbT[:, pr * P:pr * P + n],
                                                                 in_=atb[R, pr * P:(pr + 1) * P],
                                                                 identity=idb[R, R]),
                              reads=[r_atb, r_idb], writes=[r_pbT])
                    tr.op("dve", lambda e: e.tensor_copy(
                        out=dst, in_=pbT[:, 0:512].rearrange("p (k c) -> p k c", k=4)[:, :, 0:n]),
                        reads=[r_pbT], writes=[r_dst])

                def ffn_pre(src, r_src, n, to):
                    R = slice(0, n)
                    c0 = to * P
                    tr.op("act", lambda e: e.activation(out=hb2[R, :], in_=src[R, :], func=AF.Square,
                                                        accum_out=st2[R, 3:4]),
                          reads=[r_src], writes=[r_hb2, r_st2])
                    tr.op("act", lambda e: e.activation(out=st2[R, 4:5], in_=st2[R, 3:4], func=AF.Sqrt,
                                                        scale=1.0 / D, bias=EPS), reads=[r_st2], writes=[r_st2])
                    tr.op("dve", lambda e: e.reciprocal(out=st2[R, 5:6], in_=st2[R, 4:5]),
                          reads=[r_st2], writes=[r_st2])
                    tr.op("dve", lambda e: e.scalar_tensor_tensor(
                        out=tmpf[R, :], in0=src[R, :], scalar=st2[R, 5:6], in1=bG2[R, :],
                        op0=ALU.mult, op1=ALU.mult), reads=[r_src, r_st2, r_bG2], writes=[r_tmpf])
                    tr.op("pool", lambda e: e.tensor_tensor(out=hb2[R, :], in0=tmpf[R, :], in1=bS2[R, :],
                                                            op=ALU.add),
                          reads=[r_tmpf, r_bS2], writes=[r_hb2])
                    for kc in range(8):
                        tr.op("pe", lambda e, kc=kc: e.transpose(out=pbT[:, kc * P:kc * P + n],
                                                                 in_=hb2[R, kc * P:(kc + 1) * P],
                                                                 identity=idb[R, R]),
                              reads=[r_hb2, r_idb], writes=[r_pbT])
                    tr.op("act", lambda e: e.activation(
                        out=h2s[:, :, 0:n], in_=pbT[:].rearrange("p (k c) -> p k c", k=8)[:, :, 0:n],
                        func=AF.Copy), reads=[r_pbT], writes=[r_h2s])
                    tr.dma("sp", h2d[:, :, c0:c0 + n], h2s[:, :, 0:n], reads=[r_h2s], writes=[r_h2d[to]])
                    for kc in range(8):
                        tr.op("pe", lambda e, kc=kc: e.matmul(
                            pb[0][R, 0:NE], lhsT=h2s[:, kc, 0:n], rhs=RWB[:, kc, :],
                            start=(kc == 0), stop=(kc == 7)), reads=[r_h2s, r_RWB], writes=[r_pb[0]])
                    tr.op("act", lambda e: e.activation(out=sc[R, :], in_=pb[0][R, 0:NE], func=AF.Sigmoid),
                          reads=[r_pb[0]], writes=[r_sc2])
                    tr.op("dve", lambda e: e.tensor_tensor(out=bi[R, :], in0=sc[R, :], in1=RB[R, :], op=ALU.add),
                          reads=[r_sc2, r_RB], writes=[r_bi])
                    for g in range(8):
                        tr.op("dve", lambda e, g=g: e.max(out=m8g[R, g, :], in_=bi[R, g * 8:(g + 1) * 8]),
                              reads=[r_bi], writes=[r_m8g])
                    tr.op("dve", lambda e: e.tensor_tensor(out=gsr[R, :].unsqueeze(2), in0=m8g[R, :, 0:1],
                                                           in1=m8g[R, :, 1:2], op=ALU.add),
                          reads=[r_m8g], writes=[r_gsr])
                    tr.op("dve", lambda e: e.max(out=m8g[R, 0, :], in_=gsr[R, :]), reads=[r_gsr, r_m8g],
                          writes=[r_m8g])
                    tr.op("dve", lambda e: e.tensor_scalar(out=gm[R, :], in0=gsr[R, :], scalar1=m8g[R, 0, 3:4],
                                                           scalar2=None, op0=ALU.is_ge),
                          reads=[r_gsr, r_m8g], writes=[r_gm])
                    tr.op("dve", lambda e: e.scalar_tensor_tensor(
                        out=msk[R, :].rearrange("p (g k) -> p g k", g=8),
                        in0=bi[R, :].rearrange("p (g k) -> p g k", g=8), scalar=2.0,
                        in1=gm[R, :].unsqueeze(2).broadcast_to([n, 8, 8]), op0=ALU.add, op1=ALU.mult),
                        reads=[r_bi, r_gm], writes=[r_msk])
                    tr.op("dve", lambda e: e.max(out=m8g[R, 1, :], in_=msk[R, :]), reads=[r_msk, r_m8g],
                          writes=[r_m8g])
                    tr.op("dve", lambda e: e.tensor_scalar(out=em[R, :], in0=msk[R, :], scalar1=m8g[R, 1, 7:8],
                                                           scalar2=None, op0=ALU.is_ge),
                          reads=[r_msk, r_m8g], writes=[r_em])
                    tr.op("dve", lambda e: e.tensor_tensor(out=wv[R, :], in0=sc[R, :], in1=em[R, :], op=ALU.mult),
                          reads=[r_sc2, r_em], writes=[r_wv])
                    tr.op("dve", lambda e: e.tensor_reduce(out=st2[R, 6:7], in_=wv[R, :], axis=AX.X, op=ALU.add),
                          reads=[r_wv], writes=[r_st2])
                    tr.op("dve", lambda e: e.reciprocal(out=st2[R, 7:8], in_=st2[R, 6:7]), reads=[r_st2],
                          writes=[r_st2])
                    tr.op("dve", lambda e: e.tensor_scalar(out=GT[R, to, :], in0=wv[R, :], scalar1=st2[R, 7:8],
                                                           scalar2=2.5, op0=ALU.mult, op1=ALU.mult),
                          reads=[r_wv, r_st2], writes=[r_GT[to]])

                def outproj(n, xsrc_dram, att_ap, r_att, yn_ap, r_yn, to):
                    R = slice(0, n)
                    tr.dma("sp", xr[R, :], xsrc_dram, writes=[r_xr])
                    for hf in range(2):
                        bk = 1 + hf
                        for kc in range(8):
                            lhs = att_ap(kc) if kc < 4 else yn_ap(kc - 4)
                            rl = r_att if kc < 4 else r_yn
                            tr.op("pe", lambda e, kc=kc, lhs=lhs, bk=bk, hf=hf: e.matmul(
                                pb[bk][R, :], lhsT=lhs, rhs=WOUT[:, kc, hf * 512:(hf + 1) * 512],
                                start=(kc == 0), stop=(kc == 7)), reads=[rl, r_WOUT], writes=[r_pb[bk]])
                        tr.op("dve", lambda e, bk=bk, hf=hf: e.tensor_tensor(
                            out=tmpf[R, hf * 512:(hf + 1) * 512], in0=pb[bk][R, :],
                            in1=bGM[R, hf * 512:(hf + 1) * 512], op=ALU.mult),
                            reads=[r_pb[bk], r_bGM], writes=[r_tmpf])
                    tr.op("pool", lambda e: e.tensor_tensor(out=xm[R, :], in0=tmpf[R, :], in1=xr[R, :], op=ALU.add),
                          reads=[r_tmpf, r_xr], writes=[r_xm])
                    tr.dma("sp", xmid[to * P:to * P + n, :], xm[R, :], reads=[r_xm], writes=[r_xmid[to]])
                    ffn_pre(xm, r_xm, n, to)

                import types
                return types.SimpleNamespace(attn_finish=attn_finish, ffn_pre=ffn_pre, outproj=outproj,
                                             AOG=AOG, r_AOG=r_AOG, AT=AT, r_AT=r_AT, st2=st2, r_st2=r_st2,
                                             hb2=hb2, r_hb2=r_hb2, atb=atb, r_atb=r_atb)

            with contextlib.ExitStack() as es:
                C = p2_common(es, "p", [(0, 0, P)])
                BL = sb(es, "BL", [P, 8, 3, 16]); r_BL = Res()
                tr.dma("sp", BL[:].rearrange("p a b c -> p (a b c)"), blkc.partition_broadcast(P), writes=[r_BL])
                TRI = sb(es, "TRI", [P, P], BF16); r_TRI = Res()
                tr.dma("pool", TRI[:], trim[:, :], writes=[r_TRI])
                OH = sb(es, "OH", [P, 16 * P], BF16); r_OH = Res()
                tr.op("pool", lambda e: e.memset(OH[:], 0.0), writes=[r_OH])
                tr.dma("pool", OH[0:16, :], ohm[:, :], reads=[r_OH], writes=[r_OH])
                QTz = sb(es, "QTz", [P, NH, 256], BF16); r_QTz = Res()
                tr.op("pool", lambda e: e.memset(QTz[:], 0.0), writes=[r_QTz])
                gbs = sb(es, "gbs", [P, NH, 16]); r_gbs = Res()
                m8 = sb(es, "m8", [P, NH, 8]); r_m8 = Res()
                mbf = sb(es, "mbf", [P, NH, 16]); r_mbf = Res()
                mbb = sb(es, "mbb", [P, NH, 16], BF16); r_mbb = Res()
                MBT = sb(es, "MBT", [P, NH, 256], BF16); r_MBT = Res()
                tr.op("pool", lambda e: e.memset(MBT[:], 0.0), writes=[r_MBT])
                PT = [sb(es, f"PT{i}", [P, 256], BF16) for i in range(4)]; r_PT = [Res() for _ in range(4)]
                OA = sb(es, "OA", [P, 2, NH, 65]); r_OA = Res()
                rc = sb(es, "rc", [P, 2, NH]); r_rc = Res()
                ATT = sb(es, "ATT", [P, 4, 256], BF16); r_ATT = [Res(), Res()]
                pcnt = [0]

                def attention(jb):
                    t0 = 16 + 2 * jb
                    qoff = jb * 256
                    rq = [r_QTz]
                    for h in range(NH):
                        pr, p0 = h // 2, 64 * (h % 2)
                        tr.op("pool", lambda e, h=h, pr=pr, p0=p0: e.tensor_copy(
                            out=QTz[p0:p0 + 64, h, :], in_=QTA[p0:p0 + 64, pr, qoff:qoff + 256]),
                            reads=[r_QTA[2 * jb], r_QTA[2 * jb + 1], r_QTz], writes=[r_QTz])
                    for i in range(2):
                        bk = 3 + i
                        for h in range(NH):
                            pr, p0 = h // 2, 64 * (h % 2)
                            tr.op("pe", lambda e, h=h, pr=pr, bk=bk, i=i: e.matmul(
                                pb[bk][:, h * 16:(h + 1) * 16],
                                lhsT=QTz[:, h, i * P:(i + 1) * P],
                                rhs=KM[:, pr, :], start=True, stop=True),
                                reads=[r_QTz, r_KM], writes=[r_pb[bk]])
                        tr.op("dve", lambda e, bk=bk: e.tensor_tensor(
                            out=gbs[:], in0=pb[bk][:, 0:128].rearrange("p (h n) -> p h n", h=NH),
                            in1=BL[:, jb, 0, :].unsqueeze(1).broadcast_to([P, NH, 16]), op=ALU.add),
                            reads=[r_pb[bk], r_BL], writes=[r_gbs])
                        for h in range(NH):
                            tr.op("dve", lambda e, h=h: e.max(out=m8[:, h, :], in_=gbs[:, h, :]),
                                  reads=[r_gbs], writes=[r_m8])
                        for h in range(NH):
                            tr.op("dve", lambda e, h=h: e.tensor_scalar(
                                out=mbf[:, h, :], in0=gbs[:, h, :], scalar1=m8[:, h, 2:3], scalar2=NEG,
                                op0=ALU.is_lt, op1=ALU.mult), reads=[r_gbs, r_m8], writes=[r_mbf])
                        tr.op("dve", lambda e: e.tensor_tensor(
                            out=mbf[:], in0=mbf[:], in1=BL[:, jb, 1, :].unsqueeze(1).broadcast_to([P, NH, 16]),
                            op=ALU.mult), reads=[r_mbf, r_BL], writes=[r_mbf])
                        tr.op("dve", lambda e: e.tensor_tensor(
                            out=mbb[:], in0=mbf[:], in1=BL[:, jb, 2, :].unsqueeze(1).broadcast_to([P, NH, 16]),
                            op=ALU.add), reads=[r_mbf, r_BL], writes=[r_mbb])
                        for h in range(NH):
                            tr.op("pe", lambda e, h=h: e.transpose(out=pbT[0:16, h * P:(h + 1) * P],
                                                                   in_=mbb[:, h, :], identity=idb[:]),
                                  reads=[r_mbb, r_idb], writes=[r_pbT])
                        tr.op("dve", lambda e, i=i: e.tensor_copy(
                            out=MBT[0:16, :, i * P:(i + 1) * P],
                            in_=pbT[0:16, :].rearrange("p (h c) -> p h c", h=NH)),
                            reads=[r_pbT, r_MBT], writes=[r_MBT])
                    nkt = t0 + 2
                    if "L1" in stages:
                        return
                    for h in range(NH):
                        pr, p0 = h // 2, 64 * (h % 2)
                        reg = (h % 2) * 65
                        for kt in range(nkt):
                            c0 = 0 if kt <= t0 else P
                            bk = 3 + (pcnt[0] % 2)
                            pt = PT[pcnt[0] % 4]; rpt_ = r_PT[pcnt[0] % 4]
                            pcnt[0] += 1
                            diag = kt >= t0
                            nb = kt // 2
                            tr.op("pe", lambda e, kt=kt, c0=c0, bk=bk, pr=pr, h=h: e.matmul(
                                pb[bk][:, c0:256], lhsT=KT[:, pr, kt * P:(kt + 1) * P],
                                rhs=QTz[:, h, c0:256], start=True, stop=False),
                                reads=[r_KT[kt]] + rq, writes=[r_pb[bk]])
                            tr.op("pe", lambda e, nb=nb, c0=c0, bk=bk, h=h, diag=diag: e.matmul(
                                pb[bk][:, c0:256], lhsT=OH[:, nb * P:(nb + 1) * P],
                                rhs=MBT[:, h, c0:256], start=False, stop=(not diag)),
                                reads=[r_OH, r_MBT], writes=[r_pb[bk]])
                            if diag:
                                dc = (kt - t0) * P
                                tr.op("pe", lambda e, bk=bk, dc=dc: e.matmul(
                                    pb[bk][:, dc:dc + P], lhsT=idb[:], rhs=TRI[:], start=False, stop=True),
                                    reads=[r_idb, r_TRI], writes=[r_pb[bk]])
                            tr.op("act", lambda e, bk=bk, c0=c0, pt=pt: e.activation(
                                out=pt[:, c0:256], in_=pb[bk][:, c0:256], func=AF.Exp),
                                reads=[r_pb[bk]], writes=[rpt_])
                            for i in range(2):
                                if kt > t0 + i:
                                    continue
                                tr.op("pe", lambda e, i=i, kt=kt, pt=pt, h=h, reg=reg: e.matmul(
                                    pb[5 + i][:, reg:reg + 65], lhsT=pt[:, i * P:(i + 1) * P],
                                    rhs=VA[:, kt, h, :], start=(kt == 0), stop=(kt == t0 + i)),
                                    reads=[rpt_, r_VA[kt]], writes=[r_pb[5 + i]])
                        for i in range(2):
                            tr.op("act", lambda e, i=i, h=h, reg=reg: e.activation(
                                out=OA[:, i, h, :], in_=pb[5 + i][:, reg:reg + 65], func=AF.Copy),
                                reads=[r_pb[5 + i]], writes=[r_OA])
                    if "L2" in stages:
                        return
                    tr.op("dve", lambda e: e.reciprocal(out=rc[:], in_=OA[:, :, :, 64]), reads=[r_OA],
                          writes=[r_rc])
                    for i in range(2):
                        C.attn_finish(OA[:, i, :, 0:64], rc[:, i, :], P, [r_OA, r_rc],
                                    ATT[:, :, i * P:(i + 1) * P], r_ATT[i])

                if DO_ATT:
                    for jb in range(8):
                        attention(jb)
                        if "L1" in stages or "L2" in stages or "L3" in stages:
                            continue
                        for i in range(2):
                            t = 16 + 2 * jb + i
                            to = t - NCT
                            C.outproj(P, xp[t * P:(t + 1) * P, :],
                                    lambda kc, i=i: ATT[:, kc, i * P:(i + 1) * P], r_ATT[i],
                                    lambda kc, to=to: YNA[:, kc, to * P:(to + 1) * P], r_YNA[to // 4], to)
        tr.barrier()
        if DO_SMP:
            with contextlib.ExitStack() as es:
                C = p2_common(es, "s", SROWS)
                KTs = sb(es, "KTs", [P, 4, 64 * P], BF16); r_KTs = Res()
                PTs = sb(es, "PTs", [P, 64, 64], BF16); r_PTs = [Res() for _ in range(8)]
                KP = [sb(es, f"KP{i}", [P, 512]) for i in range(2)]; r_KP = [Res(), Res()]
                VP = [sb(es, f"VP{i}", [P, 512]) for i in range(2)]; r_VP = [Res(), Res()]
                VPb = [sb(es, f"VPb{i}", [P, 512], BF16) for i in range(2)]; r_VPb = [Res(), Res()]
                KS = sb(es, "KS", [P, 4, 64]); r_KS = Res()
                KSt = sb(es, "KSt", [P, 4, 32]); r_KSt = Res()
                KMs = sb(es, "KMs", [P, 4, 32], BF16); r_KMs = Res()
                QZ = sb(es, "QZ", [P, 4, 64], BF16); r_QZ = Res()
                OHS = sb(es, "OHS", [P, 32 * P], BF16); r_OHS = Res()
                tr.op("pool", lambda e: e.memset(OHS[:], 0.0), writes=[r_OHS])
                for hh_ in range(2):
                    tr.dma("pool", OHS[0:32, hh_ * 2048:(hh_ + 1) * 2048], ohs[:, hh_ * 2048:(hh_ + 1) * 2048],
                           reads=[r_OHS], writes=[r_OHS])
                TRSz = sb(es, "TRSz", [P, 4, 64], BF16); r_TRSz = Res()
                tr.op("pool", lambda e: e.memset(TRSz[:], 0.0), writes=[r_TRSz])
                tr.dma("pool", TRSz[0:32, :, :].rearrange("p a b -> p (a b)"), trs[:, :], reads=[r_TRSz],
                       writes=[r_TRSz])
                SEL = sb(es, "SEL", [P, 64]); r_SEL = Res()
                tr.dma("sp", SEL[:], selq[:, :], writes=[r_SEL])
                DM = sb(es, "DM", [64, 512]); r_DM = Res()
                tr.dma("sp", DM[:], dmask[:, :], writes=[r_DM])
                PAR = sb(es, "PAR", [64, 2]); r_PAR = Res()
                tr.dma("sp", PAR[:], par[:, :], writes=[r_PAR])
                AOG2 = sb(es, "AOG2", [64, 64]); r_AOG2 = Res()
                for h in range(NH):
                    tr.dma("sp", AOG2[h * 8:(h + 1) * 8, :], aog[h * 64:(h + 1) * 64].partition_broadcast(8),
                           writes=[r_AOG2])
                IOT = sb(es, "IOT", [P, 1]); r_IOT = Res()
                tr.dma("sp", IOT[:], iot[:, :], writes=[r_IOT])
                PTI = sb(es, "PTI", [P, 64], I32); r_PTI = Res()
                PTF = sb(es, "PTF", [P, 64]); r_PTF = Res()
                IDX = sb(es, "IDX", [P, 64], I32); r_IDX = Res()
                gbS = sb(es, "gbS", [64, 32]); r_gbS = Res()
                m8S = sb(es, "m8S", [64, 8]); r_m8S = Res()
                mbS = sb(es, "mbS", [P, 32], BF16); r_mbS = Res()
                tr.op("pool", lambda e: e.memset(mbS[:], 0.0), writes=[r_mbS])
                MBTs = sb(es, "MBTs", [P, 64], BF16); r_MBTs = Res()
                tr.op("pool", lambda e: e.memset(MBTs[:], 0.0), writes=[r_MBTs])
                PTn = sb(es, "PTn", [P, 64], BF16); r_PTn = Res()
                tr.op("pool", lambda e: e.memset(PTn[:], 0.0), writes=[r_PTn])
                ones_b = sb(es, "ones_b", [P, 2], BF16); r_onesb = Res()
                tr.op("pool", lambda e: e.memset(ones_b[:], 1.0), writes=[r_onesb])
                O2 = sb(es, "O2", [64, 512]); r_O2 = Res()
                AT2 = sb(es, "AT2", [64, 64]); r_AT2 = Res()
                FU = sb(es, "FU", [64, 64]); r_FU = Res()
                rs = sb(es, "rs", [P, 8]); r_rs = Res()
                tr.op("pool", lambda e: e.memset(rs[:], 0.0), writes=[r_rs])
                TP = sb(es, "TP", [P, P], BF16); r_TP = Res()
                tr.op("pool", lambda e: e.memset(TP[:], 0.0), writes=[r_TP])
                ATTs = sb(es, "ATTs", [P, 4, NS], BF16); r_ATTs = Res()

                for s in range(4):
                    if "S00" in stages:
                        continue
                    tr.dma("sp", PTI[:], ptab[s, :].partition_broadcast(P), writes=[r_PTI])
                    tr.op("dve", lambda e: e.tensor_copy(out=PTF[:], in_=PTI[:]), reads=[r_PTI], writes=[r_PTF])
                    tr.op("dve", lambda e: e.tensor_scalar(out=PTF[:], in0=PTF[:], scalar1=128.0,
                                                           scalar2=IOT[:, 0:1], op0=ALU.mult, op1=ALU.add),
                          reads=[r_PTF, r_IOT], writes=[r_PTF])
                    tr.op("dve", lambda e: e.tensor_copy(out=IDX[:], in_=PTF[:]), reads=[r_PTF], writes=[r_IDX])
                    tr.op("pool", lambda e: e.memset(QZ[:], 0.0), writes=[r_QZ])
                    for h in range(NH):
                        pr, p0 = h // 2, 64 * (h % 2)
                        tr.op("pool", lambda e, h=h, pr=pr, p0=p0, s=s: e.tensor_copy(
                            out=QZ[p0:p0 + 64, pr, h * 8:(h + 1) * 8], in_=QTs[p0:p0 + 64, pr, s * 8:(s + 1) * 8]),
                            reads=[r_QTs, r_QZ], writes=[r_QZ])
                    if "S0" in stages:
                        continue
                    for j in range(64):
                        kp = KP[j % 2]; rkp = r_KP[j % 2]
                        tr.gather(kp[:, :], cache_k[:, :], IDX[:, j:j + 1], reads=[r_IDX], writes=[rkp])
                        bk = j % 2
                        for pr in range(4):
                            tr.op("pe", lambda e, pr=pr, kp=kp, bk=bk: e.transpose(
                                out=pb[bk][:, pr * P:(pr + 1) * P], in_=kp[:, pr * P:(pr + 1) * P], identity=idf[:]),
                                reads=[rkp, r_idf], writes=[r_pb[bk]])
                        tr.op("act", lambda e, j=j, bk=bk: e.activation(
                            out=KTs[:, :, j * P:(j + 1) * P], in_=pb[bk][:].rearrange("p (k c) -> p k c", k=4),
                            func=AF.Copy), reads=[r_pb[bk]], writes=[r_KTs])
                        tr.op("dve", lambda e, j=j, bk=bk: e.tensor_reduce(
                            out=KS[:, :, j], in_=pb[bk][:].rearrange("p (k c) -> p k c", k=4), axis=AX.X,
                            op=ALU.add), reads=[r_pb[bk]], writes=[r_KS])
                    ks4 = KS[:].rearrange("p k (n t) -> p k n t", t=2)
                    tr.op("dve", lambda e: e.tensor_tensor(out=KSt[:].unsqueeze(3), in0=ks4[:, :, :, 0:1],
                                                           in1=ks4[:, :, :, 1:2], op=ALU.add),
                          reads=[r_KS], writes=[r_KSt])
                    tr.op("dve", lambda e: e.tensor_scalar(out=KMs[:], in0=KSt[:], scalar1=1.0 / 256.0,
                                                           scalar2=None, op0=ALU.mult),
                          reads=[r_KSt], writes=[r_KMs])
                    if "S1" in stages:
                        continue
                    for pr in range(4):
                        tr.op("pe", lambda e, pr=pr: e.matmul(pb[2][0:64, 0:32], lhsT=QZ[:, pr, :], rhs=KMs[:, pr, :],
                                                              start=(pr == 0), stop=(pr == 3)),
                              reads=[r_QZ, r_KMs], writes=[r_pb[2]])
                    tr.op("dve", lambda e: e.tensor_copy(out=gbS[:], in_=pb[2][0:64, 0:32]), reads=[r_pb[2]],
                          writes=[r_gbS])
                    tr.op("dve", lambda e: e.max(out=m8S[:], in_=gbS[:]), reads=[r_gbS], writes=[r_m8S])
                    tr.op("dve", lambda e: e.tensor_scalar(out=mbS[0:64, :], in0=gbS[:], scalar1=m8S[:, 2:3],
                                                           scalar2=NEG, op0=ALU.is_lt, op1=ALU.mult),
                          reads=[r_gbS, r_m8S, r_mbS], writes=[r_mbS])
                    tr.op("pe", lambda e: e.transpose(out=pbT[0:32, 0:P], in_=mbS[:, :], identity=idb[:]),
                          reads=[r_mbS, r_idb], writes=[r_pbT])
                    tr.op("dve", lambda e: e.tensor_copy(out=MBTs[0:32, :], in_=pbT[0:32, 0:64]),
                          reads=[r_pbT, r_MBTs], writes=[r_MBTs])
                    if "S2" in stages:
                        continue
                    for cidx in range(8):
                        bk = 3 + (cidx % 2)
                        for jj in range(8):
                            j = cidx * 8 + jj
                            reg = pb[bk][:, jj * 64:(jj + 1) * 64]
                            nb = j // 2
                            tr.op("pe", lambda e, reg=reg, nb=nb: e.matmul(
                                reg, lhsT=OHS[:, nb * P:(nb + 1) * P], rhs=MBTs[:, :], start=True, stop=False),
                                reads=[r_OHS, r_MBTs], writes=[r_pb[bk]])
                            for pr in range(4):
                                tr.op("pe", lambda e, reg=reg, pr=pr, j=j: e.matmul(
                                    reg, lhsT=KTs[:, pr, j * P:(j + 1) * P], rhs=QZ[:, pr, :],
                                    start=False, stop=(pr == 3)), reads=[r_KTs, r_QZ], writes=[r_pb[bk]])
                        tr.op("act", lambda e, cidx=cidx, bk=bk: e.activation(
                            out=PTs[:, cidx * 8:(cidx + 1) * 8, :],
                            in_=pb[bk][:].rearrange("p (a b) -> p a b", a=8), func=AF.Exp),
                            reads=[r_pb[bk]], writes=[r_PTs[cidx]])
                    tr.op("pe", lambda e, s=s: e.matmul(pb[2][0:32, 128:192], lhsT=idb[:, 0:32], rhs=TRSz[:, s, :],
                                                        start=True, stop=False),
                          reads=[r_idb, r_TRSz], writes=[r_pb[2]])
                    for pr in range(4):
                        tr.op("pe", lambda e, pr=pr: e.matmul(pb[2][0:32, 128:192], lhsT=KTn[:, pr, :], rhs=QZ[:, pr, :],
                                                              start=False, stop=(pr == 3)),
                              reads=[r_KTn, r_QZ], writes=[r_pb[2]])
                    tr.op("act", lambda e: e.activation(out=PTn[0:32, :], in_=pb[2][0:32, 128:192], func=AF.Exp),
                          reads=[r_pb[2], r_PTn], writes=[r_PTn])
                    if "S3" in stages:
                        continue
                    for j in range(64):
                        vp = VP[j % 2]; rvp = r_VP[j % 2]
                        vb = VPb[j % 2]; rvb = r_VPb[j % 2]
                        tr.gather(vp[:, :], cache_v[:, :], IDX[:, j:j + 1], reads=[r_IDX], writes=[rvp])
                        tr.op("pool", lambda e, vp=vp, vb=vb: e.tensor_copy(out=vb[:], in_=vp[:]),
                              reads=[rvp], writes=[rvb])
                        tr.op("pe", lambda e, j=j, vb=vb: e.matmul(pb[5][0:64, :], lhsT=PTs[:, j, :], rhs=vb[:],
                                                                   start=(j == 0), stop=False),
                              reads=[r_PTs[j // 8], rvb], writes=[r_pb[5]])
                        tr.op("pe", lambda e, j=j: e.matmul(pb[6][0:64, 0:2], lhsT=PTs[:, j, :], rhs=ones_b[:, :],
                                                            start=(j == 0), stop=False),
                              reads=[r_PTs[j // 8], r_onesb], writes=[r_pb[6]])
                    tr.op("pe", lambda e: e.matmul(pb[5][0:64, :], lhsT=PTn[:, :], rhs=VNb[:, :],
                                                   start=False, stop=True),
                          reads=[r_PTn, r_VNb], writes=[r_pb[5]])
                    tr.op("pe", lambda e: e.matmul(pb[6][0:64, 0:2], lhsT=PTn[:, :], rhs=ones_b[:, :],
                                                   start=False, stop=True),
                          reads=[r_PTn, r_onesb], writes=[r_pb[6]])
                    if "S4" in stages:
                        continue
                    tr.op("act", lambda e: e.activation(out=O2[:], in_=pb[5][0:64, :], func=AF.Copy),
                          reads=[r_pb[5]], writes=[r_O2])
                    tr.op("dve", lambda e: e.reciprocal(out=rs[0:64, 0:1], in_=pb[6][0:64, 0:1]),
                          reads=[r_pb[6], r_rs], writes=[r_rs])
                    tr.op("dve", lambda e: e.tensor_tensor(out=O2[:], in0=O2[:], in1=DM[:], op=ALU.mult),
                          reads=[r_O2, r_DM], writes=[r_O2])
                    tr.op("dve", lambda e: e.tensor_reduce(
                        out=AT2[:], in_=O2[:].rearrange("p (h d) -> p d h", h=NH), axis=AX.X, op=ALU.add),
                        reads=[r_O2], writes=[r_AT2])
                    tr.op("dve", lambda e: e.tensor_scalar(out=AT2[:], in0=AT2[:], scalar1=rs[0:64, 0:1],
                                                           scalar2=None, op0=ALU.mult),
                          reads=[r_AT2, r_rs], writes=[r_AT2])
                    tr.op("act", lambda e: e.activation(out=FU[:], in_=AT2[:], func=AF.Square,
                                                        accum_out=rs[0:64, 1:2]),
                          reads=[r_AT2, r_rs], writes=[r_FU, r_rs])
                    tr.op("pe", lambda e: e.matmul(pb[2][0:64, 256:258], lhsT=SEL[:, :], rhs=rs[:, 1:3],
                                                   start=True, stop=True),
                          reads=[r_SEL, r_rs], writes=[r_pb[2]])
                    tr.op("act", lambda e: e.activation(out=rs[0:64, 3:4], in_=pb[2][0:64, 256:257], func=AF.Sqrt,
                                                        scale=1.0 / 512.0, bias=EPS),
                          reads=[r_pb[2], r_rs], writes=[r_rs])
                    tr.op("dve", lambda e: e.reciprocal(out=rs[0:64, 4:5], in_=rs[0:64, 3:4]), reads=[r_rs],
                          writes=[r_rs])
                    tr.op("dve", lambda e: e.scalar_tensor_tensor(
                        out=FU[:], in0=AT2[:], scalar=rs[0:64, 4:5], in1=AOG2[:], op0=ALU.mult, op1=ALU.mult),
                        reads=[r_AT2, r_rs, r_AOG2], writes=[r_FU])
                    for pp in range(2):
                        tr.op("dve", lambda e, pp=pp: e.tensor_scalar(
                            out=TP[0:64, pp * 64:(pp + 1) * 64], in0=FU[:], scalar1=PAR[:, pp:pp + 1],
                            scalar2=None, op0=ALU.mult), reads=[r_FU, r_PAR, r_TP], writes=[r_TP])
                    tr.op("pe", lambda e: e.transpose(out=pbT[:, 0:P], in_=TP[:, :], identity=idb[:]),
                          reads=[r_TP, r_idb], writes=[r_pbT])
                    tv = pbT[:, 0:64].rearrange("p (a b q) -> p a b q", a=4, b=2)
                    for pp in range(2):
                        tr.op("dve", lambda e, pp=pp, s=s: e.tensor_copy(
                            out=ATTs[pp * 64:(pp + 1) * 64, :, s * 8:(s + 1) * 8],
                            in_=tv[pp * 64:(pp + 1) * 64, :, pp, :]),
                            reads=[r_pbT, r_ATTs], writes=[r_ATTs])
                if not any(l in stages for l in ("S00", "S0", "S1", "S2", "S3", "S4", "S5")):
                    C.outproj(NS, xs[:, :], lambda kc: ATTs[:, kc, :], r_ATTs,
                              lambda kc: YNs[:, kc, :], r_YNs, NOT_)
        tr.barrier()
        if DO_MOE:
            with contextlib.ExitStack() as es:
                H2T = sb(es, "H2T", [P, 8, TOKS], BF16); r_H2T = Res()
                tr.dma("sp", H2T[:], h2d[:, :, :], reads=r_h2d, writes=[r_H2T])
                ACC = sb(es, "ACC", [P, NOT_ + 1, D]); r_ACC = [Res() for _ in range(NOT_ + 1)]
                tr.op("pool", lambda e: e.memset(ACC[:], 0.0), writes=r_ACC)
                ACTT = [sb(es, f"ACTT{i}", [P, 2, TOKS], BF16) for i in range(2)]
                r_ACTT = [[Res() for _ in range(5)] for _ in range(2)]
                WG = [sb(es, f"WG{i}", [P, 8, 512], BF16) for i in range(2)]; r_WG = [Res(), Res()]
                WD = [sb(es, f"WD{i}", [P, 2, D], BF16) for i in range(2)]; r_WD = [Res(), Res()]
                sg = [sb(es, f"sg{i}", [P, 512]) for i in range(2)]; r_sg = [Res(), Res()]
                groups = [(0, 512), (512, 512), (1024, 512), (1536, 512), (2048, 32)]
                gcnt = [0]; dcnt = [0]

                def moe_gu(e_):
                    wg = WG[e_ % 2]; rwg = r_WG[e_ % 2]
                    wd = WD[e_ % 2]; rwd = r_WD[e_ % 2]
                    tr.dma("pool", wg[:], w_gu[e_].rearrange("(kc p) n -> p kc n", p=P), writes=[rwg])
                    tr.dma("pool", wd[:], w_dn[e_].rearrange("(fc p) n -> p fc n", p=P), writes=[rwd])
                    at = ACTT[e_ % 2]
                    for gi, (c0, w) in enumerate(groups):
                        for fc in range(2):
                            b0 = 2 * (gcnt[0] % 2)
                            sgt = sg[gcnt[0] % 2]; rsg = r_sg[gcnt[0] % 2]
                            gcnt[0] += 1
                            for (bk, col) in ((b0, fc * P), (b0 + 1, 256 + fc * P)):
                                for kc in range(8):
                                    tr.op("pe", lambda e, kc=kc, bk=bk, col=col, c0=c0, w=w: e.matmul(
                                        pb[bk][:, 0:w], lhsT=wg[:, kc, col:col + P], rhs=H2T[:, kc, c0:c0 + w],
                                        start=(kc == 0), stop=(kc == 7)), reads=[rwg, r_H2T], writes=[r_pb[bk]])
                            tr.op("act", lambda e, b0=b0, w=w, sgt=sgt: e.activation(
                                out=sgt[:, 0:w], in_=pb[b0][:, 0:w], func=AF.Silu),
                                reads=[r_pb[b0]], writes=[rsg])
                            tr.op("dve", lambda e, b0=b0, w=w, sgt=sgt, fc=fc, c0=c0, at=at: e.tensor_tensor(
                                out=at[:, fc, c0:c0 + w], in0=sgt[:, 0:w], in1=pb[b0 + 1][:, 0:w], op=ALU.mult),
                                reads=[rsg, r_pb[b0 + 1]], writes=[r_ACTT[e_ % 2][gi]])

                def moe_down(e_):
                    wd = WD[e_ % 2]; rwd = r_WD[e_ % 2]
                    at = ACTT[e_ % 2]
                    for t in range(NOT_ + 1):
                        n = P if t < NOT_ else 32
                        gi = t // 4
                        for hf in range(2):
                            bk = 4 + (dcnt[0] % 3)
                            dcnt[0] += 1
                            for fc in range(2):
                                tr.op("pe", lambda e, fc=fc, bk=bk, hf=hf, t=t, n=n: e.matmul(
                                    pb[bk][0:n, :], lhsT=at[:, fc, t * P:t * P + n],
                                    rhs=wd[:, fc, hf * 512:(hf + 1) * 512], start=(fc == 0), stop=(fc == 1)),
                                    reads=[r_ACTT[e_ % 2][gi], rwd], writes=[r_pb[bk]])
                            acc = ACC[0:n, t, hf * 512:(hf + 1) * 512]
                            if e_ < NE:
                                tr.op("dve", lambda e, bk=bk, n=n, t=t, acc=acc: e.scalar_tensor_tensor(
                                    out=acc, in0=pb[bk][0:n, :], scalar=GT[0:n, t, e_:e_ + 1], in1=acc,
                                    op0=ALU.mult, op1=ALU.add),
                                    reads=[r_pb[bk], r_GT[t], r_ACC[t]], writes=[r_ACC[t]])
                            else:
                                tr.op("dve", lambda e, bk=bk, n=n, acc=acc: e.tensor_tensor(
                                    out=acc, in0=pb[bk][0:n, :], in1=acc, op=ALU.add),
                                    reads=[r_pb[bk], r_ACC[t]], writes=[r_ACC[t]])

                for e_ in range(NE + 1):
                    moe_gu(e_)
                    if e_ > 0:
                        moe_down(e_ - 1)
                moe_down(NE)

                bGF = sb(es, "bGF", [P, D]); r_bGF = Res()
                bFG = sb(es, "bFG", [P, D]); r_bFG = Res()
                xf = sb(es, "xf", [P, D]); r_xf = Res()
                yf = sb(es, "yf", [P, D]); r_yf = Res()
                jk = sb(es, "jk", [P, D], BF16); r_jk = Res()
                st3 = sb(es, "st3", [P, 4]); r_st3 = Res()
                tr.dma("sp", bFG[:], fing.partition_broadcast(P), writes=[r_bFG])
                for t in range(NOT_ + 1):
                    n = P if t < NOT_ else 32
                    R = slice(0, n)
                    if t == 0:
                        load_bc([(bGF, r_bGF, 5, None)], [(0, 0, P)], None)
                    if t == NOT_:
                        load_bc([(bGF, r_bGF, 5, None)], SROWS, None)
                    tr.dma("sp", xf[R, :], xmid[t * P:t * P + n, :], reads=[r_xmid[t]], writes=[r_xf])
                    tr.op("dve", lambda e, t=t: e.tensor_tensor(out=yf[R, :], in0=ACC[R, t, :], in1=bGF[R, :],
                                                                op=ALU.mult),
                          reads=[r_ACC[t], r_bGF], writes=[r_yf])
                    tr.op("pool", lambda e: e.tensor_tensor(out=yf[R, :], in0=yf[R, :], in1=xf[R, :], op=ALU.add),
                          reads=[r_yf, r_xf], writes=[r_yf])
                    tr.op("act", lambda e: e.activation(out=jk[R, :], in_=yf[R, :], func=AF.Square,
                                                        accum_out=st3[R, 0:1]),
                          reads=[r_yf], writes=[r_jk, r_st3])
                    tr.op("act", lambda e: e.activation(out=st3[R, 1:2], in_=st3[R, 0:1], func=AF.Sqrt,
                                                        scale=1.0 / D, bias=EPS), reads=[r_st3], writes=[r_st3])
                    tr.op("dve", lambda e: e.reciprocal(out=st3[R, 2:3], in_=st3[R, 1:2]),
                          reads=[r_st3], writes=[r_st3])
                    tr.op("dve", lambda e: e.scalar_tensor_tensor(
                        out=xf[R, :], in0=yf[R, :], scalar=st3[R, 2:3], in1=bFG[R, :],
                        op0=ALU.mult, op1=ALU.mult), reads=[r_yf, r_st3, r_bFG], writes=[r_xf])
                    ro = Res()
                    if t < NOT_:
                        tr.dma("sp", o_y[t * P:(t + 1) * P, :], xf[:, :], reads=[r_xf], writes=[ro])
                    else:
                        tr.dma("sp", o_ys[:, :], xf[R, :], reads=[r_xf], writes=[ro])
                    out_res.append(ro)
        tr.finish(out_res)
    return nc


def _rope_table(pos):
    half = 32
    inv = (10000.0 ** (-(np.arange(half, dtype=np.float32) / np.float32(half)))).astype(np.float32)
    ang = pos.astype(np.float32)[:, None] * inv[None, :]
    return np.concatenate([np.cos(ang), np.sin(ang)], axis=1).astype(np.float32)


def prep_inputs(inp):
    f = lambda a: np.ascontiguousarray(a, dtype=np.float32)
    maps = []
    S = 4096
    tri = np.where(np.arange(P)[:, None] <= np.arange(P)[None, :], 0.0, NEG).astype(np.float32)
    ohm = np.zeros((16, 16 * P), np.float32)
    for n in range(16):
        ohm[n, n * P:(n + 1) * P] = 1.0
    ohs = np.zeros((32, 32 * P), np.float32)
    for n in range(32):
        ohs[n, n * P:(n + 1) * P] = 1.0
    trs = np.full((32, 4, NH, 8), NEG, np.float32)
    for s in range(4):
        for tk in range(8):
            for tq in range(8):
                if tk <= tq:
                    trs[8 * s + tk, s, :, tq] = 0.0
    trs = trs.reshape(32, 256)
    selq = np.zeros((P, 64), np.float32)
    for h in range(NH):
        for h2 in range(NH):
            for q in range(8):
                selq[h * 8 + q, h2 * 8 + q] = 1.0
    dmask = np.zeros((64, NH, 64), np.float32)
    for h in range(NH):
        dmask[h * 8:(h + 1) * 8, h, :] = 1.0
    dmask = dmask.reshape(64, 512)
    par = np.zeros((64, 2), np.float32)
    for h in range(NH):
        par[h * 8:(h + 1) * 8, h % 2] = 1.0
    iot = np.arange(P, dtype=np.float32).reshape(P, 1)
    wab = np.zeros((P, 4, P), np.float32); wxb = np.zeros((P, 4, P), np.float32)
    ga = inp["gate_a_w"][0]; gx = inp["gate_x_w"][0]
    for ci in range(4):
        for j in range(2):
            wab[64 * j:64 * j + 64, ci, 64 * j:64 * j + 64] = ga[2 * ci + j]
            wxb[64 * j:64 * j + 64, ci, 64 * j:64 * j + 64] = gx[2 * ci + j]
    lv = np.zeros((P, 4, 12), np.float32)
    def fm(v):
        return v.reshape(4, P).T
    cw = inp["conv_w"][0]
    for jj in range(4):
        lv[:, :, jj] = fm(cw[jj])
    lv[:, :, 4] = fm(inp["conv_b"][0]); lv[:, :, 5] = fm(inp["gate_a_b"][0])
    lv[:, :, 6] = fm(inp["gate_x_b"][0]); lv[:, :, 7] = fm(inp["lru_lambda"][0])
    lv[:, :, 8] = fm(inp["lru_out_g"][0])
    w_gu = np.concatenate([inp["exp_w_gu"][0], inp["shared_w_gu"]], axis=0)
    w_dn = np.concatenate([inp["exp_w_down"][0], inp["shared_w_down"]], axis=0)
    ck = inp["cache_k"][0].reshape(2560 * P, 512)
    cv = inp["cache_v"][0].reshape(2560 * P, 512)
    ropes = _rope_table(8192 + np.arange(8))
    for c in range(8):
        b, half = c // 2, c % 2
        x = inp["x_prompt"][b]
        if half == 1:
            xpl = x
            pos = np.arange(S)
        else:
            xpl = np.concatenate([np.zeros((2048, D), np.float32), x[:2048]], axis=0)
            pos = np.concatenate([np.zeros(2048, np.int64), np.arange(2048)])
        cs = np.concatenate([inp["c_prompt"][b:b + 1], inp["c_sample"][4 * c:4 * c + 4]], axis=0)
        c5 = cs.T.reshape(8, P, 5).transpose(1, 0, 2)
        bl = np.zeros((8, 3, 16), np.float32)
        for j in range(8):
            cur = 8 + j
            for n in range(16):
                valid_past = (n < cur) and (half == 1 or n >= 8)
                bl[j, 0, n] = 0.0 if valid_past else -1e30
                bl[j, 1, n] = 0.0 if n == cur else 1.0
                bl[j, 2, n] = 0.0 if (valid_past or n == cur) else NEG
        fl = np.zeros((P, 2), np.float32)
        fl[:, 0] = float(half); fl[:, 1] = 1.0 - float(half)
        sh = inp["state_h"][0, 4 * c:4 * c + 4]
        sc = inp["state_conv"][0, 4 * c:4 * c + 4]
        sth = sh.reshape(4, 4, P).transpose(2, 1, 0)
        stc = sc.reshape(4, 3, 4, P).transpose(3, 2, 0, 1)
        m = {
            "xp": f(xpl), "xs": f(inp["x_sample"][4 * c:4 * c + 4].reshape(32, D)), "c5": f(c5),
            "ada_w": f(inp["ada_w"][0]), "ada_b": f(inp["ada_b"][0]),
            "norm_mix_g": f(inp["norm_mix_g"][0]), "norm_ffn_g": f(inp["norm_ffn_g"][0]),
            "final_g": f(inp["final_g"]), "w_in": f(inp["w_in"][0]), "w_out": f(inp["w_out"][0]),
            "rope": _rope_table(pos), "ropes": ropes, "lruv": lv, "wab": wab, "wxb": wxb,
            "attn_out_g": f(inp["attn_out_g"][0]), "blkc": f(bl.reshape(-1)), "flags": fl,
            "trim": tri, "ohm": ohm, "router_w": f(inp["router_w"][0]),
            "router_bias": f(inp["router_bias"][0]), "w_gu": w_gu, "w_dn": w_dn,
            "cache_k": ck, "cache_v": cv,
            "ptab": np.ascontiguousarray(inp["page_table"][4 * c:4 * c + 4], dtype=np.int32),
            "sth": f(sth), "stc": f(stc), "ohs": ohs, "trs": trs, "iot": iot, "selq": selq, "dmask": dmask, "par": par,
        }
        maps.append(m)
    return maps


def assemble(results):
    y_p = np.zeros((4, 4096, D), np.float32)
    k_p = np.zeros((1, 4, 4096, NH, HD), np.float32)
    v_p = np.zeros((1, 4, 4096, NH, HD), np.float32)
    h_p = np.zeros((1, 4, 512), np.float32)
    c_p = np.zeros((1, 4, 3, 512), np.float32)
    y_s = np.zeros((32, 8, D), np.float32)
    k_s = np.zeros((1, 32, 8, NH, HD), np.float32)
    v_s = np.zeros((1, 32, 8, NH, HD), np.float32)
    h_s = np.zeros((1, 32, 512), np.float32)
    c_s = np.zeros((1, 32, 3, 512), np.float32)
    for c in range(8):
        b, half = c // 2, c % 2
        r = results[c]
        sl = slice(half * 2048, half * 2048 + 2048)
        y_p[b, sl] = r["o_y"]
        k_p[0, b, sl] = r["o_k"].reshape(2048, NH, HD)
        v_p[0, b, sl] = r["o_v"].reshape(2048, NH, HD)
        if half == 1:
            h_p[0, b] = r["o_h"]
            c_p[0, b] = r["o_c"]
        y_s[4 * c:4 * c + 4] = r["o_ys"].reshape(4, 8, D)
        k_s[0, 4 * c:4 * c + 4] = r["o_ks"].reshape(4, 8, NH, HD)
        v_s[0, 4 * c:4 * c + 4] = r["o_vs"].reshape(4, 8, NH, HD)
        h_s[0, 4 * c:4 * c + 4] = r["o_hs"]
        c_s[0, 4 * c:4 * c + 4] = r["o_cs"].reshape(4, 3, 512)
    return (y_p, y_s, k_p, v_p, h_p, c_p, k_s, v_s, h_s, c_s)


_NC_CACHE = {}


def kernel(**inputs):
    inp = {k: np.asarray(v) for k, v in inputs.items()}
    maps = prep_inputs(inp)
    if "nc" not in _NC_CACHE:
        _NC_CACHE["nc"] = build()
    res = run_bass_kernel_spmd(_NC_CACHE["nc"], maps, core_ids=list(range(8)))
    return assemble(res.results)
```

```python
import contextlib
import numpy as np
import concourse.bass as bass
import concourse.mybir as mybir
from concourse.bass_utils import run_bass_kernel_spmd

F32 = mybir.dt.float32
BF16 = mybir.dt.bfloat16
I32 = mybir.dt.int32
U32 = mybir.dt.uint32
ALU = mybir.AluOpType
AF = mybir.ActivationFunctionType
AX = mybir.AxisListType

SEM_LIMIT = 30000
P = 128
D = 1024
NH = 8
HD = 64
NCT = 16
NOT_ = 16
NTL = NCT + NOT_
NBL = 16
NEG = -30000.0
EPS = 1e-6
NE = 64
TOKS = NOT_ * P + 32


class Res:
    __slots__ = ("name", "w", "r", "excl")

    def __init__(self, name="", excl=False):
        self.name = name
        self.w = None
        self.r = {}
        self.excl = excl


class Eng:
    def __init__(self, tr, key, handle, step):
        self.tr = tr
        self.key = key
        self.h = handle
        self.step = step
        self.sems = []
        self.cnt = 0
        self.seen = {}
        self.new_epoch()

    def new_epoch(self):
        s = self.tr.nc.semaphore(f"s_{self.key}_{len(self.sems)}")
        self.sems.append(s.__enter__())
        self.tr._sem_guards.append(s)
        self.cnt = 0

    @property
    def epoch(self):
        return len(self.sems) - 1


class Tracker:
    def __init__(self, nc, ndma=8):
        self.nc = nc
        self._sem_guards = []
        self.e = {}
        for key, h in (("pe", nc.tensor), ("act", nc.scalar), ("dve", nc.vector),
                       ("pool", nc.gpsimd), ("sp", nc.sync)):
            self.e[key] = Eng(self, key, h, 1)
        self.dq = {}
        for q in ("sp", "act", "pool"):
            ring = []
            for i in range(ndma):
                eng = Eng(self, f"dq_{q}{i}", None, 16)
                ring.append(eng)
                self.e[eng.key] = eng
            self.dq[q] = [ring, 0]

    def _wait(self, eng, dep):
        k, ep, n = dep
        if eng.seen.get((k, ep), 0) >= n:
            return
        eng.seen[(k, ep)] = n
        eng.h.wait_ge(self.e[k].sems[ep], n)

    def _deps(self, reads, writes):
        deps = set()
        for res in reads:
            if res.w is not None:
                deps.add(res.w)
            if res.excl:
                for k, (ep, n) in res.r.items():
                    deps.add((k, ep, n))
        for res in writes:
            if res.w is not None:
                deps.add(res.w)
            for k, (ep, n) in res.r.items():
                deps.add((k, ep, n))
        return deps

    def op(self, engkey, fn, reads=(), writes=()):
        eng = self.e[engkey]
        for dep in sorted(self._deps(reads, writes)):
            if dep[0] == engkey and engkey == "pe":
                continue
            self._wait(eng, dep)
        if eng.cnt + 1 > SEM_LIMIT:
            eng.new_epoch()
        inst = fn(eng.h)
        eng.cnt += 1
        inst.then_inc(eng.sems[eng.epoch], 1)
        pos = (eng.epoch, eng.cnt)
        for res in writes:
            res.w = (engkey, pos[0], pos[1])
            res.r = {}
        for res in reads:
            res.r[engkey] = pos
        return inst

    def dma(self, q, out, in_, reads=(), writes=(), **kw):
        issuer = self.e[q]
        ring, idx = self.dq[q]
        deng = ring[idx % len(ring)]
        self.dq[q][1] = idx + 1
        if deng.cnt > 0:
            self._wait(issuer, (deng.key, deng.epoch, deng.cnt))
        for dep in sorted(self._deps(reads, writes)):
            self._wait(issuer, dep)
        if deng.cnt + 16 > SEM_LIMIT:
            deng.new_epoch()
        inst = issuer.h.dma_start(out=out, in_=in_, **kw)
        deng.cnt += 16
        inst.then_inc(deng.sems[deng.epoch], 16)
        pos = (deng.epoch, deng.cnt)
        for res in writes:
            res.w = (deng.key, pos[0], pos[1])
            res.r = {}
        for res in reads:
            res.r[deng.key] = pos
        return inst

    def gather(self, out, in_, idx, reads=(), writes=()):
        issuer = self.e["pool"]
        ring, i = self.dq["pool"]
        deng = ring[i % len(ring)]
        self.dq["pool"][1] = i + 1
        if deng.cnt > 0:
            self._wait(issuer, (deng.key, deng.epoch, deng.cnt))
        for dep in sorted(self._deps(reads, writes)):
            self._wait(issuer, dep)
        if deng.cnt + 16 > SEM_LIMIT:
            deng.new_epoch()
        inst = issuer.h.indirect_dma_start(out=out, out_offset=None, in_=in_,
                                           in_offset=bass.IndirectOffsetOnAxis(ap=idx, axis=0))
        deng.cnt += 16
        inst.then_inc(deng.sems[deng.epoch], 16)
        pos = (deng.epoch, deng.cnt)
        for res in writes:
            res.w = (deng.key, pos[0], pos[1])
            res.r = {}
        for res in reads:
            res.r[deng.key] = pos
        return inst

    def barrier(self):
        targets = [(k, e.epoch, e.cnt) for k, e in self.e.items() if e.cnt > 0]
        for key in ("pe", "act", "dve", "pool", "sp"):
            eng = self.e[key]
            for dep in targets:
                if dep[0] == key:
                    continue
                self._wait(eng, dep)

    def finish(self, outs):
        eng = self.e["sp"]
        for res in outs:
            if res.w is not None:
                self._wait(eng, res.w)
            for k, (ep, n) in res.r.items():
                self._wait(eng, (k, ep, n))

    def close(self):
        for g in reversed(self._sem_guards):
            g.__exit__(None, None, None)


def build(stages=("all",), npool=2560):
    nc = bass.Bass("TRN2", target_bir_lowering=False)
    tr = Tracker(nc)
    ALL = "all" in stages
    DO_ATT = ALL or "ATT" in stages
    DO_MOE = ALL or "MOE" in stages
    DO_SMP = ALL or "S" in stages

    def din(name, shape, dt=F32):
        return nc.dram_tensor(name, list(shape), dt, kind="ExternalInput").ap()

    def dout(name, shape, dt=F32):
        return nc.dram_tensor(name, list(shape), dt, kind="ExternalOutput").ap()

    def dscr(name, shape, dt=F32):
        return nc.dram_tensor(name, list(shape), dt, kind="Internal").ap()

    xp = din("xp", [NTL * P, D])
    xs = din("xs", [32, D])
    c5 = din("c5", [P, 8, 5])
    ada_w = din("ada_w", [D, 6 * D])
    ada_b = din("ada_b", [6 * D])
    nmg = din("norm_mix_g", [D])
    nfg = din("norm_ffn_g", [D])
    fing = din("final_g", [D])
    w_in = din("w_in", [D, 2560])
    w_out = din("w_out", [D, D])
    rope = din("rope", [NTL * P, 64])
    ropes = din("ropes", [8, 64])
    lruv = din("lruv", [P, 4, 12])
    wab = din("wab", [P, 4, P])
    wxb = din("wxb", [P, 4, P])
    aog = din("attn_out_g", [512])
    blkc = din("blkc", [8 * 3 * 16])
    flags = din("flags", [P, 2])
    trim = din("trim", [P, P])
    ohm = din("ohm", [16, 16 * P])
    router_w = din("router_w", [D, NE])
    router_b = din("router_bias", [NE])
    if DO_MOE:
        w_gu = din("w_gu", [NE + 1, D, 512])
        w_dn = din("w_dn", [NE + 1, 256, D])
    if DO_SMP:
        cache_k = din("cache_k", [npool * P, 512])
        cache_v = din("cache_v", [npool * P, 512])
    ptab = din("ptab", [4, 64], I32)
    sth = din("sth", [P, 4, 4])
    stc = din("stc", [P, 4, 4, 3])
    ohs = din("ohs", [32, 32 * P])
    trs = din("trs", [32, 256])
    selq = din("selq", [P, 64])
    dmask = din("dmask", [64, 512])
    par = din("par", [64, 2])
    iot = din("iot", [P, 1])

    o_y = dout("o_y", [NOT_ * P, D])
    o_k = dout("o_k", [NOT_ * P, 512])
    o_v = dout("o_v", [NOT_ * P, 512])
    o_h = dout("o_h", [512])
    o_c = dout("o_c", [3, 512])
    o_ys = dout("o_ys", [32, D])
    o_ks = dout("o_ks", [32, 512])
    o_vs = dout("o_vs", [32, 512])
    o_hs = dout("o_hs", [4, 512])
    o_cs = dout("o_cs", [12, 512])

    modd = dscr("modd", [5, 6 * D])
    xmid = dscr("xmid", [TOKS, D])
    h2d = dscr("h2d", [P, 8, TOKS], BF16)

    out_res = []
    r_xmid = [Res() for _ in range(NOT_ + 1)]
    r_h2d = [Res() for _ in range(NOT_ + 1)]

    with contextlib.ExitStack() as top:
        def sb(es, name, shape, dt=F32):
            return es.enter_context(nc.sbuf_tensor(name, list(shape), dt))

        def ps(es, name, shape, dt=F32):
            return es.enter_context(nc.psum_tensor(name, list(shape), dt))

        pbT = ps(top, "pbT", [P, 1024], BF16); r_pbT = Res(excl=True)
        pb = [ps(top, f"pb{i}", [P, 512], F32) for i in range(7)]
        r_pb = [Res(excl=True) for _ in range(7)]

        idb = sb(top, "idb", [P, P], BF16); r_idb = Res()
        idf = sb(top, "idf", [P, P], F32); r_idf = Res()
        ones_f = sb(top, "ones_f", [P, P], F32); r_ones = Res()
        GT = sb(top, "GT", [P, NOT_ + 1, NE], F32); r_GT = [Res() for _ in range(NOT_ + 1)]

        tr.op("pool", lambda e: e.memset(idb[:], 0.0), writes=[r_idb])
        tr.op("pool", lambda e: e.affine_select(out=idb[:], in_=idb[:], pattern=[[-1, P]],
                                                 compare_op=ALU.not_equal, fill=1.0, base=0,
                                                 channel_multiplier=1), reads=[r_idb], writes=[r_idb])
        tr.op("pool", lambda e: e.memset(idf[:], 0.0), writes=[r_idf])
        tr.op("pool", lambda e: e.affine_select(out=idf[:], in_=idf[:], pattern=[[-1, P]],
                                                 compare_op=ALU.not_equal, fill=1.0, base=0,
                                                 channel_multiplier=1), reads=[r_idf], writes=[r_idf])
        tr.op("pool", lambda e: e.memset(ones_f[:], 1.0), writes=[r_ones])

        r_modd = Res()
        with contextlib.ExitStack() as es:
            c5t = sb(es, "c5t", [P, 8, 5]); r_c5 = Res()
            sct = sb(es, "sct", [P, 8, 5]); r_sc = Res()
            adab = sb(es, "adab", [5, 6 * D]); r_adab = Res()
            modt = sb(es, "modt", [5, 6 * D]); r_modt = Res()
            aw = [sb(es, f"aw{i}", [P, 8, 512]) for i in range(2)]
            r_aw = [Res(), Res()]
            tr.dma("sp", c5t[:], c5[:, :, :], writes=[r_c5])
            tr.dma("sp", adab[:], ada_b.partition_broadcast(5), writes=[r_adab])
            tr.op("act", lambda e: e.activation(out=sct[:], in_=c5t[:], func=AF.Silu),
                  reads=[r_c5], writes=[r_sc])
            awv = ada_w.rearrange("(kc p) n -> p kc n", p=P)
            for cc in range(12):
                a = aw[cc % 2]; ra = r_aw[cc % 2]
                tr.dma("sp", a[:], awv[:, :, cc * 512:(cc + 1) * 512], writes=[ra])
                bank = pb[cc % 2]; rb = r_pb[cc % 2]
                for kc in range(8):
                    tr.op("pe", lambda e, kc=kc, a=a, bank=bank: e.matmul(
                        bank[0:5, :], lhsT=sct[:, kc, :], rhs=a[:, kc, :],
                        start=(kc == 0), stop=(kc == 7)), reads=[r_sc, ra], writes=[rb])
                tr.op("dve", lambda e, bank=bank, cc=cc: e.tensor_tensor(
                    out=modt[:, cc * 512:(cc + 1) * 512], in0=bank[0:5, :],
                    in1=adab[:, cc * 512:(cc + 1) * 512], op=ALU.add),
                    reads=[rb, r_adab], writes=[r_modt])
            tr.dma("sp", modd[:, :], modt[:], reads=[r_modt], writes=[r_modd])
        tr.barrier()

        def load_bc(specs, rows, scratch):
            gtmp, r_g = scratch if scratch is not None else (None, None)
            hi_all = max(r[2] for r in rows)
            for (tile, res, col, g) in specs:
                for (mr, lo, hi) in rows:
                    tr.dma("sp", tile[lo:hi, :],
                           modd[mr, col * D:(col + 1) * D].partition_broadcast(hi - lo),
                           reads=[r_modd], writes=[res])
                if g is not None:
                    tr.dma("sp", gtmp[0:hi_all, :], g.partition_broadcast(hi_all), writes=[r_g])
                    tr.op("dve", lambda e, tile=tile: e.scalar_tensor_tensor(
                        out=tile[0:hi_all, :], in0=tile[0:hi_all, :], scalar=1.0,
                        in1=gtmp[0:hi_all, :], op0=ALU.add, op1=ALU.mult),
                        reads=[res, r_g], writes=[res])

        SROWS = [(1 + s, 8 * s, 8 * s + 8) for s in range(4)]
        NS = 32
        QTs = sb(top, "QTs", [P, 4, NS], BF16); r_QTs = Res()
        KTn = sb(top, "KTn", [P, 4, NS], BF16); r_KTn = Res()
        VNb = sb(top, "VNb", [P, 512], BF16); r_VNb = Res()
        YNs = sb(top, "YNs", [P, 4, NS], BF16); r_YNs = Res()
        tr.op("pool", lambda e: e.memset(VNb[:], 0.0), writes=[r_VNb])

        with contextlib.ExitStack() as es0:
            KT = sb(es0, "KT", [P, 4, NTL * P], BF16); r_KT = [Res() for _ in range(NTL)]
            VA = sb(es0, "VA", [P, NTL, NH, 65], BF16); r_VA = [Res() for _ in range(NTL)]
            tr.op("pool", lambda e: e.memset(VA[:, :, :, 64:65], 1.0), writes=r_VA)
            QTA = sb(es0, "QTA", [P, 4, NOT_ * P], BF16); r_QTA = [Res() for _ in range(NOT_)]
            YNA = sb(es0, "YNA", [P, 4, NOT_ * P], BF16); r_YNA = [Res() for _ in range(4)]
            KM = sb(es0, "KM", [P, 4, 16], BF16); r_KM = Res()

            with contextlib.ExitStack() as es:
                bS1 = sb(es, "bS1", [P, D]); r_bS1 = Res()
                bG1 = sb(es, "bG1", [P, D]); r_bG1 = Res()
                WIN = sb(es, "WIN", [P, 8, 2560], BF16); r_WIN = Res()
                winv = w_in.rearrange("(kc p) n -> p kc n", p=P)
                for cc in range(5):
                    tr.dma("pool", WIN[:, :, cc * 512:(cc + 1) * 512], winv[:, :, cc * 512:(cc + 1) * 512],
                           writes=[r_WIN])
                xt = [sb(es, f"xt{i}", [P, D]) for i in range(2)]; r_xt = [Res() for _ in range(2)]
                hb = sb(es, "hb", [P, D], BF16); r_hb = Res()
                HT = sb(es, "HT", [P, 8, 512], BF16); r_HT = [Res() for _ in range(4)]
                rp = [sb(es, f"rp{i}", [P, 64]) for i in range(2)]; r_rp = [Res(), Res()]
                krb = sb(es, "krb", [P, 512], BF16); r_krb = Res()
                rt_full = sb(es, "rt", [P, NH, 32]); r_rt = Res()
                st = sb(es, "st", [P, 8]); r_st = Res()
                LT = sb(es, "LT", [P, 9, 512]); r_LT = [Res() for _ in range(9)]
                G = [LT[:, i, :] for i in range(9)]
                cv, rr, ii, a2, aa, bb, hh, gsb, uu = G
                r_cv, r_rr, r_ii, r_a2, r_aa, r_bb, r_hh, r_gsb, r_uu = r_LT
                qs, ks, vs, kr = G[0], G[1], G[2], G[3]
                r_qs, r_ks, r_vs, r_kr = r_LT[0], r_LT[1], r_LT[2], r_LT[3]
                qr, r_qr = G[4], r_LT[4]
                tmpf = LT[:, 5:7, :].rearrange("p a b -> p (a b)"); r_tmpf2 = [r_LT[5], r_LT[6]]
                XPc = sb(es, "XPc", [P, 515]); r_XPc = Res()
                XH = sb(es, "XH", [P, 4, 3]); r_XH = [Res() for _ in range(4)]
                HS = sb(es, "HS", [P, 4]); r_HS = [Res() for _ in range(4)]
                YY = sb(es, "YY", [P, 4, 512], BF16); r_YY = [Res() for _ in range(4)]
                YQ = sb(es, "YQ", [P, 512]); r_YQ = Res()
                tcol = sb(es, "tcol", [P, 4]); r_tcol = Res()
                LV = sb(es, "LV", [P, 4, 12]); r_LV = Res()
                WAB = sb(es, "WAB", [P, 4, P]); r_WAB = Res()
                WXB = sb(es, "WXB", [P, 4, P]); r_WXB = Res()
                LC1 = sb(es, "LC1", [P, 4]); r_LC1 = Res()
                KMf = sb(es, "KMf", [P, 4, 16]); r_KMf = Res()
                FL = sb(es, "FL", [P, 2]); r_FL = Res()
                tr.dma("sp", FL[:], flags[:, :], writes=[r_FL])
                tr.dma("sp", LV[:], lruv[:, :, :], writes=[r_LV])
                tr.dma("sp", WAB[:], wab[:, :, :], writes=[r_WAB])
                tr.dma("sp", WXB[:], wxb[:, :, :], writes=[r_WXB])
                tr.op("act", lambda e: e.activation(out=LC1[:], in_=LV[:, :, 7], func=AF.Exp, scale=-1.0),
                      reads=[r_LV], writes=[r_LC1])
                tr.op("act", lambda e: e.activation(out=LC1[:], in_=LC1[:], func=AF.Ln, bias=1.0),
                      reads=[r_LC1], writes=[r_LC1])
                tr.op("dve", lambda e: e.tensor_scalar(out=LC1[:], in0=LC1[:], scalar1=-8.0, scalar2=None,
                                                       op0=ALU.mult), reads=[r_LC1], writes=[r_LC1])
                tr.op("pool", lambda e: e.memset(KMf[:], 0.0), writes=[r_KMf])
                tr.op("pool", lambda e: e.memset(HS[:], 0.0), writes=r_HS)
                tr.op("pool", lambda e: e.memset(XH[:], 0.0), writes=r_XH)
                load_bc([(bS1, r_bS1, 0, None), (bG1, r_bG1, 1, nmg)], [(0, 0, P)], (xt[1], r_xt[1]))

                def rope_apply(src, r_src, dst, r_dst, rpt, rrp, n=P):
                    s3 = src[0:n, :].rearrange("p (h d) -> p h d", h=NH)
                    d3 = dst[0:n, :].rearrange("p (h d) -> p h d", h=NH)
                    cosb = rpt[0:n, 0:32].unsqueeze(1).broadcast_to([n, NH, 32])
                    sinb = rpt[0:n, 32:64].unsqueeze(1).broadcast_to([n, NH, 32])
                    rt = rt_full[0:n]
                    x1 = s3[:, :, 0:32]; x2 = s3[:, :, 32:64]
                    tr.op("pool", lambda e: e.tensor_tensor(out=d3[:, :, 0:32], in0=x1, in1=cosb, op=ALU.mult),
                          reads=[r_src, rrp], writes=[r_dst])
                    tr.op("pool", lambda e: e.tensor_tensor(out=rt, in0=x2, in1=sinb, op=ALU.mult),
                          reads=[r_src, rrp], writes=[r_rt])
                    tr.op("pool", lambda e: e.tensor_tensor(out=d3[:, :, 0:32], in0=d3[:, :, 0:32], in1=rt,
                                                            op=ALU.subtract),
                          reads=[r_dst, r_rt], writes=[r_dst])
                    tr.op("pool", lambda e: e.tensor_tensor(out=d3[:, :, 32:64], in0=x2, in1=cosb, op=ALU.mult),
                          reads=[r_src, rrp], writes=[r_dst])
                    tr.op("pool", lambda e: e.tensor_tensor(out=rt, in0=x1, in1=sinb, op=ALU.mult),
                          reads=[r_src, rrp], writes=[r_rt])
                    tr.op("pool", lambda e: e.tensor_tensor(out=d3[:, :, 32:64], in0=d3[:, :, 32:64], in1=rt,
                                                            op=ALU.add),
                          reads=[r_dst, r_rt], writes=[r_dst])

                def norm_to_HT(x, rx, n, j0):
                    R = slice(0, n)
                    tr.op("act", lambda e: e.activation(out=hb[R, :], in_=x[R, :], func=AF.Square,
                                                        accum_out=st[R, 0:1]),
                          reads=[rx], writes=[r_hb, r_st])
                    tr.op("act", lambda e: e.activation(out=st[R, 1:2], in_=st[R, 0:1], func=AF.Sqrt,
                                                        scale=1.0 / D, bias=EPS), reads=[r_st], writes=[r_st])
                    tr.op("dve", lambda e: e.reciprocal(out=st[R, 2:3], in_=st[R, 1:2]),
                          reads=[r_st], writes=[r_st])
                    tr.op("dve", lambda e: e.scalar_tensor_tensor(
                        out=tmpf[R, :], in0=x[R, :], scalar=st[R, 2:3], in1=bG1[R, :],
                        op0=ALU.mult, op1=ALU.mult), reads=[rx, r_st, r_bG1], writes=r_tmpf2)
                    tr.op("pool", lambda e: e.tensor_tensor(out=hb[R, :], in0=tmpf[R, :], in1=bS1[R, :], op=ALU.add),
                          reads=r_tmpf2 + [r_bS1], writes=[r_hb])
                    for kc in range(8):
                        tr.op("pe", lambda e, kc=kc: e.transpose(out=pbT[:, kc * P:kc * P + n],
                                                                 in_=hb[R, kc * P:(kc + 1) * P],
                                                                 identity=idb[R, R]),
                              reads=[r_hb, r_idb], writes=[r_pbT])
                    tr.op("act", lambda e: e.activation(
                        out=HT[:, :, j0:j0 + n], in_=pbT[:].rearrange("p (k c) -> p k c", k=8)[:, :, 0:n],
                        func=AF.Copy), reads=[r_pbT], writes=[r_HT[j0 // P]])

                def proj_tok(n, j0, which):
                    for (bi_, c0) in which:
                        for kc in range(8):
                            tr.op("pe", lambda e, kc=kc, bi_=bi_, c0=c0: e.matmul(
                                pb[bi_][0:n, :], lhsT=HT[:, kc, j0:j0 + n], rhs=WIN[:, kc, c0:c0 + 512],
                                start=(kc == 0), stop=(kc == 7)),
                                reads=[r_HT[j0 // P], r_WIN], writes=[r_pb[bi_]])

                def transpose4(srcb, r_srcb, n, dst, r_dst):
                    for pr in range(4):
                        tr.op("pe", lambda e, pr=pr: e.transpose(out=pbT[:, pr * P:pr * P + n],
                                                                 in_=srcb[0:n, pr * P:(pr + 1) * P],
                                                                 identity=idb[0:n, 0:n]),
                              reads=[r_srcb, r_idb], writes=[r_pbT])
                    tr.op("dve", lambda e: e.tensor_copy(
                        out=dst, in_=pbT[:, 0:512].rearrange("p (k c) -> p k c", k=4)[:, :, 0:n]),
                        reads=[r_pbT], writes=[r_dst])

                def stage_front(t):
                    x = xt[t % 2]; rx = r_xt[t % 2]
                    tr.dma("sp", x[:], xp[t * P:(t + 1) * P, :], writes=[rx])
                    rpt = rp[t % 2]; rrp = r_rp[t % 2]
                    tr.dma("sp", rpt[:], rope[t * P:(t + 1) * P, :], writes=[rrp])
                    norm_to_HT(x, rx, P, (t % 4) * P)

                def stage_a(t):
                    own = t >= NCT
                    rpt = rp[t % 2]; rrp = r_rp[t % 2]
                    j = t % 4
                    proj_tok(P, j * P, [(1, 512), (2, 1024)] + ([(0, 0)] if (own and DO_ATT) else []))
                    tr.op("act", lambda e: e.activation(out=ks, in_=pb[1][:], func=AF.Copy),
                          reads=[r_pb[1]], writes=[r_ks])
                    tr.op("act", lambda e: e.activation(out=vs, in_=pb[2][:], func=AF.Copy),
                          reads=[r_pb[2]], writes=[r_vs])
                    tr.op("pool", lambda e: e.tensor_copy(
                        out=VA[:, t, :, 0:64], in_=vs.rearrange("p (h d) -> p h d", h=NH)),
                        reads=[r_vs], writes=[r_VA[t]])
                    rope_apply(ks, r_ks, kr, r_kr, rpt, rrp)
                    tr.op("pool", lambda e: e.tensor_copy(out=krb[:], in_=kr), reads=[r_kr], writes=[r_krb])
                    transpose4(krb, r_krb, P, KT[:, :, t * P:(t + 1) * P], r_KT[t])
                    if own:
                        to = t - NCT
                        r1 = Res(); r2 = Res()
                        tr.dma("sp", o_k[to * P:(to + 1) * P, :], kr, reads=[r_kr], writes=[r1])
                        tr.dma("sp", o_v[to * P:(to + 1) * P, :], vs, reads=[r_vs], writes=[r2])
                        out_res.extend([r1, r2])
                        if DO_ATT:
                            tr.op("act", lambda e: e.activation(out=qs, in_=pb[0][:], func=AF.Copy, scale=0.125),
                                  reads=[r_pb[0]], writes=[r_qs])
                            rope_apply(qs, r_qs, qr, r_qr, rpt, rrp)
                            tr.op("pool", lambda e: e.tensor_copy(out=krb[:], in_=qr), reads=[r_qr],
                                  writes=[r_krb])
                            transpose4(krb, r_krb, P, QTA[:, :, to * P:(to + 1) * P], r_QTA[to])

                def kmean(blk):
                    tr.op("dve", lambda e: e.tensor_reduce(
                        out=KMf[:, :, blk], in_=KT[:, :, blk * 256:(blk + 1) * 256], axis=AX.X, op=ALU.add),
                        reads=[r_KT[2 * blk], r_KT[2 * blk + 1]], writes=[r_KMf])
                    tr.op("dve", lambda e: e.tensor_scalar(out=KM[:, :, blk], in0=KMf[:, :, blk],
                                                           scalar1=1.0 / 256.0, scalar2=None, op0=ALU.mult),
                          reads=[r_KMf], writes=[r_KM])

                def lru_core(ci, W, nseg, resets, hist_src, init_fn, own, ydst):
                    L = W // nseg
                    xv = XPc[:, 0:nseg * (L + 3)].rearrange("p (s c) -> p s c", s=nseg)

                    def v3(ap2):
                        return ap2[:, 0:W].rearrange("p (s c) -> p s c", s=nseg)
                    tr.op("dve", lambda e: e.tensor_scalar(
                        out=v3(cv), in0=xv[:, :, 3:3 + L], scalar1=LV[:, ci, 3:4], scalar2=LV[:, ci, 4:5],
                        op0=ALU.mult, op1=ALU.add), reads=[r_XPc, r_LV], writes=[r_cv])
                    for jj in range(3):
                        tr.op("dve", lambda e, jj=jj: e.scalar_tensor_tensor(
                            out=v3(cv), in0=xv[:, :, jj:jj + L], scalar=LV[:, ci, jj:jj + 1], in1=v3(cv),
                            op0=ALU.mult, op1=ALU.add), reads=[r_XPc, r_LV, r_cv], writes=[r_cv])
                    tr.op("pe", lambda e: e.matmul(pb[5][:, 0:W], lhsT=WAB[:, ci, :], rhs=cv[:, 0:W],
                                                   start=True, stop=True),
                          reads=[r_WAB, r_cv], writes=[r_pb[5]])
                    tr.op("pe", lambda e: e.matmul(pb[6][:, 0:W], lhsT=WXB[:, ci, :], rhs=cv[:, 0:W],
                                                   start=True, stop=True),
                          reads=[r_WXB, r_cv], writes=[r_pb[6]])
                    tr.op("act", lambda e: e.activation(out=rr[:, 0:W], in_=pb[5][:, 0:W], func=AF.Sigmoid,
                                                        bias=LV[:, ci, 5:6]),
                          reads=[r_pb[5], r_LV], writes=[r_rr])
                    tr.op("act", lambda e: e.activation(out=ii[:, 0:W], in_=pb[6][:, 0:W], func=AF.Sigmoid,
                                                        bias=LV[:, ci, 6:7]),
                          reads=[r_pb[6], r_LV], writes=[r_ii])
                    tr.op("act", lambda e: e.activation(out=aa[:, 0:W], in_=rr[:, 0:W], func=AF.Exp,
                                                        scale=LC1[:, ci:ci + 1]),
                          reads=[r_rr, r_LC1], writes=[r_aa])
                    tr.op("pool", lambda e: e.tensor_tensor(out=a2[:, 0:W], in0=aa[:, 0:W], in1=aa[:, 0:W],
                                                            op=ALU.mult), reads=[r_aa], writes=[r_a2])
                    tr.op("act", lambda e: e.activation(out=a2[:, 0:W], in_=a2[:, 0:W], func=AF.Sqrt,
                                                        scale=-1.0, bias=1.0), reads=[r_a2], writes=[r_a2])
                    tr.op("pool", lambda e: e.tensor_tensor(out=bb[:, 0:W], in0=a2[:, 0:W], in1=ii[:, 0:W],
                                                            op=ALU.mult), reads=[r_a2, r_ii], writes=[r_bb])
                    tr.op("pool", lambda e: e.tensor_tensor(out=bb[:, 0:W], in0=bb[:, 0:W], in1=cv[:, 0:W],
                                                            op=ALU.mult), reads=[r_bb, r_cv], writes=[r_bb])
                    if resets is not None:
                        tr.op("dve", lambda e: e.tensor_tensor(
                            out=tcol[:, ci:ci + 1], in0=ii[:, 0:1], in1=cv[:, 0:1], op=ALU.mult),
                            reads=[r_ii, r_cv], writes=[r_tcol])
                        if resets == "hard":
                            tr.op("dve", lambda e: e.memset(aa[:, 0:1], 0.0), reads=[r_aa], writes=[r_aa])
                            tr.op("dve", lambda e: e.tensor_copy(out=bb[:, 0:1], in_=tcol[:, ci:ci + 1]),
                                  reads=[r_tcol, r_bb], writes=[r_bb])
                        else:
                            tr.op("dve", lambda e: e.tensor_scalar(
                                out=aa[:, 0:1], in0=aa[:, 0:1], scalar1=FL[:, 0:1], scalar2=None, op0=ALU.mult),
                                reads=[r_aa, r_FL], writes=[r_aa])
                            tr.op("dve", lambda e: e.tensor_scalar(
                                out=tcol[:, ci:ci + 1], in0=tcol[:, ci:ci + 1], scalar1=FL[:, 1:2], scalar2=None,
                                op0=ALU.mult), reads=[r_tcol, r_FL], writes=[r_tcol])
                            tr.op("dve", lambda e: e.scalar_tensor_tensor(
                                out=bb[:, 0:1], in0=bb[:, 0:1], scalar=FL[:, 0:1], in1=tcol[:, ci:ci + 1],
                                op0=ALU.mult, op1=ALU.add), reads=[r_bb, r_FL, r_tcol], writes=[r_bb])
                    for s in range(nseg):
                        init_ap, init_res = init_fn(s)
                        tr.op("dve", lambda e, s=s, init_ap=init_ap: e.tensor_tensor_scan(
                            out=hh[:, s * L:(s + 1) * L], data0=aa[:, s * L:(s + 1) * L],
                            data1=bb[:, s * L:(s + 1) * L], initial=init_ap, op0=ALU.mult, op1=ALU.add),
                            reads=[r_aa, r_bb] + init_res, writes=[r_hh])
                    if own:
                        for kc in range(8):
                            tr.op("pe", lambda e, kc=kc: e.matmul(
                                pb[4][:, 0:W], lhsT=WIN[:, kc, 2048 + ci * P:2048 + (ci + 1) * P],
                                rhs=HT[:, kc, 0:W], start=(kc == 0), stop=(kc == 7)),
                                reads=r_HT + [r_WIN], writes=[r_pb[4]])
                        tr.op("act", lambda e: e.activation(out=gsb[:, 0:W], in_=pb[4][:, 0:W], func=AF.Copy),
                              reads=[r_pb[4]], writes=[r_gsb])
                        tr.op("pool", lambda e: e.tensor_tensor(out=uu[:, 0:W], in0=gsb[:, 0:W], in1=gsb[:, 0:W],
                                                                op=ALU.mult), reads=[r_gsb], writes=[r_uu])
                        tr.op("pool", lambda e: e.tensor_scalar(out=uu[:, 0:W], in0=uu[:, 0:W], scalar1=0.044715,
                                                                scalar2=1.0, op0=ALU.mult, op1=ALU.add),
                              reads=[r_uu], writes=[r_uu])
                        tr.op("pool", lambda e: e.tensor_tensor(out=uu[:, 0:W], in0=uu[:, 0:W], in1=gsb[:, 0:W],
                                                                op=ALU.mult), reads=[r_uu, r_gsb], writes=[r_uu])
                        tr.op("act", lambda e: e.activation(out=uu[:, 0:W], in_=uu[:, 0:W], func=AF.Sigmoid,
                                                            scale=1.5957691216057308),
                              reads=[r_uu], writes=[r_uu])
                        tr.op("pool", lambda e: e.tensor_tensor(out=uu[:, 0:W], in0=uu[:, 0:W], in1=gsb[:, 0:W],
                                                                op=ALU.mult), reads=[r_uu, r_gsb], writes=[r_uu])
                        tr.op("dve", lambda e: e.tensor_tensor(out=YY[:, ci, 0:W], in0=hh[:, 0:W], in1=uu[:, 0:W],
                                                               op=ALU.mult),
                              reads=[r_hh, r_uu], writes=[r_YY[ci]])

                def lru_norm(W, ydst, r_ydst):
                    for ci in range(4):
                        tr.op("act", lambda e, ci=ci: e.activation(out=YQ[:, 0:W], in_=YY[:, ci, 0:W],
                                                                   func=AF.Square),
                              reads=[r_YY[ci]], writes=[r_YQ])
                        tr.op("pe", lambda e, ci=ci: e.matmul(pb[5][:, 0:W], lhsT=ones_f[:], rhs=YQ[:, 0:W],
                                                              start=(ci == 0), stop=(ci == 3)),
                              reads=[r_ones, r_YQ], writes=[r_pb[5]])
                    tr.op("act", lambda e: e.activation(out=gsb[:, 0:W], in_=pb[5][:, 0:W], func=AF.Sqrt,
                                                        scale=1.0 / 512.0, bias=EPS),
                          reads=[r_pb[5]], writes=[r_gsb])
                    tr.op("dve", lambda e: e.reciprocal(out=gsb[:, 0:W], in_=gsb[:, 0:W]), reads=[r_gsb],
                          writes=[r_gsb])
                    for ci in range(4):
                        tr.op("dve", lambda e, ci=ci: e.scalar_tensor_tensor(
                            out=ydst(ci), in0=YY[:, ci, 0:W], scalar=LV[:, ci, 8:9], in1=gsb[:, 0:W],
                            op0=ALU.mult, op1=ALU.mult), reads=[r_YY[ci], r_LV, r_gsb], writes=[r_ydst])

                def lru_group(g):
                    own = g >= 4
                    for ci in range(4):
                        tr.op("pool", lambda e, ci=ci: e.tensor_copy(out=XPc[:, 0:3], in_=XH[:, ci, :]),
                              reads=[r_XH[ci]], writes=[r_XPc])
                        if g == 4:
                            tr.op("dve", lambda e: e.tensor_scalar(
                                out=XPc[:, 0:3], in0=XPc[:, 0:3], scalar1=FL[:, 0:1], scalar2=None,
                                op0=ALU.mult), reads=[r_XPc, r_FL], writes=[r_XPc])
                        for kc in range(8):
                            tr.op("pe", lambda e, kc=kc, ci=ci: e.matmul(
                                pb[3][:], lhsT=WIN[:, kc, 1536 + ci * P:1536 + (ci + 1) * P], rhs=HT[:, kc, :],
                                start=(kc == 0), stop=(kc == 7)), reads=r_HT + [r_WIN], writes=[r_pb[3]])
                        tr.op("act", lambda e: e.activation(out=XPc[:, 3:515], in_=pb[3][:], func=AF.Copy),
                              reads=[r_pb[3]], writes=[r_XPc])
                        tr.op("pool", lambda e, ci=ci: e.tensor_copy(out=XH[:, ci, :], in_=XPc[:, 512:515]),
                              reads=[r_XPc], writes=[r_XH[ci]])
                        resets = "hard" if g == 0 else ("soft" if g == 4 else None)
                        lru_core(ci, 512, 1, resets, None,
                                 lambda s, ci=ci: (HS[:, ci:ci + 1], [r_HS[ci]]), own, None)
                        tr.op("dve", lambda e, ci=ci: e.tensor_copy(out=HS[:, ci:ci + 1], in_=hh[:, 511:512]),
                              reads=[r_hh], writes=[r_HS[ci]])
                    if own:
                        c0 = (g - 4) * 512
                        lru_norm(512, lambda ci: YNA[:, ci, c0:c0 + 512], r_YNA[g - 4])

                for g in range(8):
                    stage_front(4 * g)
                    for j in range(4):
                        t = 4 * g + j
                        if j < 3:
                            stage_front(t + 1)
                        stage_a(t)
                        if t % 2 == 1:
                            kmean(t // 2)
                    lru_group(g)
                tr.op("pe", lambda e: e.transpose(out=pb[0][0:4, 0:P], in_=HS[:, 0:4], identity=idf[:]),
                      reads=r_HS + [r_idf], writes=[r_pb[0]])
                for ci in range(4):
                    tr.op("pe", lambda e, ci=ci: e.transpose(out=pb[1][0:3, ci * P:(ci + 1) * P],
                                                             in_=XH[:, ci, :], identity=idf[:]),
                          reads=[r_XH[ci], r_idf], writes=[r_pb[1]])
                tr.op("dve", lambda e: e.tensor_copy(out=qs[0:4, 0:P], in_=pb[0][0:4, 0:P]),
                      reads=[r_pb[0]], writes=[r_qs])
                tr.op("dve", lambda e: e.tensor_copy(out=ks[0:3, :], in_=pb[1][0:3, :]),
                      reads=[r_pb[1]], writes=[r_ks])
                r1 = Res(); r2 = Res()
                tr.dma("sp", o_h.rearrange("(c p) -> c p", p=P), qs[0:4, 0:P], reads=[r_qs], writes=[r1])
                tr.dma("sp", o_c[:, :], ks[0:3, :], reads=[r_ks], writes=[r2])
                out_res.extend([r1, r2])

                load_bc([(bS1, r_bS1, 0, None), (bG1, r_bG1, 1, nmg)], SROWS, (xt[1], r_xt[1]))
                xs_t = xt[0]; rxs = r_xt[0]
                tr.dma("sp", xs_t[0:NS, :], xs[:, :], writes=[rxs])
                rps = rp[0]; rrps = r_rp[0]
                for s in range(4):
                    tr.dma("sp", rps[8 * s:8 * s + 8, :], ropes[:, :], writes=[rrps])
                norm_to_HT(xs_t, rxs, NS, 0)
                proj_tok(NS, 0, [(1, 512), (2, 1024), (0, 0)])
                tr.op("act", lambda e: e.activation(out=ks[0:NS, :], in_=pb[1][0:NS, :], func=AF.Copy),
                      reads=[r_pb[1]], writes=[r_ks])
                tr.op("act", lambda e: e.activation(out=vs[0:NS, :], in_=pb[2][0:NS, :], func=AF.Copy),
                      reads=[r_pb[2]], writes=[r_vs])
                tr.op("act", lambda e: e.activation(out=qs[0:NS, :], in_=pb[0][0:NS, :], func=AF.Copy, scale=0.125),
                      reads=[r_pb[0]], writes=[r_qs])
                tr.op("pool", lambda e: e.tensor_copy(out=VNb[0:NS, :], in_=vs[0:NS, :]),
                      reads=[r_vs, r_VNb], writes=[r_VNb])
                rope_apply(ks, r_ks, kr, r_kr, rps, rrps, n=NS)
                r1 = Res(); r2 = Res()
                tr.dma("sp", o_ks[:, :], kr[0:NS, :], reads=[r_kr], writes=[r1])
                tr.dma("sp", o_vs[:, :], vs[0:NS, :], reads=[r_vs], writes=[r2])
                out_res.extend([r1, r2])
                tr.op("pool", lambda e: e.tensor_copy(out=krb[0:NS, :], in_=kr[0:NS, :]), reads=[r_kr],
                      writes=[r_krb])
                transpose4(krb, r_krb, NS, KTn[:, :, :], r_KTn)
                rope_apply(qs, r_qs, qr, r_qr, rps, rrps, n=NS)
                tr.op("pool", lambda e: e.tensor_copy(out=krb[0:NS, :], in_=qr[0:NS, :]), reads=[r_qr],
                      writes=[r_krb])
                transpose4(krb, r_krb, NS, QTs[:, :, :], r_QTs)
                STH = sb(es, "STH", [P, 4, 4]); r_STH = Res()
                STC = sb(es, "STC", [P, 4, 4, 3]); r_STC = Res()
                HSs = sb(es, "HSs", [P, 4, 4]); r_HSs = Res()
                CSs = sb(es, "CSs", [P, 4, 12]); r_CSs = Res()
                tr.dma("sp", STH[:], sth[:, :, :], writes=[r_STH])
                tr.dma("sp", STC[:], stc[:, :, :, :], writes=[r_STC])
                for ci in range(4):
                    xv = XPc[:, 0:44].rearrange("p (s c) -> p s c", s=4)
                    tr.op("pool", lambda e, ci=ci: e.tensor_copy(out=xv[:, :, 0:3], in_=STC[:, ci, :, :]),
                          reads=[r_STC], writes=[r_XPc])
                    for kc in range(8):
                        tr.op("pe", lambda e, kc=kc, ci=ci: e.matmul(
                            pb[3][:, 0:NS], lhsT=WIN[:, kc, 1536 + ci * P:1536 + (ci + 1) * P], rhs=HT[:, kc, 0:NS],
                            start=(kc == 0), stop=(kc == 7)), reads=r_HT + [r_WIN], writes=[r_pb[3]])
                    tr.op("act", lambda e: e.activation(
                        out=xv[:, :, 3:11], in_=pb[3][:, 0:NS].rearrange("p (s c) -> p s c", s=4), func=AF.Copy),
                        reads=[r_pb[3]], writes=[r_XPc])
                    tr.op("pool", lambda e, ci=ci: e.tensor_copy(
                        out=CSs[:, ci, :].rearrange("p (s c) -> p s c", s=4), in_=xv[:, :, 8:11]),
                        reads=[r_XPc], writes=[r_CSs])
                    lru_core(ci, NS, 4, None, None,
                             lambda s, ci=ci: (STH[:, ci, s:s + 1], [r_STH]), True, None)
                    tr.op("dve", lambda e, ci=ci: e.tensor_copy(
                        out=HSs[:, ci, :].unsqueeze(2), in_=hh[:, 0:NS].rearrange("p (s c) -> p s c", s=4)[:, :, 7:8]),
                        reads=[r_hh], writes=[r_HSs])
                lru_norm(NS, lambda ci: YNs[:, ci, :], r_YNs)
                for ci in range(4):
                    tr.op("pe", lambda e, ci=ci: e.transpose(out=pb[0][0:4, ci * P:(ci + 1) * P],
                                                             in_=HSs[:, ci, :], identity=idf[:]),
                          reads=[r_HSs, r_idf], writes=[r_pb[0]])
                    tr.op("pe", lambda e, ci=ci: e.transpose(out=pb[1][0:12, ci * P:(ci + 1) * P],
                                                             in_=CSs[:, ci, :], identity=idf[:]),
                          reads=[r_CSs, r_idf], writes=[r_pb[1]])
                tr.op("dve", lambda e: e.tensor_copy(out=qs[0:4, :], in_=pb[0][0:4, :]),
                      reads=[r_pb[0]], writes=[r_qs])
                tr.op("dve", lambda e: e.tensor_copy(out=ks[0:12, :], in_=pb[1][0:12, :]),
                      reads=[r_pb[1]], writes=[r_ks])
                r1 = Res(); r2 = Res()
                tr.dma("sp", o_hs[:, :], qs[0:4, :], reads=[r_qs], writes=[r1])
                tr.dma("sp", o_cs[:, :], ks[0:12, :], reads=[r_ks], writes=[r2])
                out_res.extend([r1, r2])
            tr.barrier()
            def p2_common(es, sfx, rows):
                bGM = sb(es, "bGM" + sfx, [P, D]); r_bGM = Res()
                bS2 = sb(es, "bS2" + sfx, [P, D]); r_bS2 = Res()
                bG2 = sb(es, "bG2" + sfx, [P, D]); r_bG2 = Res()
                xr = sb(es, "xr" + sfx, [P, D]); r_xr = Res()
                xm = sb(es, "xm" + sfx, [P, D]); r_xm = Res()
                tmpf = sb(es, "tmpf2" + sfx, [P, D]); r_tmpf = Res()
                hb2 = sb(es, "hb2" + sfx, [P, D], BF16); r_hb2 = Res()
                h2s = sb(es, "h2s" + sfx, [P, 8, P], BF16); r_h2s = Res()
                WOUT = sb(es, "WOUT" + sfx, [P, 8, D], BF16); r_WOUT = Res()
                woutv = w_out.rearrange("(kc p) n -> p kc n", p=P)
                for cc in range(2):
                    tr.dma("pool", WOUT[:, :, cc * 512:(cc + 1) * 512], woutv[:, :, cc * 512:(cc + 1) * 512],
                           writes=[r_WOUT])
                RWB = sb(es, "RWB" + sfx, [P, 8, NE], BF16); r_RWB = Res()
                tr.dma("pool", RWB[:], router_w.rearrange("(kc p) n -> p kc n", p=P), writes=[r_RWB])
                RB = sb(es, "RB" + sfx, [P, NE]); r_RB = Res()
                tr.dma("sp", RB[:], router_b.partition_broadcast(P), writes=[r_RB])
                AOG = sb(es, "AOG" + sfx, [P, 512]); r_AOG = Res()
                tr.dma("sp", AOG[:], aog.partition_broadcast(P), writes=[r_AOG])
                AT = sb(es, "AT" + sfx, [P, 512]); r_AT = Res()
                atb = sb(es, "atb" + sfx, [P, 512], BF16); r_atb = Res()
                sc = sb(es, "sc" + sfx, [P, NE]); r_sc2 = Res()
                bi = sb(es, "bi" + sfx, [P, NE]); r_bi = Res()
                m8g = sb(es, "m8g" + sfx, [P, 8, 8]); r_m8g = Res()
                gsr = sb(es, "gsr" + sfx, [P, 8]); r_gsr = Res()
                gm = sb(es, "gm" + sfx, [P, 8]); r_gm = Res()
                msk = sb(es, "msk" + sfx, [P, NE]); r_msk = Res()
                em = sb(es, "em" + sfx, [P, NE]); r_em = Res()
                wv = sb(es, "wv" + sfx, [P, NE]); r_wv = Res()
                st2 = sb(es, "st2" + sfx, [P, 8]); r_st2 = Res()
                load_bc([(bGM, r_bGM, 2, None), (bS2, r_bS2, 3, None), (bG2, r_bG2, 4, nfg)], rows, (xr, r_xr))

                def attn_finish(o3, rc2, n, rds, dst, r_dst):
                    R = slice(0, n)
                    tr.op("dve", lambda e: e.tensor_tensor(
                        out=AT[R, :].rearrange("p (h d) -> p h d", h=NH), in0=o3,
                        in1=rc2.unsqueeze(2).broadcast_to([n, NH, 64]), op=ALU.mult),
                        reads=rds, writes=[r_AT])
                    tr.op("act", lambda e: e.activation(out=hb2[R, 0:512], in_=AT[R, :], func=AF.Square,
                                                        accum_out=st2[R, 0:1]),
                          reads=[r_AT], writes=[r_hb2, r_st2])
                    tr.op("act", lambda e: e.activation(out=st2[R, 1:2], in_=st2[R, 0:1], func=AF.Sqrt,
                                                        scale=1.0 / 512.0, bias=EPS),
                          reads=[r_st2], writes=[r_st2])
                    tr.op("dve", lambda e: e.reciprocal(out=st2[R, 2:3], in_=st2[R, 1:2]),
                          reads=[r_st2], writes=[r_st2])
                    tr.op("dve", lambda e: e.scalar_tensor_tensor(
                        out=atb[R, :], in0=AT[R, :], scalar=st2[R, 2:3], in1=AOG[R, :], op0=ALU.mult,
                        op1=ALU.mult), reads=[r_AT, r_st2, r_AOG], writes=[r_atb])
                    for pr in range(4):
                        tr.op("pe", lambda e, pr=pr: e.transpose(out=pbT[:, pr * P:pr * P + n],
                                                                 in_=atb[R, pr * P:(pr + 1) * P],
                                                                 identity=idb[R, R]),
                              reads=[r_atb, r_idb], writes=[r_pbT])
                    tr.op("dve", lambda e: e.tensor_copy(
                        out=dst, in_=pbT[:, 0:512].rearrange("p (k c) -> p k c", k=4)[:, :, 0:n]),
                        reads=[r_pbT], writes=[r_dst])

                def ffn_pre(src, r_src, n, to):
                    R = slice(0, n)
                    c0 = to * P
                    tr.op("act", lambda e: e.activation(out=hb2[R, :], in_=src[R, :], func=AF.Square,
                                                        accum_out=st2[R, 3:4]),
                          reads=[r_src], writes=[r_hb2, r_st2])
                    tr.op("act", lambda e: e.activation(out=st2[R, 4:5], in_=st2[R, 3:4], func=AF.Sqrt,
                                                        scale=1.0 / D, bias=EPS), reads=[r_st2], writes=[r_st2])
                    tr.op("dve", lambda e: e.reciprocal(out=st2[R, 5:6], in_=st2[R, 4:5]),
                          reads=[r_st2], writes=[r_st2])
                    tr.op("dve", lambda e: e.scalar_tensor_tensor(
                        out=tmpf[R, :], in0=src[R, :], scalar=st2[R, 5:6], in1=bG2[R, :],
                        op0=ALU.mult, op1=ALU.mult), reads=[r_src, r_st2, r_bG2], writes=[r_tmpf])
                    tr.op("pool", lambda e: e.tensor_tensor(out=hb2[R, :], in0=tmpf[R, :], in1=bS2[R, :],
                                                            op=ALU.add),
                          reads=[r_tmpf, r_bS2], writes=[r_hb2])
                    for kc in range(8):
                        tr.op("pe", lambda e, kc=kc: e.transpose(out=pbT[:, kc * P:kc * P + n],
                                                                 in_=hb2[R, kc * P:(kc + 1) * P],
                                                                 identity=idb[R, R]),
                              reads=[r_hb2, r_idb], writes=[r_pbT])
                    tr.op("act", lambda e: e.activation(
                        out=h2s[:, :, 0:n], in_=pbT[:].rearrange("p (k c) -> p k c", k=8)[:, :, 0:n],
                        func=AF.Copy), reads=[r_pbT], writes=[r_h2s])
                    tr.dma("sp", h2d[:, :, c0:c0 + n], h2s[:, :, 0:n], reads=[r_h2s], writes=[r_h2d[to]])
                    for kc in range(8):
                        tr.op("pe", lambda e, kc=kc: e.matmul(
                            pb[0][R, 0:NE], lhsT=h2s[:, kc, 0:n], rhs=RWB[:, kc, :],
                            start=(kc == 0), stop=(kc == 7)), reads=[r_h2s, r_RWB], writes=[r_pb[0]])
                    tr.op("act", lambda e: e.activation(out=sc[R, :], in_=pb[0][R, 0:NE], func=AF.Sigmoid),
                          reads=[r_pb[0]], writes=[r_sc2])
                    tr.op("dve", lambda e: e.tensor_tensor(out=bi[R, :], in0=sc[R, :], in1=RB[R, :], op=ALU.add),
                          reads=[r_sc2, r_RB], writes=[r_bi])
                    for g in range(8):
                        tr.op("dve", lambda e, g=g: e.max(out=m8g[R, g, :], in_=bi[R, g * 8:(g + 1) * 8]),
                              reads=[r_bi], writes=[r_m8g])
                    tr.op("dve", lambda e: e.tensor_tensor(out=gsr[R, :].unsqueeze(2), in0=m8g[R, :, 0:1],
                                                           in1=m8g[R, :, 1:2], op=ALU.add),
                          reads=[r_m8g], writes=[r_gsr])
                    tr.op("dve", lambda e: e.max(out=m8g[R, 0, :], in_=gsr[R, :]), reads=[r_gsr, r_m8g],
                          writes=[r_m8g])
                    tr.op("dve", lambda e: e.tensor_scalar(out=gm[R, :], in0=gsr[R, :], scalar1=m8g[R, 0, 3:4],
                                                           scalar2=None, op0=ALU.is_ge),
                          reads=[r_gsr, r_m8g], writes=[r_gm])
                    tr.op("dve", lambda e: e.scalar_tensor_tensor(
                        out=msk[R, :].rearrange("p (g k) -> p g k", g=8),
                        in0=bi[R, :].rearrange("p (g k) -> p g k", g=8), scalar=2.0,
                        in1=gm[R, :].unsqueeze(2).broadcast_to([n, 8, 8]), op0=ALU.add, op1=ALU.mult),
                        reads=[r_bi, r_gm], writes=[r_msk])
                    tr.op("dve", lambda e: e.max(out=m8g[R, 1, :], in_=msk[R, :]), reads=[r_msk, r_m8g],
                          writes=[r_m8g])
                    tr.op("dve", lambda e: e.tensor_scalar(out=em[R, :], in0=msk[R, :], scalar1=m8g[R, 1, 7:8],
                                                           scalar2=None, op0=ALU.is_ge),
                          reads=[r_msk, r_m8g], writes=[r_em])
                    tr.op("dve", lambda e: e.tensor_tensor(out=wv[R, :], in0=sc[R, :], in1=em[R, :], op=ALU.mult),
                          reads=[r_sc2, r_em], writes=[r_wv])
                    tr.op("dve", lambda e: e.tensor_reduce(out=st2[R, 6:7], in_=wv[R, :], axis=AX.X, op=ALU.add),
                          reads=[r_wv], writes=[r_st2])
                    tr.op("dve", lambda e: e.reciprocal(out=st2[R, 7:8], in_=st2[R, 6:7]), reads=[r_st2],
                          writes=[r_st2])
                    tr.op("dve", lambda e: e.tensor_scalar(out=GT[R, to, :], in0=wv[R, :], scalar1=st2[R, 7:8],
                                                           scalar2=2.5, op0=ALU.mult, op1=ALU.mult),
                          reads=[r_wv, r_st2], writes=[r_GT[to]])

                def outproj(n, xsrc_dram, att_ap, r_att, yn_ap, r_yn, to):
                    R = slice(0, n)
                    tr.dma("sp", xr[R, :], xsrc_dram, writes=[r_xr])
                    for hf in range(2):
                        bk = 1 + hf
                        for kc in range(8):
                            lhs = att_ap(kc) if kc < 4 else yn_ap(kc - 4)
                            rl = r_att if kc < 4 else r_yn
                            tr.op("pe", lambda e, kc=kc, lhs=lhs, bk=bk, hf=hf: e.matmul(
                                pb[bk][R, :], lhsT=lhs, rhs=WOUT[:, kc, hf * 512:(hf + 1) * 512],
                                start=(kc == 0), stop=(kc == 7)), reads=[rl, r_WOUT], writes=[r_pb[bk]])
                        tr.op("dve", lambda e, bk=bk, hf=hf: e.tensor_tensor(
                            out=tmpf[R, hf * 512:(hf + 1) * 512], in0=pb[bk][R, :],
                            in1=bGM[R, hf * 512:(hf + 1) * 512], op=ALU.mult),
                            reads=[r_pb[bk], r_bGM], writes=[r_tmpf])
                    tr.op("pool", lambda e: e.tensor_tensor(out=xm[R, :], in0=tmpf[R, :], in1=xr[R, :], op=ALU.add),
                          reads=[r_tmpf, r_xr], writes=[r_xm])
                    tr.dma("sp", xmid[to * P:to * P + n, :], xm[R, :], reads=[r_xm], writes=[r_xmid[to]])
                    ffn_pre(xm, r_xm, n, to)

                import types
                return types.SimpleNamespace(attn_finish=attn_finish, ffn_pre=ffn_pre, outproj=outproj,
                                             AOG=AOG, r_AOG=r_AOG, AT=AT, r_AT=r_AT, st2=st2, r_st2=r_st2,
                                             hb2=hb2, r_hb2=r_hb2, atb=atb, r_atb=r_atb)

            with contextlib.ExitStack() as es:
                C = p2_common(es, "p", [(0, 0, P)])
                BL = sb(es, "BL", [P, 8, 3, 16]); r_BL = Res()
                tr.dma("sp", BL[:].rearrange("p a b c -> p (a b c)"), blkc.partition_broadcast(P), writes=[r_BL])
                TRI = sb(es, "TRI", [P, P], BF16); r_TRI = Res()
                tr.dma("pool", TRI[:], trim[:, :], writes=[r_TRI])
                OH = sb(es, "OH", [P, 16 * P], BF16); r_OH = Res()
                tr.op("pool", lambda e: e.memset(OH[:], 0.0), writes=[r_OH])
                tr.dma("pool", OH[0:16, :], ohm[:, :], reads=[r_OH], writes=[r_OH])
                QTz = sb(es, "QTz", [P, NH, 256], BF16); r_QTz = Res()
                tr.op("pool", lambda e: e.memset(QTz[:], 0.0), writes=[r_QTz])
                gbs = sb(es, "gbs", [P, NH, 16]); r_gbs = Res()
                m8 = sb(es, "m8", [P, NH, 8]); r_m8 = Res()
                mbf = sb(es, "mbf", [P, NH, 16]); r_mbf = Res()
                mbb = sb(es, "mbb", [P, NH, 16], BF16); r_mbb = Res()
                MBT = sb(es, "MBT", [P, NH, 256], BF16); r_MBT = Res()
                tr.op("pool", lambda e: e.memset(MBT[:], 0.0), writes=[r_MBT])
                PT = [sb(es, f"PT{i}", [P, 256], BF16) for i in range(4)]; r_PT = [Res() for _ in range(4)]
                OA = sb(es, "OA", [P, 2, NH, 65]); r_OA = Res()
                rc = sb(es, "rc", [P, 2, NH]); r_rc = Res()
                ATT = sb(es, "ATT", [P, 4, 256], BF16); r_ATT = [Res(), Res()]
                pcnt = [0]

                def attention(jb):
                    t0 = 16 + 2 * jb
                    qoff = jb * 256
                    rq = [r_QTz]
                    for h in range(NH):
                        pr, p0 = h // 2, 64 * (h % 2)
                        tr.op("pool", lambda e, h=h, pr=pr, p0=p0: e.tensor_copy(
                            out=QTz[p0:p0 + 64, h, :], in_=QTA[p0:p0 + 64, pr, qoff:qoff + 256]),
                            reads=[r_QTA[2 * jb], r_QTA[2 * jb + 1], r_QTz], writes=[r_QTz])
                    for i in range(2):
                        bk = 3 + i
                        for h in range(NH):
                            pr, p0 = h // 2, 64 * (h % 2)
                            tr.op("pe", lambda e, h=h, pr=pr, bk=bk, i=i: e.matmul(
                                pb[bk][:, h * 16:(h + 1) * 16],
                                lhsT=QTz[:, h, i * P:(i + 1) * P],
                                rhs=KM[:, pr, :], start=True, stop=True),
                                reads=[r_QTz, r_KM], writes=[r_pb[bk]])
                        tr.op("dve", lambda e, bk=bk: e.tensor_tensor(
                            out=gbs[:], in0=pb[bk][:, 0:128].rearrange("p (h n) -> p h n", h=NH),
                            in1=BL[:, jb, 0, :].unsqueeze(1).broadcast_to([P, NH, 16]), op=ALU.add),
                            reads=[r_pb[bk], r_BL], writes=[r_gbs])
                        for h in range(NH):
                            tr.op("dve", lambda e, h=h: e.max(out=m8[:, h, :], in_=gbs[:, h, :]),
                                  reads=[r_gbs], writes=[r_m8])
                        for h in range(NH):
                            tr.op("dve", lambda e, h=h: e.tensor_scalar(
                                out=mbf[:, h, :], in0=gbs[:, h, :], scalar1=m8[:, h, 2:3], scalar2=NEG,
                                op0=ALU.is_lt, op1=ALU.mult), reads=[r_gbs, r_m8], writes=[r_mbf])
                        tr.op("dve", lambda e: e.tensor_tensor(
                            out=mbf[:], in0=mbf[:], in1=BL[:, jb, 1, :].unsqueeze(1).broadcast_to([P, NH, 16]),
                            op=ALU.mult), reads=[r_mbf, r_BL], writes=[r_mbf])
                        tr.op("dve", lambda e: e.tensor_tensor(
                            out=mbb[:], in0=mbf[:], in1=BL[:, jb, 2, :].unsqueeze(1).broadcast_to([P, NH, 16]),
                            op=ALU.add), reads=[r_mbf, r_BL], writes=[r_mbb])
                        for h in range(NH):
                            tr.op("pe", lambda e, h=h: e.transpose(out=pbT[0:16, h * P:(h + 1) * P],
                                                                   in_=mbb[:, h, :], identity=idb[:]),
                                  reads=[r_mbb, r_idb], writes=[r_pbT])
                        tr.op("dve", lambda e, i=i: e.tensor_copy(
                            out=MBT[0:16, :, i * P:(i + 1) * P],
                            in_=pbT[0:16, :].rearrange("p (h c) -> p h c", h=NH)),
                            reads=[r_pbT, r_MBT], writes=[r_MBT])
                    nkt = t0 + 2
                    if "L1" in stages:
                        return
                    pend = [None]
                    for h in range(NH):
                        pr, p0 = h // 2, 64 * (h % 2)
                        reg = (h % 2) * 65
                        for kt in range(nkt):
                            c0 = 0 if kt <= t0 else P
                            bk = 3 + (pcnt[0] % 2)
                            pt = PT[pcnt[0] % 4]; rpt_ = r_PT[pcnt[0] % 4]
                            pcnt[0] += 1
                            diag = kt >= t0
                            nb = kt // 2
                            tr.op("pe", lambda e, kt=kt, c0=c0, bk=bk, pr=pr, h=h: e.matmul(
                                pb[bk][:, c0:256], lhsT=KT[:, pr, kt * P:(kt + 1) * P],
                                rhs=QTz[:, h, c0:256], start=True, stop=False),
                                reads=[r_KT[kt]] + rq, writes=[r_pb[bk]])
                            tr.op("pe", lambda e, nb=nb, c0=c0, bk=bk, h=h, diag=diag: e.matmul(
                                pb[bk][:, c0:256], lhsT=OH[:, nb * P:(nb + 1) * P],
                                rhs=MBT[:, h, c0:256], start=False, stop=(not diag)),
                                reads=[r_OH, r_MBT], writes=[r_pb[bk]])
                            if diag:
                                dc = (kt - t0) * P
                                tr.op("pe", lambda e, bk=bk, dc=dc: e.matmul(
                                    pb[bk][:, dc:dc + P], lhsT=idb[:], rhs=TRI[:], start=False, stop=True),
                                    reads=[r_idb, r_TRI], writes=[r_pb[bk]])
                            tr.op("act", lambda e, bk=bk, c0=c0, pt=pt: e.activation(
                                out=pt[:, c0:256], in_=pb[bk][:, c0:256], func=AF.Exp),
                                reads=[r_pb[bk]], writes=[rpt_])

                            def emit_pv(kt=kt, pt=pt, rpt_=rpt_, h=h, reg=reg, last=(kt == nkt - 1)):
                                for i in range(2):
                                    if kt > t0 + i:
                                        continue
                                    tr.op("pe", lambda e, i=i: e.matmul(
                                        pb[5 + i][:, reg:reg + 65], lhsT=pt[:, i * P:(i + 1) * P],
                                        rhs=VA[:, kt, h, :], start=(kt == 0), stop=(kt == t0 + i)),
                                        reads=[rpt_, r_VA[kt]], writes=[r_pb[5 + i]])
                                if last:
                                    for i in range(2):
                                        tr.op("act", lambda e, i=i: e.activation(
                                            out=OA[:, i, h, :], in_=pb[5 + i][:, reg:reg + 65], func=AF.Copy),
                                            reads=[r_pb[5 + i]], writes=[r_OA])
                            if pend[0] is not None:
                                pend[0]()
                            pend[0] = emit_pv
                    pend[0]()
                    pend[0] = None
                    if "L2" in stages:
                        return
                    tr.op("dve", lambda e: e.reciprocal(out=rc[:], in_=OA[:, :, :, 64]), reads=[r_OA],
                          writes=[r_rc])
                    for i in range(2):
                        C.attn_finish(OA[:, i, :, 0:64], rc[:, i, :], P, [r_OA, r_rc],
                                    ATT[:, :, i * P:(i + 1) * P], r_ATT[i])

                if DO_ATT:
                    for jb in range(8):
                        attention(jb)
                        if "L1" in stages or "L2" in stages or "L3" in stages:
                            continue
                        for i in range(2):
                            t = 16 + 2 * jb + i
                            to = t - NCT
                            C.outproj(P, xp[t * P:(t + 1) * P, :],
                                    lambda kc, i=i: ATT[:, kc, i * P:(i + 1) * P], r_ATT[i],
                                    lambda kc, to=to: YNA[:, kc, to * P:(to + 1) * P], r_YNA[to // 4], to)
        tr.barrier()
        if DO_SMP:
            with contextlib.ExitStack() as es:
                C = p2_common(es, "s", SROWS)
                KTs = sb(es, "KTs", [P, 4, 64 * P], BF16); r_KTs = Res()
                PTs = sb(es, "PTs", [P, 64, 64], BF16); r_PTs = [Res() for _ in range(8)]
                KP = [sb(es, f"KP{i}", [P, 512]) for i in range(4)]; r_KP = [Res() for _ in range(4)]
                VP = [sb(es, f"VP{i}", [P, 512]) for i in range(4)]; r_VP = [Res() for _ in range(4)]
                VPb = [sb(es, f"VPb{i}", [P, 512], BF16) for i in range(4)]; r_VPb = [Res() for _ in range(4)]
                KS = sb(es, "KS", [P, 4, 64]); r_KS = Res()
                KSt = sb(es, "KSt", [P, 4, 32]); r_KSt = Res()
                KMs = sb(es, "KMs", [P, 4, 32], BF16); r_KMs = Res()
                QZ = sb(es, "QZ", [P, 4, 64], BF16); r_QZ = Res()
                OHS = sb(es, "OHS", [P, 32 * P], BF16); r_OHS = Res()
                tr.op("pool", lambda e: e.memset(OHS[:], 0.0), writes=[r_OHS])
                for hh_ in range(2):
                    tr.dma("pool", OHS[0:32, hh_ * 2048:(hh_ + 1) * 2048], ohs[:, hh_ * 2048:(hh_ + 1) * 2048],
                           reads=[r_OHS], writes=[r_OHS])
                TRSz = sb(es, "TRSz", [P, 4, 64], BF16); r_TRSz = Res()
                tr.op("pool", lambda e: e.memset(TRSz[:], 0.0), writes=[r_TRSz])
                tr.dma("pool", TRSz[0:32, :, :].rearrange("p a b -> p (a b)"), trs[:, :], reads=[r_TRSz],
                       writes=[r_TRSz])
                SEL = sb(es, "SEL", [P, 64]); r_SEL = Res()
                tr.dma("sp", SEL[:], selq[:, :], writes=[r_SEL])
                DM = sb(es, "DM", [64, 512]); r_DM = Res()
                tr.dma("sp", DM[:], dmask[:, :], writes=[r_DM])
                PAR = sb(es, "PAR", [64, 2]); r_PAR = Res()
                tr.dma("sp", PAR[:], par[:, :], writes=[r_PAR])
                AOG2 = sb(es, "AOG2", [64, 64]); r_AOG2 = Res()
                for h in range(NH):
                    tr.dma("sp", AOG2[h * 8:(h + 1) * 8, :], aog[h * 64:(h + 1) * 64].partition_broadcast(8),
                           writes=[r_AOG2])
                IOT = sb(es, "IOT", [P, 1]); r_IOT = Res()
                tr.dma("sp", IOT[:], iot[:, :], writes=[r_IOT])
                PTI = sb(es, "PTI", [P, 64], I32); r_PTI = Res()
                PTF = sb(es, "PTF", [P, 64]); r_PTF = Res()
                IDX = sb(es, "IDX", [P, 64], I32); r_IDX = Res()
                gbS = sb(es, "gbS", [64, 32]); r_gbS = Res()
                m8S = sb(es, "m8S", [64, 8]); r_m8S = Res()
                mbS = sb(es, "mbS", [P, 32], BF16); r_mbS = Res()
                tr.op("pool", lambda e: e.memset(mbS[:], 0.0), writes=[r_mbS])
                MBTs = sb(es, "MBTs", [P, 64], BF16); r_MBTs = Res()
                tr.op("pool", lambda e: e.memset(MBTs[:], 0.0), writes=[r_MBTs])
                PTn = sb(es, "PTn", [P, 64], BF16); r_PTn = Res()
                tr.op("pool", lambda e: e.memset(PTn[:], 0.0), writes=[r_PTn])
                ones_b = sb(es, "ones_b", [P, 2], BF16); r_onesb = Res()
                tr.op("pool", lambda e: e.memset(ones_b[:], 1.0), writes=[r_onesb])
                O2 = sb(es, "O2", [64, 512]); r_O2 = Res()
                AT2 = sb(es, "AT2", [64, 64]); r_AT2 = Res()
                FU = sb(es, "FU", [64, 64]); r_FU = Res()
                rs = sb(es, "rs", [P, 8]); r_rs = Res()
                tr.op("pool", lambda e: e.memset(rs[:], 0.0), writes=[r_rs])
                TP = sb(es, "TP", [P, P], BF16); r_TP = Res()
                tr.op("pool", lambda e: e.memset(TP[:], 0.0), writes=[r_TP])
                ATTs = sb(es, "ATTs", [P, 4, NS], BF16); r_ATTs = Res()

                for s in range(4):
                    if "S00" in stages:
                        continue
                    tr.dma("sp", PTI[:], ptab[s, :].partition_broadcast(P), writes=[r_PTI])
                    tr.op("dve", lambda e: e.tensor_copy(out=PTF[:], in_=PTI[:]), reads=[r_PTI], writes=[r_PTF])
                    tr.op("dve", lambda e: e.tensor_scalar(out=PTF[:], in0=PTF[:], scalar1=128.0,
                                                           scalar2=IOT[:, 0:1], op0=ALU.mult, op1=ALU.add),
                          reads=[r_PTF, r_IOT], writes=[r_PTF])
                    tr.op("dve", lambda e: e.tensor_copy(out=IDX[:], in_=PTF[:]), reads=[r_PTF], writes=[r_IDX])
                    tr.op("pool", lambda e: e.memset(QZ[:], 0.0), writes=[r_QZ])
                    for h in range(NH):
                        pr, p0 = h // 2, 64 * (h % 2)
                        tr.op("pool", lambda e, h=h, pr=pr, p0=p0, s=s: e.tensor_copy(
                            out=QZ[p0:p0 + 64, pr, h * 8:(h + 1) * 8], in_=QTs[p0:p0 + 64, pr, s * 8:(s + 1) * 8]),
                            reads=[r_QTs, r_QZ], writes=[r_QZ])
                    if "S0" in stages:
                        continue
                    for j in range(64):
                        kp = KP[j % 4]; rkp = r_KP[j % 4]
                        tr.gather(kp[:, :], cache_k[:, :], IDX[:, j:j + 1], reads=[r_IDX], writes=[rkp])
                        bk = j % 2
                        for pr in range(4):
                            tr.op("pe", lambda e, pr=pr, kp=kp, bk=bk: e.transpose(
                                out=pb[bk][:, pr * P:(pr + 1) * P], in_=kp[:, pr * P:(pr + 1) * P], identity=idf[:]),
                                reads=[rkp, r_idf], writes=[r_pb[bk]])
                        tr.op("act", lambda e, j=j, bk=bk: e.activation(
                            out=KTs[:, :, j * P:(j + 1) * P], in_=pb[bk][:].rearrange("p (k c) -> p k c", k=4),
                            func=AF.Copy), reads=[r_pb[bk]], writes=[r_KTs])
                        tr.op("dve", lambda e, j=j, bk=bk: e.tensor_reduce(
                            out=KS[:, :, j], in_=pb[bk][:].rearrange("p (k c) -> p k c", k=4), axis=AX.X,
                            op=ALU.add), reads=[r_pb[bk]], writes=[r_KS])
                    ks4 = KS[:].rearrange("p k (n t) -> p k n t", t=2)
                    tr.op("dve", lambda e: e.tensor_tensor(out=KSt[:].unsqueeze(3), in0=ks4[:, :, :, 0:1],
                                                           in1=ks4[:, :, :, 1:2], op=ALU.add),
                          reads=[r_KS], writes=[r_KSt])
                    tr.op("dve", lambda e: e.tensor_scalar(out=KMs[:], in0=KSt[:], scalar1=1.0 / 256.0,
                                                           scalar2=None, op0=ALU.mult),
                          reads=[r_KSt], writes=[r_KMs])
                    if "S1" in stages:
                        continue
                    for pr in range(4):
                        tr.op("pe", lambda e, pr=pr: e.matmul(pb[2][0:64, 0:32], lhsT=QZ[:, pr, :], rhs=KMs[:, pr, :],
                                                              start=(pr == 0), stop=(pr == 3)),
                              reads=[r_QZ, r_KMs], writes=[r_pb[2]])
                    tr.op("dve", lambda e: e.tensor_copy(out=gbS[:], in_=pb[2][0:64, 0:32]), reads=[r_pb[2]],
                          writes=[r_gbS])
                    tr.op("dve", lambda e: e.max(out=m8S[:], in_=gbS[:]), reads=[r_gbS], writes=[r_m8S])
                    tr.op("dve", lambda e: e.tensor_scalar(out=mbS[0:64, :], in0=gbS[:], scalar1=m8S[:, 2:3],
                                                           scalar2=NEG, op0=ALU.is_lt, op1=ALU.mult),
                          reads=[r_gbS, r_m8S, r_mbS], writes=[r_mbS])
                    tr.op("pe", lambda e: e.transpose(out=pbT[0:32, 0:P], in_=mbS[:, :], identity=idb[:]),
                          reads=[r_mbS, r_idb], writes=[r_pbT])
                    tr.op("dve", lambda e: e.tensor_copy(out=MBTs[0:32, :], in_=pbT[0:32, 0:64]),
                          reads=[r_pbT, r_MBTs], writes=[r_MBTs])
                    if "S2" in stages:
                        continue
                    for cidx in range(8):
                        bk = 3 + (cidx % 2)
                        for jj in range(8):
                            j = cidx * 8 + jj
                            reg = pb[bk][:, jj * 64:(jj + 1) * 64]
                            nb = j // 2
                            tr.op("pe", lambda e, reg=reg, nb=nb: e.matmul(
                                reg, lhsT=OHS[:, nb * P:(nb + 1) * P], rhs=MBTs[:, :], start=True, stop=False),
                                reads=[r_OHS, r_MBTs], writes=[r_pb[bk]])
                            for pr in range(4):
                                tr.op("pe", lambda e, reg=reg, pr=pr, j=j: e.matmul(
                                    reg, lhsT=KTs[:, pr, j * P:(j + 1) * P], rhs=QZ[:, pr, :],
                                    start=False, stop=(pr == 3)), reads=[r_KTs, r_QZ], writes=[r_pb[bk]])
                        tr.op("act", lambda e, cidx=cidx, bk=bk: e.activation(
                            out=PTs[:, cidx * 8:(cidx + 1) * 8, :],
                            in_=pb[bk][:].rearrange("p (a b) -> p a b", a=8), func=AF.Exp),
                            reads=[r_pb[bk]], writes=[r_PTs[cidx]])
                    tr.op("pe", lambda e, s=s: e.matmul(pb[2][0:32, 128:192], lhsT=idb[:, 0:32], rhs=TRSz[:, s, :],
                                                        start=True, stop=False),
                          reads=[r_idb, r_TRSz], writes=[r_pb[2]])
                    for pr in range(4):
                        tr.op("pe", lambda e, pr=pr: e.matmul(pb[2][0:32, 128:192], lhsT=KTn[:, pr, :], rhs=QZ[:, pr, :],
                                                              start=False, stop=(pr == 3)),
                              reads=[r_KTn, r_QZ], writes=[r_pb[2]])
                    tr.op("act", lambda e: e.activation(out=PTn[0:32, :], in_=pb[2][0:32, 128:192], func=AF.Exp),
                          reads=[r_pb[2], r_PTn], writes=[r_PTn])
                    if "S3" in stages:
                        continue
                    for j in range(64):
                        vp = VP[j % 4]; rvp = r_VP[j % 4]
                        vb = VPb[j % 4]; rvb = r_VPb[j % 4]
                        tr.gather(vp[:, :], cache_v[:, :], IDX[:, j:j + 1], reads=[r_IDX], writes=[rvp])
                        tr.op("act", lambda e, vp=vp, vb=vb: e.activation(out=vb[:], in_=vp[:], func=AF.Copy),
                              reads=[rvp], writes=[rvb])
                        tr.op("pe", lambda e, j=j, vb=vb: e.matmul(pb[5][0:64, :], lhsT=PTs[:, j, :], rhs=vb[:],
                                                                   start=(j == 0), stop=False),
                              reads=[r_PTs[j // 8], rvb], writes=[r_pb[5]])
                        tr.op("pe", lambda e, j=j: e.matmul(pb[6][0:64, 0:2], lhsT=PTs[:, j, :], rhs=ones_b[:, :],
                                                            start=(j == 0), stop=False),
                              reads=[r_PTs[j // 8], r_onesb], writes=[r_pb[6]])
                    tr.op("pe", lambda e: e.matmul(pb[5][0:64, :], lhsT=PTn[:, :], rhs=VNb[:, :],
                                                   start=False, stop=True),
                          reads=[r_PTn, r_VNb], writes=[r_pb[5]])
                    tr.op("pe", lambda e: e.matmul(pb[6][0:64, 0:2], lhsT=PTn[:, :], rhs=ones_b[:, :],
                                                   start=False, stop=True),
                          reads=[r_PTn, r_onesb], writes=[r_pb[6]])
                    if "S4" in stages:
                        continue
                    tr.op("act", lambda e: e.activation(out=O2[:], in_=pb[5][0:64, :], func=AF.Copy),
                          reads=[r_pb[5]], writes=[r_O2])
                    tr.op("dve", lambda e: e.reciprocal(out=rs[0:64, 0:1], in_=pb[6][0:64, 0:1]),
                          reads=[r_pb[6], r_rs], writes=[r_rs])
                    tr.op("dve", lambda e: e.tensor_tensor(out=O2[:], in0=O2[:], in1=DM[:], op=ALU.mult),
                          reads=[r_O2, r_DM], writes=[r_O2])
                    tr.op("dve", lambda e: e.tensor_reduce(
                        out=AT2[:], in_=O2[:].rearrange("p (h d) -> p d h", h=NH), axis=AX.X, op=ALU.add),
                        reads=[r_O2], writes=[r_AT2])
                    tr.op("dve", lambda e: e.tensor_scalar(out=AT2[:], in0=AT2[:], scalar1=rs[0:64, 0:1],
                                                           scalar2=None, op0=ALU.mult),
                          reads=[r_AT2, r_rs], writes=[r_AT2])
                    tr.op("act", lambda e: e.activation(out=FU[:], in_=AT2[:], func=AF.Square,
                                                        accum_out=rs[0:64, 1:2]),
                          reads=[r_AT2, r_rs], writes=[r_FU, r_rs])
                    tr.op("pe", lambda e: e.matmul(pb[2][0:64, 256:258], lhsT=SEL[:, :], rhs=rs[:, 1:3],
                                                   start=True, stop=True),
                          reads=[r_SEL, r_rs], writes=[r_pb[2]])
                    tr.op("act", lambda e: e.activation(out=rs[0:64, 3:4], in_=pb[2][0:64, 256:257], func=AF.Sqrt,
                                                        scale=1.0 / 512.0, bias=EPS),
                          reads=[r_pb[2], r_rs], writes=[r_rs])
                    tr.op("dve", lambda e: e.reciprocal(out=rs[0:64, 4:5], in_=rs[0:64, 3:4]), reads=[r_rs],
                          writes=[r_rs])
                    tr.op("dve", lambda e: e.scalar_tensor_tensor(
                        out=FU[:], in0=AT2[:], scalar=rs[0:64, 4:5], in1=AOG2[:], op0=ALU.mult, op1=ALU.mult),
                        reads=[r_AT2, r_rs, r_AOG2], writes=[r_FU])
                    for pp in range(2):
                        tr.op("dve", lambda e, pp=pp: e.tensor_scalar(
                            out=TP[0:64, pp * 64:(pp + 1) * 64], in0=FU[:], scalar1=PAR[:, pp:pp + 1],
                            scalar2=None, op0=ALU.mult), reads=[r_FU, r_PAR, r_TP], writes=[r_TP])
                    tr.op("pe", lambda e: e.transpose(out=pbT[:, 0:P], in_=TP[:, :], identity=idb[:]),
                          reads=[r_TP, r_idb], writes=[r_pbT])
                    tv = pbT[:, 0:64].rearrange("p (a b q) -> p a b q", a=4, b=2)
                    for pp in range(2):
                        tr.op("dve", lambda e, pp=pp, s=s: e.tensor_copy(
                            out=ATTs[pp * 64:(pp + 1) * 64, :, s * 8:(s + 1) * 8],
                            in_=tv[pp * 64:(pp + 1) * 64, :, pp, :]),
                            reads=[r_pbT, r_ATTs], writes=[r_ATTs])
                if not any(l in stages for l in ("S00", "S0", "S1", "S2", "S3", "S4", "S5")):
                    C.outproj(NS, xs[:, :], lambda kc: ATTs[:, kc, :], r_ATTs,
                              lambda kc: YNs[:, kc, :], r_YNs, NOT_)
        tr.barrier()
        if DO_MOE:
            with contextlib.ExitStack() as es:
                H2T = sb(es, "H2T", [P, 8, TOKS], BF16); r_H2T = Res()
                tr.dma("sp", H2T[:], h2d[:, :, :], reads=r_h2d, writes=[r_H2T])
                ACC = sb(es, "ACC", [P, NOT_ + 1, D]); r_ACC = [Res() for _ in range(NOT_ + 1)]
                tr.op("pool", lambda e: e.memset(ACC[:], 0.0), writes=r_ACC)
                ACTT = [sb(es, f"ACTT{i}", [P, 2, TOKS], BF16) for i in range(2)]
                r_ACTT = [[Res() for _ in range(5)] for _ in range(2)]
                WG = [sb(es, f"WG{i}", [P, 8, 512], BF16) for i in range(2)]; r_WG = [Res(), Res()]
                WD = [sb(es, f"WD{i}", [P, 2, D], BF16) for i in range(2)]; r_WD = [Res(), Res()]
                sg = [sb(es, f"sg{i}", [P, 512]) for i in range(2)]; r_sg = [Res(), Res()]
                groups = [(0, 512), (512, 512), (1024, 512), (1536, 512), (2048, 32)]
                gcnt = [0]; dcnt = [0]

                def moe_gu(e_, dgen):
                    wg = WG[e_ % 2]; rwg = r_WG[e_ % 2]
                    wd = WD[e_ % 2]; rwd = r_WD[e_ % 2]
                    tr.dma("pool", wg[:], w_gu[e_].rearrange("(kc p) n -> p kc n", p=P), writes=[rwg])
                    tr.dma("pool", wd[:], w_dn[e_].rearrange("(fc p) n -> p fc n", p=P), writes=[rwd])
                    at = ACTT[e_ % 2]
                    for gi, (c0, w) in enumerate(groups):
                        for fc in range(2):
                            b0 = 2 * (gcnt[0] % 2)
                            sgt = sg[gcnt[0] % 2]; rsg = r_sg[gcnt[0] % 2]
                            gcnt[0] += 1
                            for (bk, col) in ((b0, fc * P), (b0 + 1, 256 + fc * P)):
                                for kc in range(8):
                                    tr.op("pe", lambda e, kc=kc, bk=bk, col=col, c0=c0, w=w: e.matmul(
                                        pb[bk][:, 0:w], lhsT=wg[:, kc, col:col + P], rhs=H2T[:, kc, c0:c0 + w],
                                        start=(kc == 0), stop=(kc == 7)), reads=[rwg, r_H2T], writes=[r_pb[bk]])
                            tr.op("act", lambda e, b0=b0, w=w, sgt=sgt: e.activation(
                                out=sgt[:, 0:w], in_=pb[b0][:, 0:w], func=AF.Silu),
                                reads=[r_pb[b0]], writes=[rsg])
                            tr.op("dve", lambda e, b0=b0, w=w, sgt=sgt, fc=fc, c0=c0, at=at: e.tensor_tensor(
                                out=at[:, fc, c0:c0 + w], in0=sgt[:, 0:w], in1=pb[b0 + 1][:, 0:w], op=ALU.mult),
                                reads=[rsg, r_pb[b0 + 1]], writes=[r_ACTT[e_ % 2][gi]])
                            if dgen is not None:
                                for _ in range(4):
                                    next(dgen, None)
                    if dgen is not None:
                        for _ in dgen:
                            pass

                def moe_down(e_):
                    wd = WD[e_ % 2]; rwd = r_WD[e_ % 2]
                    at = ACTT[e_ % 2]
                    for t in range(NOT_ + 1):
                        n = P if t < NOT_ else 32
                        gi = t // 4
                        for hf in range(2):
                            bk = 4 + (dcnt[0] % 3)
                            dcnt[0] += 1
                            for fc in range(2):
                                tr.op("pe", lambda e, fc=fc, bk=bk, hf=hf, t=t, n=n: e.matmul(
                                    pb[bk][0:n, :], lhsT=at[:, fc, t * P:t * P + n],
                                    rhs=wd[:, fc, hf * 512:(hf + 1) * 512], start=(fc == 0), stop=(fc == 1)),
                                    reads=[r_ACTT[e_ % 2][gi], rwd], writes=[r_pb[bk]])
                            acc = ACC[0:n, t, hf * 512:(hf + 1) * 512]
                            if e_ < NE:
                                tr.op("dve", lambda e, bk=bk, n=n, t=t, acc=acc: e.scalar_tensor_tensor(
                                    out=acc, in0=pb[bk][0:n, :], scalar=GT[0:n, t, e_:e_ + 1], in1=acc,
                                    op0=ALU.mult, op1=ALU.add),
                                    reads=[r_pb[bk], r_GT[t], r_ACC[t]], writes=[r_ACC[t]])
                            else:
                                tr.op("dve", lambda e, bk=bk, n=n, acc=acc: e.tensor_tensor(
                                    out=acc, in0=pb[bk][0:n, :], in1=acc, op=ALU.add),
                                    reads=[r_pb[bk], r_ACC[t]], writes=[r_ACC[t]])
                            yield

                for e_ in range(NE + 1):
                    moe_gu(e_, moe_down(e_ - 1) if e_ > 0 else None)
                for _ in moe_down(NE):
                    pass

                bGF = sb(es, "bGF", [P, D]); r_bGF = Res()
                bFG = sb(es, "bFG", [P, D]); r_bFG = Res()
                xf = sb(es, "xf", [P, D]); r_xf = Res()
                yf = sb(es, "yf", [P, D]); r_yf = Res()
                jk = sb(es, "jk", [P, D], BF16); r_jk = Res()
                st3 = sb(es, "st3", [P, 4]); r_st3 = Res()
                tr.dma("sp", bFG[:], fing.partition_broadcast(P), writes=[r_bFG])
                for t in range(NOT_ + 1):
                    n = P if t < NOT_ else 32
                    R = slice(0, n)
                    if t == 0:
                        load_bc([(bGF, r_bGF, 5, None)], [(0, 0, P)], None)
                    if t == NOT_:
                        load_bc([(bGF, r_bGF, 5, None)], SROWS, None)
                    tr.dma("sp", xf[R, :], xmid[t * P:t * P + n, :], reads=[r_xmid[t]], writes=[r_xf])
                    tr.op("dve", lambda e, t=t: e.tensor_tensor(out=yf[R, :], in0=ACC[R, t, :], in1=bGF[R, :],
                                                                op=ALU.mult),
                          reads=[r_ACC[t], r_bGF], writes=[r_yf])
                    tr.op("pool", lambda e: e.tensor_tensor(out=yf[R, :], in0=yf[R, :], in1=xf[R, :], op=ALU.add),
                          reads=[r_yf, r_xf], writes=[r_yf])
                    tr.op("act", lambda e: e.activation(out=jk[R, :], in_=yf[R, :], func=AF.Square,
                                                        accum_out=st3[R, 0:1]),
                          reads=[r_yf], writes=[r_jk, r_st3])
                    tr.op("act", lambda e: e.activation(out=st3[R, 1:2], in_=st3[R, 0:1], func=AF.Sqrt,
                                                        scale=1.0 / D, bias=EPS), reads=[r_st3], writes=[r_st3])
                    tr.op("dve", lambda e: e.reciprocal(out=st3[R, 2:3], in_=st3[R, 1:2]),
                          reads=[r_st3], writes=[r_st3])
                    tr.op("dve", lambda e: e.scalar_tensor_tensor(
                        out=xf[R, :], in0=yf[R, :], scalar=st3[R, 2:3], in1=bFG[R, :],
                        op0=ALU.mult, op1=ALU.mult), reads=[r_yf, r_st3, r_bFG], writes=[r_xf])
                    ro = Res()
                    if t < NOT_:
                        tr.dma("sp", o_y[t * P:(t + 1) * P, :], xf[:, :], reads=[r_xf], writes=[ro])
                    else:
                        tr.dma("sp", o_ys[:, :], xf[R, :], reads=[r_xf], writes=[ro])
                    out_res.append(ro)
        tr.finish(out_res)
    return nc


def _rope_table(pos):
    half = 32
    inv = (10000.0 ** (-(np.arange(half, dtype=np.float32) / np.float32(half)))).astype(np.float32)
    ang = pos.astype(np.float32)[:, None] * inv[None, :]
    return np.concatenate([np.cos(ang), np.sin(ang)], axis=1).astype(np.float32)


def prep_inputs(inp):
    f = lambda a: np.ascontiguousarray(a, dtype=np.float32)
    maps = []
    S = 4096
    tri = np.where(np.arange(P)[:, None] <= np.arange(P)[None, :], 0.0, NEG).astype(np.float32)
    ohm = np.zeros((16, 16 * P), np.float32)
    for n in range(16):
        ohm[n, n * P:(n + 1) * P] = 1.0
    ohs = np.zeros((32, 32 * P), np.float32)
    for n in range(32):
        ohs[n, n * P:(n + 1) * P] = 1.0
    trs = np.full((32, 4, NH, 8), NEG, np.float32)
    for s in range(4):
        for tk in range(8):
            for tq in range(8):
                if tk <= tq:
                    trs[8 * s + tk, s, :, tq] = 0.0
    trs = trs.reshape(32, 256)
    selq = np.zeros((P, 64), np.float32)
    for h in range(NH):
        for h2 in range(NH):
            for q in range(8):
                selq[h * 8 + q, h2 * 8 + q] = 1.0
    dmask = np.zeros((64, NH, 64), np.float32)
    for h in range(NH):
        dmask[h * 8:(h + 1) * 8, h, :] = 1.0
    dmask = dmask.reshape(64, 512)
    par = np.zeros((64, 2), np.float32)
    for h in range(NH):
        par[h * 8:(h + 1) * 8, h % 2] = 1.0
    iot = np.arange(P, dtype=np.float32).reshape(P, 1)
    wab = np.zeros((P, 4, P), np.float32); wxb = np.zeros((P, 4, P), np.float32)
    ga = inp["gate_a_w"][0]; gx = inp["gate_x_w"][0]
    for ci in range(4):
        for j in range(2):
            wab[64 * j:64 * j + 64, ci, 64 * j:64 * j + 64] = ga[2 * ci + j]
            wxb[64 * j:64 * j + 64, ci, 64 * j:64 * j + 64] = gx[2 * ci + j]
    lv = np.zeros((P, 4, 12), np.float32)
    def fm(v):
        return v.reshape(4, P).T
    cw = inp["conv_w"][0]
    for jj in range(4):
        lv[:, :, jj] = fm(cw[jj])
    lv[:, :, 4] = fm(inp["conv_b"][0]); lv[:, :, 5] = fm(inp["gate_a_b"][0])
    lv[:, :, 6] = fm(inp["gate_x_b"][0]); lv[:, :, 7] = fm(inp["lru_lambda"][0])
    lv[:, :, 8] = fm(inp["lru_out_g"][0])
    w_gu = np.concatenate([inp["exp_w_gu"][0], inp["shared_w_gu"]], axis=0)
    w_dn = np.concatenate([inp["exp_w_down"][0], inp["shared_w_down"]], axis=0)
    ck = inp["cache_k"][0].reshape(2560 * P, 512)
    cv = inp["cache_v"][0].reshape(2560 * P, 512)
    ropes = _rope_table(8192 + np.arange(8))
    for c in range(8):
        b, half = c // 2, c % 2
        x = inp["x_prompt"][b]
        if half == 1:
            xpl = x
            pos = np.arange(S)
        else:
            xpl = np.concatenate([np.zeros((2048, D), np.float32), x[:2048]], axis=0)
            pos = np.concatenate([np.zeros(2048, np.int64), np.arange(2048)])
        cs = np.concatenate([inp["c_prompt"][b:b + 1], inp["c_sample"][4 * c:4 * c + 4]], axis=0)
        c5 = cs.T.reshape(8, P, 5).transpose(1, 0, 2)
        bl = np.zeros((8, 3, 16), np.float32)
        for j in range(8):
            cur = 8 + j
            for n in range(16):
                valid_past = (n < cur) and (half == 1 or n >= 8)
                bl[j, 0, n] = 0.0 if valid_past else -1e30
                bl[j, 1, n] = 0.0 if n == cur else 1.0
                bl[j, 2, n] = 0.0 if (valid_past or n == cur) else NEG
        fl = np.zeros((P, 2), np.float32)
        fl[:, 0] = float(half); fl[:, 1] = 1.0 - float(half)
        sh = inp["state_h"][0, 4 * c:4 * c + 4]
        sc = inp["state_conv"][0, 4 * c:4 * c + 4]
        sth = sh.reshape(4, 4, P).transpose(2, 1, 0)
        stc = sc.reshape(4, 3, 4, P).transpose(3, 2, 0, 1)
        m = {
            "xp": f(xpl), "xs": f(inp["x_sample"][4 * c:4 * c + 4].reshape(32, D)), "c5": f(c5),
            "ada_w": f(inp["ada_w"][0]), "ada_b": f(inp["ada_b"][0]),
            "norm_mix_g": f(inp["norm_mix_g"][0]), "norm_ffn_g": f(inp["norm_ffn_g"][0]),
            "final_g": f(inp["final_g"]), "w_in": f(inp["w_in"][0]), "w_out": f(inp["w_out"][0]),
            "rope": _rope_table(pos), "ropes": ropes, "lruv": lv, "wab": wab, "wxb": wxb,
            "attn_out_g": f(inp["attn_out_g"][0]), "blkc": f(bl.reshape(-1)), "flags": fl,
            "trim": tri, "ohm": ohm, "router_w": f(inp["router_w"][0]),
            "router_bias": f(inp["router_bias"][0]), "w_gu": w_gu, "w_dn": w_dn,
            "cache_k": ck, "cache_v": cv,
            "ptab": np.ascontiguousarray(inp["page_table"][4 * c:4 * c + 4], dtype=np.int32),
            "sth": f(sth), "stc": f(stc), "ohs": ohs, "trs": trs, "iot": iot, "selq": selq, "dmask": dmask, "par": par,
        }
        maps.append(m)
    return maps


def assemble(results):
    y_p = np.zeros((4, 4096, D), np.float32)
    k_p = np.zeros((1, 4, 4096, NH, HD), np.float32)
    v_p = np.zeros((1, 4, 4096, NH, HD), np.float32)
    h_p = np.zeros((1, 4, 512), np.float32)
    c_p = np.zeros((1, 4, 3, 512), np.float32)
    y_s = np.zeros((32, 8, D), np.float32)
    k_s = np.zeros((1, 32, 8, NH, HD), np.float32)
    v_s = np.zeros((1, 32, 8, NH, HD), np.float32)
    h_s = np.zeros((1, 32, 512), np.float32)
    c_s = np.zeros((1, 32, 3, 512), np.float32)
    for c in range(8):
        b, half = c // 2, c % 2
        r = results[c]
        sl = slice(half * 2048, half * 2048 + 2048)
        y_p[b, sl] = r["o_y"]
        k_p[0, b, sl] = r["o_k"].reshape(2048, NH, HD)
        v_p[0, b, sl] = r["o_v"].reshape(2048, NH, HD)
        if half == 1:
            h_p[0, b] = r["o_h"]
            c_p[0, b] = r["o_c"]
        y_s[4 * c:4 * c + 4] = r["o_ys"].reshape(4, 8, D)
        k_s[0, 4 * c:4 * c + 4] = r["o_ks"].reshape(4, 8, NH, HD)
        v_s[0, 4 * c:4 * c + 4] = r["o_vs"].reshape(4, 8, NH, HD)
        h_s[0, 4 * c:4 * c + 4] = r["o_hs"]
        c_s[0, 4 * c:4 * c + 4] = r["o_cs"].reshape(4, 3, 512)
    return (y_p, y_s, k_p, v_p, h_p, c_p, k_s, v_s, h_s, c_s)


_NC_CACHE = {}


def kernel(**inputs):
    inp = {k: np.asarray(v) for k, v in inputs.items()}
    maps = prep_inputs(inp)
    if "nc" not in _NC_CACHE:
        _NC_CACHE["nc"] = build()
    res = run_bass_kernel_spmd(_NC_CACHE["nc"], maps, core_ids=list(range(8)))
    return assemble(res.results)
```

```python
import contextlib
import numpy as np
import concourse.bass as bass
import concourse.mybir as mybir
from concourse.bass_utils import run_bass_kernel_spmd

F32 = mybir.dt.float32
BF16 = mybir.dt.bfloat16
I32 = mybir.dt.int32
U32 = mybir.dt.uint32
ALU = mybir.AluOpType
AF = mybir.ActivationFunctionType
AX = mybir.AxisListType

SEM_LIMIT = 30000
P = 128
D = 1024
NH = 8
HD = 64
NCT = 16
NOT_ = 16
NTL = NCT + NOT_
NBL = 16
NEG = -30000.0
EPS = 1e-6
NE = 64
TOKS = NOT_ * P + 32


class Res:
    __slots__ = ("name", "w", "r", "excl")

    def __init__(self, name="", excl=False):
        self.name = name
        self.w = None
        self.r = {}
        self.excl = excl


class Eng:
    def __init__(self, tr, key, handle, step):
        self.tr = tr
        self.key = key
        self.h = handle
        self.step = step
        self.sems = []
        self.cnt = 0
        self.seen = {}
        self.new_epoch()

    def new_epoch(self):
        s = self.tr.nc.semaphore(f"s_{self.key}_{len(self.sems)}")
        self.sems.append(s.__enter__())
        self.tr._sem_guards.append(s)
        self.cnt = 0

    @property
    def epoch(self):
        return len(self.sems) - 1


class Tracker:
    def __init__(self, nc, ndma=8):
        self.nc = nc
        self._sem_guards = []
        self.e = {}
        for key, h in (("pe", nc.tensor), ("act", nc.scalar), ("dve", nc.vector),
                       ("pool", nc.gpsimd), ("sp", nc.sync)):
            self.e[key] = Eng(self, key, h, 1)
        self.dq = {}
        for q in ("sp", "act", "pool"):
            ring = []
            for i in range(ndma):
                eng = Eng(self, f"dq_{q}{i}", None, 16)
                ring.append(eng)
                self.e[eng.key] = eng
            self.dq[q] = [ring, 0]

    def _wait(self, eng, dep):
        k, ep, n = dep
        if eng.seen.get((k, ep), 0) >= n:
            return
        eng.seen[(k, ep)] = n
        eng.h.wait_ge(self.e[k].sems[ep], n)

    def _deps(self, reads, writes):
        deps = set()
        for res in reads:
            if res.w is not None:
                deps.add(res.w)
            if res.excl:
                for k, (ep, n) in res.r.items():
                    deps.add((k, ep, n))
        for res in writes:
            if res.w is not None:
                deps.add(res.w)
            for k, (ep, n) in res.r.items():
                deps.add((k, ep, n))
        return deps

    def op(self, engkey, fn, reads=(), writes=()):
        eng = self.e[engkey]
        for dep in sorted(self._deps(reads, writes)):
            if dep[0] == engkey and engkey == "pe":
                continue
            self._wait(eng, dep)
        if eng.cnt + 1 > SEM_LIMIT:
            eng.new_epoch()
        inst = fn(eng.h)
        eng.cnt += 1
        inst.then_inc(eng.sems[eng.epoch], 1)
        pos = (eng.epoch, eng.cnt)
        for res in writes:
            res.w = (engkey, pos[0], pos[1])
            res.r = {}
        for res in reads:
            res.r[engkey] = pos
        return inst

    def dma(self, q, out, in_, reads=(), writes=(), **kw):
        issuer = self.e[q]
        ring, idx = self.dq[q]
        deng = ring[idx % len(ring)]
        self.dq[q][1] = idx + 1
        if deng.cnt > 0:
            self._wait(issuer, (deng.key, deng.epoch, deng.cnt))
        for dep in sorted(self._deps(reads, writes)):
            self._wait(issuer, dep)
        if deng.cnt + 16 > SEM_LIMIT:
            deng.new_epoch()
        inst = issuer.h.dma_start(out=out, in_=in_, **kw)
        deng.cnt += 16
        inst.then_inc(deng.sems[deng.epoch], 16)
        pos = (deng.epoch, deng.cnt)
        for res in writes:
            res.w = (deng.key, pos[0], pos[1])
            res.r = {}
        for res in reads:
            res.r[deng.key] = pos
        return inst

    def gather(self, out, in_, idx, reads=(), writes=()):
        issuer = self.e["pool"]
        ring, i = self.dq["pool"]
        deng = ring[i % len(ring)]
        self.dq["pool"][1] = i + 1
        if deng.cnt > 0:
            self._wait(issuer, (deng.key, deng.epoch, deng.cnt))
        for dep in sorted(self._deps(reads, writes)):
            self._wait(issuer, dep)
        if deng.cnt + 16 > SEM_LIMIT:
            deng.new_epoch()
        inst = issuer.h.indirect_dma_start(out=out, out_offset=None, in_=in_,
                                           in_offset=bass.IndirectOffsetOnAxis(ap=idx, axis=0))
        deng.cnt += 16
        inst.then_inc(deng.sems[deng.epoch], 16)
        pos = (deng.epoch, deng.cnt)
        for res in writes:
            res.w = (deng.key, pos[0], pos[1])
            res.r = {}
        for res in reads:
            res.r[deng.key] = pos
        return inst

    def barrier(self):
        targets = [(k, e.epoch, e.cnt) for k, e in self.e.items() if e.cnt > 0]
        for key in ("pe", "act", "dve", "pool", "sp"):
            eng = self.e[key]
            for dep in targets:
                if dep[0] == key:
                    continue
                self._wait(eng, dep)

    def finish(self, outs):
        eng = self.e["sp"]
        for res in outs:
            if res.w is not None:
                self._wait(eng, res.w)
            for k, (ep, n) in res.r.items():
                self._wait(eng, (k, ep, n))

    def close(self):
        for g in reversed(self._sem_guards):
            g.__exit__(None, None, None)


def build(stages=("all",), npool=2560):
    nc = bass.Bass("TRN2", target_bir_lowering=False)
    tr = Tracker(nc)
    ALL = "all" in stages
    DO_ATT = ALL or "ATT" in stages
    DO_MOE = ALL or "MOE" in stages
    DO_SMP = ALL or "S" in stages

    def din(name, shape, dt=F32):
        return nc.dram_tensor(name, list(shape), dt, kind="ExternalInput").ap()

    def dout(name, shape, dt=F32):
        return nc.dram_tensor(name, list(shape), dt, kind="ExternalOutput").ap()

    def dscr(name, shape, dt=F32):
        return nc.dram_tensor(name, list(shape), dt, kind="Internal").ap()

    xp = din("xp", [NTL * P, D])
    xs = din("xs", [32, D])
    c5 = din("c5", [P, 8, 5])
    ada_w = din("ada_w", [D, 6 * D])
    ada_b = din("ada_b", [6 * D])
    nmg = din("norm_mix_g", [D])
    nfg = din("norm_ffn_g", [D])
    fing = din("final_g", [D])
    w_in = din("w_in", [D, 2560])
    w_out = din("w_out", [D, D])
    rope = din("rope", [NTL * P, 64])
    ropes = din("ropes", [8, 64])
    lruv = din("lruv", [P, 4, 12])
    wab = din("wab", [P, 4, P])
    wxb = din("wxb", [P, 4, P])
    aog = din("attn_out_g", [512])
    blkc = din("blkc", [8 * 3 * 16])
    flags = din("flags", [P, 2])
    trim = din("trim", [P, P])
    ohm = din("ohm", [16, 16 * P])
    router_w = din("router_w", [D, NE])
    router_b = din("router_bias", [NE])
    if DO_MOE:
        w_gu = din("w_gu", [NE + 1, D, 512])
        w_dn = din("w_dn", [NE + 1, 256, D])
    if DO_SMP:
        cache_k = din("cache_k", [npool * P, 512])
        cache_v = din("cache_v", [npool * P, 512])
    ptab = din("ptab", [4, 64], I32)
    sth = din("sth", [P, 4, 4])
    stc = din("stc", [P, 4, 4, 3])
    ohs = din("ohs", [32, 32 * P])
    trs = din("trs", [32, 256])
    selq = din("selq", [P, 64])
    dmask = din("dmask", [64, 512])
    par = din("par", [64, 2])
    iot = din("iot", [P, 1])

    o_y = dout("o_y", [NOT_ * P, D])
    o_k = dout("o_k", [NOT_ * P, 512])
    o_v = dout("o_v", [NOT_ * P, 512])
    o_h = dout("o_h", [512])
    o_c = dout("o_c", [3, 512])
    o_ys = dout("o_ys", [32, D])
    o_ks = dout("o_ks", [32, 512])
    o_vs = dout("o_vs", [32, 512])
    o_hs = dout("o_hs", [4, 512])
    o_cs = dout("o_cs", [12, 512])

    modd = dscr("modd", [5, 6 * D])
    xmid = dscr("xmid", [TOKS, D])
    h2d = dscr("h2d", [P, 8, TOKS], BF16)

    out_res = []
    r_xmid = [Res() for _ in range(NOT_ + 1)]
    r_h2d = [Res() for _ in range(NOT_ + 1)]

    with contextlib.ExitStack() as top:
        def sb(es, name, shape, dt=F32):
            return es.enter_context(nc.sbuf_tensor(name, list(shape), dt))

        def ps(es, name, shape, dt=F32):
            return es.enter_context(nc.psum_tensor(name, list(shape), dt))

        pbT = ps(top, "pbT", [P, 1024], BF16); r_pbT = Res(excl=True)
        pb = [ps(top, f"pb{i}", [P, 512], F32) for i in range(7)]
        r_pb = [Res(excl=True) for _ in range(7)]

        idb = sb(top, "idb", [P, P], BF16); r_idb = Res()
        idf = sb(top, "idf", [P, P], F32); r_idf = Res()
        ones_f = sb(top, "ones_f", [P, P], F32); r_ones = Res()
        GT = sb(top, "GT", [P, NOT_ + 1, NE], F32); r_GT = [Res() for _ in range(NOT_ + 1)]

        tr.op("pool", lambda e: e.memset(idb[:], 0.0), writes=[r_idb])
        tr.op("pool", lambda e: e.affine_select(out=idb[:], in_=idb[:], pattern=[[-1, P]],
                                                 compare_op=ALU.not_equal, fill=1.0, base=0,
                                                 channel_multiplier=1), reads=[r_idb], writes=[r_idb])
        tr.op("pool", lambda e: e.memset(idf[:], 0.0), writes=[r_idf])
        tr.op("pool", lambda e: e.affine_select(out=idf[:], in_=idf[:], pattern=[[-1, P]],
                                                 compare_op=ALU.not_equal, fill=1.0, base=0,
                                                 channel_multiplier=1), reads=[r_idf], writes=[r_idf])
        tr.op("pool", lambda e: e.memset(ones_f[:], 1.0), writes=[r_ones])

        r_modd = Res()
        with contextlib.ExitStack() as es:
            c5t = sb(es, "c5t", [P, 8, 5]); r_c5 = Res()
            sct = sb(es, "sct", [P, 8, 5]); r_sc = Res()
            adab = sb(es, "adab", [5, 6 * D]); r_adab = Res()
            modt = sb(es, "modt", [5, 6 * D]); r_modt = Res()
            aw = [sb(es, f"aw{i}", [P, 8, 512]) for i in range(2)]
            r_aw = [Res(), Res()]
            tr.dma("sp", c5t[:], c5[:, :, :], writes=[r_c5])
            tr.dma("sp", adab[:], ada_b.partition_broadcast(5), writes=[r_adab])
            tr.op("act", lambda e: e.activation(out=sct[:], in_=c5t[:], func=AF.Silu),
                  reads=[r_c5], writes=[r_sc])
            awv = ada_w.rearrange("(kc p) n -> p kc n", p=P)
            for cc in range(12):
                a = aw[cc % 2]; ra = r_aw[cc % 2]
                tr.dma("sp", a[:], awv[:, :, cc * 512:(cc + 1) * 512], writes=[ra])
                bank = pb[cc % 2]; rb = r_pb[cc % 2]
                for kc in range(8):
                    tr.op("pe", lambda e, kc=kc, a=a, bank=bank: e.matmul(
                        bank[0:5, :], lhsT=sct[:, kc, :], rhs=a[:, kc, :],
                        start=(kc == 0), stop=(kc == 7)), reads=[r_sc, ra], writes=[rb])
                tr.op("dve", lambda e, bank=bank, cc=cc: e.tensor_tensor(
                    out=modt[:, cc * 512:(cc + 1) * 512], in0=bank[0:5, :],
                    in1=adab[:, cc * 512:(cc + 1) * 512], op=ALU.add),
                    reads=[rb, r_adab], writes=[r_modt])
            tr.dma("sp", modd[:, :], modt[:], reads=[r_modt], writes=[r_modd])
        tr.barrier()

        def load_bc(specs, rows, scratch):
            gtmp, r_g = scratch if scratch is not None else (None, None)
            hi_all = max(r[2] for r in rows)
            for (tile, res, col, g) in specs:
                for (mr, lo, hi) in rows:
                    tr.dma("sp", tile[lo:hi, :],
                           modd[mr, col * D:(col + 1) * D].partition_broadcast(hi - lo),
                           reads=[r_modd], writes=[res])
                if g is not None:
                    tr.dma("sp", gtmp[0:hi_all, :], g.partition_broadcast(hi_all), writes=[r_g])
                    tr.op("dve", lambda e, tile=tile: e.scalar_tensor_tensor(
                        out=tile[0:hi_all, :], in0=tile[0:hi_all, :], scalar=1.0,
                        in1=gtmp[0:hi_all, :], op0=ALU.add, op1=ALU.mult),
                        reads=[res, r_g], writes=[res])

        SROWS = [(1 + s, 8 * s, 8 * s + 8) for s in range(4)]
        NS = 32
        QTs = sb(top, "QTs", [P, 4, NS], BF16); r_QTs = Res()
        KTn = sb(top, "KTn", [P, 4, NS], BF16); r_KTn = Res()
        VNb = sb(top, "VNb", [P, 512], BF16); r_VNb = Res()
        YNs = sb(top, "YNs", [P, 4, NS], BF16); r_YNs = Res()
        tr.op("pool", lambda e: e.memset(VNb[:], 0.0), writes=[r_VNb])

        with contextlib.ExitStack() as es0:
            KT = sb(es0, "KT", [P, 4, NTL * P], BF16); r_KT = [Res() for _ in range(NTL)]
            VA = sb(es0, "VA", [P, NTL, NH, 65], BF16); r_VA = [Res() for _ in range(NTL)]
            tr.op("pool", lambda e: e.memset(VA[:, :, :, 64:65], 1.0), writes=r_VA)
            QTA = sb(es0, "QTA", [P, 4, NOT_ * P], BF16); r_QTA = [Res() for _ in range(NOT_)]
            YNA = sb(es0, "YNA", [P, 4, NOT_ * P], BF16); r_YNA = [Res() for _ in range(4)]
            KM = sb(es0, "KM", [P, 4, 16], BF16); r_KM = Res()

            with contextlib.ExitStack() as es:
                bS1 = sb(es, "bS1", [P, D]); r_bS1 = Res()
                bG1 = sb(es, "bG1", [P, D]); r_bG1 = Res()
                WIN = sb(es, "WIN", [P, 8, 2560], BF16); r_WIN = Res()
                winv = w_in.rearrange("(kc p) n -> p kc n", p=P)
                for cc in range(5):
                    tr.dma("pool", WIN[:, :, cc * 512:(cc + 1) * 512], winv[:, :, cc * 512:(cc + 1) * 512],
                           writes=[r_WIN])
                xt = [sb(es, f"xt{i}", [P, D]) for i in range(2)]; r_xt = [Res() for _ in range(2)]
                hb = sb(es, "hb", [P, D], BF16); r_hb = Res()
                HT = sb(es, "HT", [P, 8, 512], BF16); r_HT = [Res() for _ in range(4)]
                rp = [sb(es, f"rp{i}", [P, 64]) for i in range(2)]; r_rp = [Res(), Res()]
                krb = sb(es, "krb", [P, 512], BF16); r_krb = Res()
                rt_full = sb(es, "rt", [P, NH, 32]); r_rt = Res()
                st = sb(es, "st", [P, 8]); r_st = Res()
                LT = sb(es, "LT", [P, 9, 512]); r_LT = [Res() for _ in range(9)]
                G = [LT[:, i, :] for i in range(9)]
                cv, rr, ii, a2, aa, bb, hh, gsb, uu = G
                r_cv, r_rr, r_ii, r_a2, r_aa, r_bb, r_hh, r_gsb, r_uu = r_LT
                qs, ks, vs, kr = G[0], G[1], G[2], G[3]
                r_qs, r_ks, r_vs, r_kr = r_LT[0], r_LT[1], r_LT[2], r_LT[3]
                qr, r_qr = G[4], r_LT[4]
                tmpf = LT[:, 5:7, :].rearrange("p a b -> p (a b)"); r_tmpf2 = [r_LT[5], r_LT[6]]
                XPc = sb(es, "XPc", [P, 515]); r_XPc = Res()
                XH = sb(es, "XH", [P, 4, 3]); r_XH = [Res() for _ in range(4)]
                HS = sb(es, "HS", [P, 4]); r_HS = [Res() for _ in range(4)]
                YY = sb(es, "YY", [P, 4, 512], BF16); r_YY = [Res() for _ in range(4)]
                YQ = sb(es, "YQ", [P, 512]); r_YQ = Res()
                tcol = sb(es, "tcol", [P, 4]); r_tcol = Res()
                LV = sb(es, "LV", [P, 4, 12]); r_LV = Res()
                WAB = sb(es, "WAB", [P, 4, P]); r_WAB = Res()
                WXB = sb(es, "WXB", [P, 4, P]); r_WXB = Res()
                LC1 = sb(es, "LC1", [P, 4]); r_LC1 = Res()
                KMf = sb(es, "KMf", [P, 4, 16]); r_KMf = Res()
                FL = sb(es, "FL", [P, 2]); r_FL = Res()
                tr.dma("sp", FL[:], flags[:, :], writes=[r_FL])
                tr.dma("sp", LV[:], lruv[:, :, :], writes=[r_LV])
                tr.dma("sp", WAB[:], wab[:, :, :], writes=[r_WAB])
                tr.dma("sp", WXB[:], wxb[:, :, :], writes=[r_WXB])
                tr.op("act", lambda e: e.activation(out=LC1[:], in_=LV[:, :, 7], func=AF.Exp, scale=-1.0),
                      reads=[r_LV], writes=[r_LC1])
                tr.op("act", lambda e: e.activation(out=LC1[:], in_=LC1[:], func=AF.Ln, bias=1.0),
                      reads=[r_LC1], writes=[r_LC1])
                tr.op("dve", lambda e: e.tensor_scalar(out=LC1[:], in0=LC1[:], scalar1=-8.0, scalar2=None,
                                                       op0=ALU.mult), reads=[r_LC1], writes=[r_LC1])
                tr.op("pool", lambda e: e.memset(KMf[:], 0.0), writes=[r_KMf])
                tr.op("pool", lambda e: e.memset(HS[:], 0.0), writes=r_HS)
                tr.op("pool", lambda e: e.memset(XH[:], 0.0), writes=r_XH)
                load_bc([(bS1, r_bS1, 0, None), (bG1, r_bG1, 1, nmg)], [(0, 0, P)], (xt[1], r_xt[1]))

                def rope_apply(src, r_src, dst, r_dst, rpt, rrp, n=P):
                    s3 = src[0:n, :].rearrange("p (h d) -> p h d", h=NH)
                    d3 = dst[0:n, :].rearrange("p (h d) -> p h d", h=NH)
                    cosb = rpt[0:n, 0:32].unsqueeze(1).broadcast_to([n, NH, 32])
                    sinb = rpt[0:n, 32:64].unsqueeze(1).broadcast_to([n, NH, 32])
                    rt = rt_full[0:n]
                    x1 = s3[:, :, 0:32]; x2 = s3[:, :, 32:64]
                    tr.op("pool", lambda e: e.tensor_tensor(out=d3[:, :, 0:32], in0=x1, in1=cosb, op=ALU.mult),
                          reads=[r_src, rrp], writes=[r_dst])
                    tr.op("pool", lambda e: e.tensor_tensor(out=rt, in0=x2, in1=sinb, op=ALU.mult),
                          reads=[r_src, rrp], writes=[r_rt])
                    tr.op("pool", lambda e: e.tensor_tensor(out=d3[:, :, 0:32], in0=d3[:, :, 0:32], in1=rt,
                                                            op=ALU.subtract),
                          reads=[r_dst, r_rt], writes=[r_dst])
                    tr.op("pool", lambda e: e.tensor_tensor(out=d3[:, :, 32:64], in0=x2, in1=cosb, op=ALU.mult),
                          reads=[r_src, rrp], writes=[r_dst])
                    tr.op("pool", lambda e: e.tensor_tensor(out=rt, in0=x1, in1=sinb, op=ALU.mult),
                          reads=[r_src, rrp], writes=[r_rt])
                    tr.op("pool", lambda e: e.tensor_tensor(out=d3[:, :, 32:64], in0=d3[:, :, 32:64], in1=rt,
                                                            op=ALU.add),
                          reads=[r_dst, r_rt], writes=[r_dst])

                def norm_to_HT(x, rx, n, j0):
                    R = slice(0, n)
                    tr.op("act", lambda e: e.activation(out=hb[R, :], in_=x[R, :], func=AF.Square,
                                                        accum_out=st[R, 0:1]),
                          reads=[rx], writes=[r_hb, r_st])
                    tr.op("act", lambda e: e.activation(out=st[R, 1:2], in_=st[R, 0:1], func=AF.Sqrt,
                                                        scale=1.0 / D, bias=EPS), reads=[r_st], writes=[r_st])
                    tr.op("dve", lambda e: e.reciprocal(out=st[R, 2:3], in_=st[R, 1:2]),
                          reads=[r_st], writes=[r_st])
                    tr.op("dve", lambda e: e.scalar_tensor_tensor(
                        out=tmpf[R, :], in0=x[R, :], scalar=st[R, 2:3], in1=bG1[R, :],
                        op0=ALU.mult, op1=ALU.mult), reads=[rx, r_st, r_bG1], writes=r_tmpf2)
                    tr.op("pool", lambda e: e.tensor_tensor(out=hb[R, :], in0=tmpf[R, :], in1=bS1[R, :], op=ALU.add),
                          reads=r_tmpf2 + [r_bS1], writes=[r_hb])
                    for kc in range(8):
                        tr.op("pe", lambda e, kc=kc: e.transpose(out=pbT[:, kc * P:kc * P + n],
                                                                 in_=hb[R, kc * P:(kc + 1) * P],
                                                                 identity=idb[R, R]),
                              reads=[r_hb, r_idb], writes=[r_pbT])
                    tr.op("act", lambda e: e.activation(
                        out=HT[:, :, j0:j0 + n], in_=pbT[:].rearrange("p (k c) -> p k c", k=8)[:, :, 0:n],
                        func=AF.Copy), reads=[r_pbT], writes=[r_HT[j0 // P]])

                def proj_tok(n, j0, which):
                    for (bi_, c0) in which:
                        for kc in range(8):
                            tr.op("pe", lambda e, kc=kc, bi_=bi_, c0=c0: e.matmul(
                                pb[bi_][0:n, :], lhsT=HT[:, kc, j0:j0 + n], rhs=WIN[:, kc, c0:c0 + 512],
                                start=(kc == 0), stop=(kc == 7)),
                                reads=[r_HT[j0 // P], r_WIN], writes=[r_pb[bi_]])

                def transpose4(srcb, r_srcb, n, dst, r_dst):
                    for pr in range(4):
                        tr.op("pe", lambda e, pr=pr: e.transpose(out=pbT[:, pr * P:pr * P + n],
                                                                 in_=srcb[0:n, pr * P:(pr + 1) * P],
                                                                 identity=idb[0:n, 0:n]),
                              reads=[r_srcb, r_idb], writes=[r_pbT])
                    tr.op("dve", lambda e: e.tensor_copy(
                        out=dst, in_=pbT[:, 0:512].rearrange("p (k c) -> p k c", k=4)[:, :, 0:n]),
                        reads=[r_pbT], writes=[r_dst])

                def stage_front(t):
                    x = xt[t % 2]; rx = r_xt[t % 2]
                    tr.dma("sp", x[:], xp[t * P:(t + 1) * P, :], writes=[rx])
                    rpt = rp[t % 2]; rrp = r_rp[t % 2]
                    tr.dma("sp", rpt[:], rope[t * P:(t + 1) * P, :], writes=[rrp])
                    norm_to_HT(x, rx, P, (t % 4) * P)

                def stage_a(t):
                    own = t >= NCT
                    rpt = rp[t % 2]; rrp = r_rp[t % 2]
                    j = t % 4
                    proj_tok(P, j * P, [(1, 512), (2, 1024)] + ([(0, 0)] if (own and DO_ATT) else []))
                    tr.op("act", lambda e: e.activation(out=ks, in_=pb[1][:], func=AF.Copy),
                          reads=[r_pb[1]], writes=[r_ks])
                    tr.op("act", lambda e: e.activation(out=vs, in_=pb[2][:], func=AF.Copy),
                          reads=[r_pb[2]], writes=[r_vs])
                    tr.op("pool", lambda e: e.tensor_copy(
                        out=VA[:, t, :, 0:64], in_=vs.rearrange("p (h d) -> p h d", h=NH)),
                        reads=[r_vs], writes=[r_VA[t]])
                    rope_apply(ks, r_ks, kr, r_kr, rpt, rrp)
                    tr.op("pool", lambda e: e.tensor_copy(out=krb[:], in_=kr), reads=[r_kr], writes=[r_krb])
                    transpose4(krb, r_krb, P, KT[:, :, t * P:(t + 1) * P], r_KT[t])
                    if own:
                        to = t - NCT
                        r1 = Res(); r2 = Res()
                        tr.dma("sp", o_k[to * P:(to + 1) * P, :], kr, reads=[r_kr], writes=[r1])
                        tr.dma("sp", o_v[to * P:(to + 1) * P, :], vs, reads=[r_vs], writes=[r2])
                        out_res.extend([r1, r2])
                        if DO_ATT:
                            tr.op("act", lambda e: e.activation(out=qs, in_=pb[0][:], func=AF.Copy, scale=0.125),
                                  reads=[r_pb[0]], writes=[r_qs])
                            rope_apply(qs, r_qs, qr, r_qr, rpt, rrp)
                            tr.op("pool", lambda e: e.tensor_copy(out=krb[:], in_=qr), reads=[r_qr],
                                  writes=[r_krb])
                            transpose4(krb, r_krb, P, QTA[:, :, to * P:(to + 1) * P], r_QTA[to])

                def kmean(blk):
                    tr.op("dve", lambda e: e.tensor_reduce(
                        out=KMf[:, :, blk], in_=KT[:, :, blk * 256:(blk + 1) * 256], axis=AX.X, op=ALU.add),
                        reads=[r_KT[2 * blk], r_KT[2 * blk + 1]], writes=[r_KMf])
                    tr.op("dve", lambda e: e.tensor_scalar(out=KM[:, :, blk], in0=KMf[:, :, blk],
                                                           scalar1=1.0 / 256.0, scalar2=None, op0=ALU.mult),
                          reads=[r_KMf], writes=[r_KM])

                def lru_core(ci, W, nseg, resets, hist_src, init_fn, own, ydst):
                    L = W // nseg
                    xv = XPc[:, 0:nseg * (L + 3)].rearrange("p (s c) -> p s c", s=nseg)

                    def v3(ap2):
                        return ap2[:, 0:W].rearrange("p (s c) -> p s c", s=nseg)
                    tr.op("dve", lambda e: e.tensor_scalar(
                        out=v3(cv), in0=xv[:, :, 3:3 + L], scalar1=LV[:, ci, 3:4], scalar2=LV[:, ci, 4:5],
                        op0=ALU.mult, op1=ALU.add), reads=[r_XPc, r_LV], writes=[r_cv])
                    for jj in range(3):
                        tr.op("dve", lambda e, jj=jj: e.scalar_tensor_tensor(
                            out=v3(cv), in0=xv[:, :, jj:jj + L], scalar=LV[:, ci, jj:jj + 1], in1=v3(cv),
                            op0=ALU.mult, op1=ALU.add), reads=[r_XPc, r_LV, r_cv], writes=[r_cv])
                    tr.op("pe", lambda e: e.matmul(pb[5][:, 0:W], lhsT=WAB[:, ci, :], rhs=cv[:, 0:W],
                                                   start=True, stop=True),
                          reads=[r_WAB, r_cv], writes=[r_pb[5]])
                    tr.op("pe", lambda e: e.matmul(pb[6][:, 0:W], lhsT=WXB[:, ci, :], rhs=cv[:, 0:W],
                                                   start=True, stop=True),
                          reads=[r_WXB, r_cv], writes=[r_pb[6]])
                    tr.op("act", lambda e: e.activation(out=rr[:, 0:W], in_=pb[5][:, 0:W], func=AF.Sigmoid,
                                                        bias=LV[:, ci, 5:6]),
                          reads=[r_pb[5], r_LV], writes=[r_rr])
                    tr.op("act", lambda e: e.activation(out=ii[:, 0:W], in_=pb[6][:, 0:W], func=AF.Sigmoid,
                                                        bias=LV[:, ci, 6:7]),
                          reads=[r_pb[6], r_LV], writes=[r_ii])
                    tr.op("act", lambda e: e.activation(out=aa[:, 0:W], in_=rr[:, 0:W], func=AF.Exp,
                                                        scale=LC1[:, ci:ci + 1]),
                          reads=[r_rr, r_LC1], writes=[r_aa])
                    tr.op("pool", lambda e: e.tensor_tensor(out=a2[:, 0:W], in0=aa[:, 0:W], in1=aa[:, 0:W],
                                                            op=ALU.mult), reads=[r_aa], writes=[r_a2])
                    tr.op("act", lambda e: e.activation(out=a2[:, 0:W], in_=a2[:, 0:W], func=AF.Sqrt,
                                                        scale=-1.0, bias=1.0), reads=[r_a2], writes=[r_a2])
                    tr.op("pool", lambda e: e.tensor_tensor(out=bb[:, 0:W], in0=a2[:, 0:W], in1=ii[:, 0:W],
                                                            op=ALU.mult), reads=[r_a2, r_ii], writes=[r_bb])
                    tr.op("pool", lambda e: e.tensor_tensor(out=bb[:, 0:W], in0=bb[:, 0:W], in1=cv[:, 0:W],
                                                            op=ALU.mult), reads=[r_bb, r_cv], writes=[r_bb])
                    if resets is not None:
                        tr.op("dve", lambda e: e.tensor_tensor(
                            out=tcol[:, ci:ci + 1], in0=ii[:, 0:1], in1=cv[:, 0:1], op=ALU.mult),
                            reads=[r_ii, r_cv], writes=[r_tcol])
                        if resets == "hard":
                            tr.op("dve", lambda e: e.memset(aa[:, 0:1], 0.0), reads=[r_aa], writes=[r_aa])
                            tr.op("dve", lambda e: e.tensor_copy(out=bb[:, 0:1], in_=tcol[:, ci:ci + 1]),
                                  reads=[r_tcol, r_bb], writes=[r_bb])
                        else:
                            tr.op("dve", lambda e: e.tensor_scalar(
                                out=aa[:, 0:1], in0=aa[:, 0:1], scalar1=FL[:, 0:1], scalar2=None, op0=ALU.mult),
                                reads=[r_aa, r_FL], writes=[r_aa])
                            tr.op("dve", lambda e: e.tensor_scalar(
                                out=tcol[:, ci:ci + 1], in0=tcol[:, ci:ci + 1], scalar1=FL[:, 1:2], scalar2=None,
                                op0=ALU.mult), reads=[r_tcol, r_FL], writes=[r_tcol])
                            tr.op("dve", lambda e: e.scalar_tensor_tensor(
                                out=bb[:, 0:1], in0=bb[:, 0:1], scalar=FL[:, 0:1], in1=tcol[:, ci:ci + 1],
                                op0=ALU.mult, op1=ALU.add), reads=[r_bb, r_FL, r_tcol], writes=[r_bb])
                    for s in range(nseg):
                        init_ap, init_res = init_fn(s)
                        tr.op("dve", lambda e, s=s, init_ap=init_ap: e.tensor_tensor_scan(
                            out=hh[:, s * L:(s + 1) * L], data0=aa[:, s * L:(s + 1) * L],
                            data1=bb[:, s * L:(s + 1) * L], initial=init_ap, op0=ALU.mult, op1=ALU.add),
                            reads=[r_aa, r_bb] + init_res, writes=[r_hh])
                    if own:
                        for kc in range(8):
                            tr.op("pe", lambda e, kc=kc: e.matmul(
                                pb[4][:, 0:W], lhsT=WIN[:, kc, 2048 + ci * P:2048 + (ci + 1) * P],
                                rhs=HT[:, kc, 0:W], start=(kc == 0), stop=(kc == 7)),
                                reads=r_HT + [r_WIN], writes=[r_pb[4]])
                        tr.op("act", lambda e: e.activation(out=gsb[:, 0:W], in_=pb[4][:, 0:W], func=AF.Copy),
                              reads=[r_pb[4]], writes=[r_gsb])
                        tr.op("pool", lambda e: e.tensor_tensor(out=uu[:, 0:W], in0=gsb[:, 0:W], in1=gsb[:, 0:W],
                                                                op=ALU.mult), reads=[r_gsb], writes=[r_uu])
                        tr.op("pool", lambda e: e.tensor_scalar(out=uu[:, 0:W], in0=uu[:, 0:W], scalar1=0.044715,
                                                                scalar2=1.0, op0=ALU.mult, op1=ALU.add),
                              reads=[r_uu], writes=[r_uu])
                        tr.op("pool", lambda e: e.tensor_tensor(out=uu[:, 0:W], in0=uu[:, 0:W], in1=gsb[:, 0:W],
                                                                op=ALU.mult), reads=[r_uu, r_gsb], writes=[r_uu])
                        tr.op("act", lambda e: e.activation(out=uu[:, 0:W], in_=uu[:, 0:W], func=AF.Sigmoid,
                                                            scale=1.5957691216057308),
                              reads=[r_uu], writes=[r_uu])
                        tr.op("pool", lambda e: e.tensor_tensor(out=uu[:, 0:W], in0=uu[:, 0:W], in1=gsb[:, 0:W],
                                                                op=ALU.mult), reads=[r_uu, r_gsb], writes=[r_uu])
                        tr.op("dve", lambda e: e.tensor_tensor(out=YY[:, ci, 0:W], in0=hh[:, 0:W], in1=uu[:, 0:W],
                                                               op=ALU.mult),
                              reads=[r_hh, r_uu], writes=[r_YY[ci]])

                def lru_norm(W, ydst, r_ydst):
                    for ci in range(4):
                        tr.op("act", lambda e, ci=ci: e.activation(out=YQ[:, 0:W], in_=YY[:, ci, 0:W],
                                                                   func=AF.Square),
                              reads=[r_YY[ci]], writes=[r_YQ])
                        tr.op("pe", lambda e, ci=ci: e.matmul(pb[5][:, 0:W], lhsT=ones_f[:], rhs=YQ[:, 0:W],
                                                              start=(ci == 0), stop=(ci == 3)),
                              reads=[r_ones, r_YQ], writes=[r_pb[5]])
                    tr.op("act", lambda e: e.activation(out=gsb[:, 0:W], in_=pb[5][:, 0:W], func=AF.Sqrt,
                                                        scale=1.0 / 512.0, bias=EPS),
                          reads=[r_pb[5]], writes=[r_gsb])
                    tr.op("dve", lambda e: e.reciprocal(out=gsb[:, 0:W], in_=gsb[:, 0:W]), reads=[r_gsb],
                          writes=[r_gsb])
                    for ci in range(4):
                        tr.op("dve", lambda e, ci=ci: e.scalar_tensor_tensor(
                            out=ydst(ci), in0=YY[:, ci, 0:W], scalar=LV[:, ci, 8:9], in1=gsb[:, 0:W],
                            op0=ALU.mult, op1=ALU.mult), reads=[r_YY[ci], r_LV, r_gsb], writes=[r_ydst])

                def lru_group(g):
                    own = g >= 4
                    for ci in range(4):
                        tr.op("pool", lambda e, ci=ci: e.tensor_copy(out=XPc[:, 0:3], in_=XH[:, ci, :]),
                              reads=[r_XH[ci]], writes=[r_XPc])
                        if g == 4:
                            tr.op("dve", lambda e: e.tensor_scalar(
                                out=XPc[:, 0:3], in0=XPc[:, 0:3], scalar1=FL[:, 0:1], scalar2=None,
                                op0=ALU.mult), reads=[r_XPc, r_FL], writes=[r_XPc])
                        for kc in range(8):
                            tr.op("pe", lambda e, kc=kc, ci=ci: e.matmul(
                                pb[3][:], lhsT=WIN[:, kc, 1536 + ci * P:1536 + (ci + 1) * P], rhs=HT[:, kc, :],
                                start=(kc == 0), stop=(kc == 7)), reads=r_HT + [r_WIN], writes=[r_pb[3]])
                        tr.op("act", lambda e: e.activation(out=XPc[:, 3:515], in_=pb[3][:], func=AF.Copy),
                              reads=[r_pb[3]], writes=[r_XPc])
                        tr.op("pool", lambda e, ci=ci: e.tensor_copy(out=XH[:, ci, :], in_=XPc[:, 512:515]),
                              reads=[r_XPc], writes=[r_XH[ci]])
                        resets = "hard" if g == 0 else ("soft" if g == 4 else None)
                        lru_core(ci, 512, 1, resets, None,
                                 lambda s, ci=ci: (HS[:, ci:ci + 1], [r_HS[ci]]), own, None)
                        tr.op("dve", lambda e, ci=ci: e.tensor_copy(out=HS[:, ci:ci + 1], in_=hh[:, 511:512]),
                              reads=[r_hh], writes=[r_HS[ci]])
                    if own:
                        c0 = (g - 4) * 512
                        lru_norm(512, lambda ci: YNA[:, ci, c0:c0 + 512], r_YNA[g - 4])

                for g in range(8):
                    stage_front(4 * g)
                    for j in range(4):
                        t = 4 * g + j
                        if j < 3:
                            stage_front(t + 1)
                        stage_a(t)
                        if t % 2 == 1:
                            kmean(t // 2)
                    lru_group(g)
                tr.op("pe", lambda e: e.transpose(out=pb[0][0:4, 0:P], in_=HS[:, 0:4], identity=idf[:]),
                      reads=r_HS + [r_idf], writes=[r_pb[0]])
                for ci in range(4):
                    tr.op("pe", lambda e, ci=ci: e.transpose(out=pb[1][0:3, ci * P:(ci + 1) * P],
                                                             in_=XH[:, ci, :], identity=idf[:]),
                          reads=[r_XH[ci], r_idf], writes=[r_pb[1]])
                tr.op("dve", lambda e: e.tensor_copy(out=qs[0:4, 0:P], in_=pb[0][0:4, 0:P]),
                      reads=[r_pb[0]], writes=[r_qs])
                tr.op("dve", lambda e: e.tensor_copy(out=ks[0:3, :], in_=pb[1][0:3, :]),
                      reads=[r_pb[1]], writes=[r_ks])
                r1 = Res(); r2 = Res()
                tr.dma("sp", o_h.rearrange("(c p) -> c p", p=P), qs[0:4, 0:P], reads=[r_qs], writes=[r1])
                tr.dma("sp", o_c[:, :], ks[0:3, :], reads=[r_ks], writes=[r2])
                out_res.extend([r1, r2])

                load_bc([(bS1, r_bS1, 0, None), (bG1, r_bG1, 1, nmg)], SROWS, (xt[1], r_xt[1]))
                xs_t = xt[0]; rxs = r_xt[0]
                tr.dma("sp", xs_t[0:NS, :], xs[:, :], writes=[rxs])
                rps = rp[0]; rrps = r_rp[0]
                for s in range(4):
                    tr.dma("sp", rps[8 * s:8 * s + 8, :], ropes[:, :], writes=[rrps])
                norm_to_HT(xs_t, rxs, NS, 0)
                proj_tok(NS, 0, [(1, 512), (2, 1024), (0, 0)])
                tr.op("act", lambda e: e.activation(out=ks[0:NS, :], in_=pb[1][0:NS, :], func=AF.Copy),
                      reads=[r_pb[1]], writes=[r_ks])
                tr.op("act", lambda e: e.activation(out=vs[0:NS, :], in_=pb[2][0:NS, :], func=AF.Copy),
                      reads=[r_pb[2]], writes=[r_vs])
                tr.op("act", lambda e: e.activation(out=qs[0:NS, :], in_=pb[0][0:NS, :], func=AF.Copy, scale=0.125),
                      reads=[r_pb[0]], writes=[r_qs])
                tr.op("pool", lambda e: e.tensor_copy(out=VNb[0:NS, :], in_=vs[0:NS, :]),
                      reads=[r_vs, r_VNb], writes=[r_VNb])
                rope_apply(ks, r_ks, kr, r_kr, rps, rrps, n=NS)
                r1 = Res(); r2 = Res()
                tr.dma("sp", o_ks[:, :], kr[0:NS, :], reads=[r_kr], writes=[r1])
                tr.dma("sp", o_vs[:, :], vs[0:NS, :], reads=[r_vs], writes=[r2])
                out_res.extend([r1, r2])
                tr.op("pool", lambda e: e.tensor_copy(out=krb[0:NS, :], in_=kr[0:NS, :]), reads=[r_kr],
                      writes=[r_krb])
                transpose4(krb, r_krb, NS, KTn[:, :, :], r_KTn)
                rope_apply(qs, r_qs, qr, r_qr, rps, rrps, n=NS)
                tr.op("pool", lambda e: e.tensor_copy(out=krb[0:NS, :], in_=qr[0:NS, :]), reads=[r_qr],
                      writes=[r_krb])
                transpose4(krb, r_krb, NS, QTs[:, :, :], r_QTs)
                STH = sb(es, "STH", [P, 4, 4]); r_STH = Res()
                STC = sb(es, "STC", [P, 4, 4, 3]); r_STC = Res()
                HSs = sb(es, "HSs", [P, 4, 4]); r_HSs = Res()
                CSs = sb(es, "CSs", [P, 4, 12]); r_CSs = Res()
                tr.dma("sp", STH[:], sth[:, :, :], writes=[r_STH])
                tr.dma("sp", STC[:], stc[:, :, :, :], writes=[r_STC])
                for ci in range(4):
                    xv = XPc[:, 0:44].rearrange("p (s c) -> p s c", s=4)
                    tr.op("pool", lambda e, ci=ci: e.tensor_copy(out=xv[:, :, 0:3], in_=STC[:, ci, :, :]),
                          reads=[r_STC], writes=[r_XPc])
                    for kc in range(8):
                        tr.op("pe", lambda e, kc=kc, ci=ci: e.matmul(
                            pb[3][:, 0:NS], lhsT=WIN[:, kc, 1536 + ci * P:1536 + (ci + 1) * P], rhs=HT[:, kc, 0:NS],
                            start=(kc == 0), stop=(kc == 7)), reads=r_HT + [r_WIN], writes=[r_pb[3]])
                    tr.op("act", lambda e: e.activation(
                        out=xv[:, :, 3:11], in_=pb[3][:, 0:NS].rearrange("p (s c) -> p s c", s=4), func=AF.Copy),
                        reads=[r_pb[3]], writes=[r_XPc])
                    tr.op("pool", lambda e, ci=ci: e.tensor_copy(
                        out=CSs[:, ci, :].rearrange("p (s c) -> p s c", s=4), in_=xv[:, :, 8:11]),
                        reads=[r_XPc], writes=[r_CSs])
                    lru_core(ci, NS, 4, None, None,
                             lambda s, ci=ci: (STH[:, ci, s:s + 1], [r_STH]), True, None)
                    tr.op("dve", lambda e, ci=ci: e.tensor_copy(
                        out=HSs[:, ci, :].unsqueeze(2), in_=hh[:, 0:NS].rearrange("p (s c) -> p s c", s=4)[:, :, 7:8]),
                        reads=[r_hh], writes=[r_HSs])
                lru_norm(NS, lambda ci: YNs[:, ci, :], r_YNs)
                for ci in range(4):
                    tr.op("pe", lambda e, ci=ci: e.transpose(out=pb[0][0:4, ci * P:(ci + 1) * P],
                                                             in_=HSs[:, ci, :], identity=idf[:]),
                          reads=[r_HSs, r_idf], writes=[r_pb[0]])
                    tr.op("pe", lambda e, ci=ci: e.transpose(out=pb[1][0:12, ci * P:(ci + 1) * P],
                                                             in_=CSs[:, ci, :], identity=idf[:]),
                          reads=[r_CSs, r_idf], writes=[r_pb[1]])
                tr.op("dve", lambda e: e.tensor_copy(out=qs[0:4, :], in_=pb[0][0:4, :]),
                      reads=[r_pb[0]], writes=[r_qs])
                tr.op("dve", lambda e: e.tensor_copy(out=ks[0:12, :], in_=pb[1][0:12, :]),
                      reads=[r_pb[1]], writes=[r_ks])
                r1 = Res(); r2 = Res()
                tr.dma("sp", o_hs[:, :], qs[0:4, :], reads=[r_qs], writes=[r1])
                tr.dma("sp", o_cs[:, :], ks[0:12, :], reads=[r_ks], writes=[r2])
                out_res.extend([r1, r2])
            tr.barrier()
            def p2_common(es, sfx, rows):
                bGM = sb(es, "bGM" + sfx, [P, D]); r_bGM = Res()
                bS2 = sb(es, "bS2" + sfx, [P, D]); r_bS2 = Res()
                bG2 = sb(es, "bG2" + sfx, [P, D]); r_bG2 = Res()
                xr = sb(es, "xr" + sfx, [P, D]); r_xr = Res()
                xm = sb(es, "xm" + sfx, [P, D]); r_xm = Res()
                tmpf = sb(es, "tmpf2" + sfx, [P, D]); r_tmpf = Res()
                hb2 = sb(es, "hb2" + sfx, [P, D], BF16); r_hb2 = Res()
                h2s = sb(es, "h2s" + sfx, [P, 8, P], BF16); r_h2s = Res()
                WOUT = sb(es, "WOUT" + sfx, [P, 8, D], BF16); r_WOUT = Res()
                woutv = w_out.rearrange("(kc p) n -> p kc n", p=P)
                for cc in range(2):
                    tr.dma("pool", WOUT[:, :, cc * 512:(cc + 1) * 512], woutv[:, :, cc * 512:(cc + 1) * 512],
                           writes=[r_WOUT])
                RWB = sb(es, "RWB" + sfx, [P, 8, NE], BF16); r_RWB = Res()
                tr.dma("pool", RWB[:], router_w.rearrange("(kc p) n -> p kc n", p=P), writes=[r_RWB])
                RB = sb(es, "RB" + sfx, [P, NE]); r_RB = Res()
                tr.dma("sp", RB[:], router_b.partition_broadcast(P), writes=[r_RB])
                AOG = sb(es, "AOG" + sfx, [P, 512]); r_AOG = Res()
                tr.dma("sp", AOG[:], aog.partition_broadcast(P), writes=[r_AOG])
                AT = sb(es, "AT" + sfx, [P, 512]); r_AT = Res()
                atb = sb(es, "atb" + sfx, [P, 512], BF16); r_atb = Res()
                sc = sb(es, "sc" + sfx, [P, NE]); r_sc2 = Res()
                bi = sb(es, "bi" + sfx, [P, NE]); r_bi = Res()
                m8g = sb(es, "m8g" + sfx, [P, 8, 8]); r_m8g = Res()
                gsr = sb(es, "gsr" + sfx, [P, 8]); r_gsr = Res()
                gm = sb(es, "gm" + sfx, [P, 8]); r_gm = Res()
                msk = sb(es, "msk" + sfx, [P, NE]); r_msk = Res()
                em = sb(es, "em" + sfx, [P, NE]); r_em = Res()
                wv = sb(es, "wv" + sfx, [P, NE]); r_wv = Res()
                st2 = sb(es, "st2" + sfx, [P, 8]); r_st2 = Res()
                load_bc([(bGM, r_bGM, 2, None), (bS2, r_bS2, 3, None), (bG2, r_bG2, 4, nfg)], rows, (xr, r_xr))

                def attn_finish(o3, rc2, n, rds, dst, r_dst):
                    R = slice(0, n)
                    tr.op("dve", lambda e: e.tensor_tensor(
                        out=AT[R, :].rearrange("p (h d) -> p h d", h=NH), in0=o3,
                        in1=rc2.unsqueeze(2).broadcast_to([n, NH, 64]), op=ALU.mult),
                        reads=rds, writes=[r_AT])
                    tr.op("act", lambda e: e.activation(out=hb2[R, 0:512], in_=AT[R, :], func=AF.Square,
                                                        accum_out=st2[R, 0:1]),
                          reads=[r_AT], writes=[r_hb2, r_st2])
                    tr.op("act", lambda e: e.activation(out=st2[R, 1:2], in_=st2[R, 0:1], func=AF.Sqrt,
                                                        scale=1.0 / 512.0, bias=EPS),
                          reads=[r_st2], writes=[r_st2])
                    tr.op("dve", lambda e: e.reciprocal(out=st2[R, 2:3], in_=st2[R, 1:2]),
                          reads=[r_st2], writes=[r_st2])
                    tr.op("dve", lambda e: e.scalar_tensor_tensor(
                        out=atb[R, :], in0=AT[R, :], scalar=st2[R, 2:3], in1=AOG[R, :], op0=ALU.mult,
                        op1=ALU.mult), reads=[r_AT, r_st2, r_AOG], writes=[r_atb])
                    for pr in range(4):
                        tr.op("pe", lambda e, pr=pr: e.transpose(out=pbT[:, pr * P:pr * P + n],
                                                                 in_=atb[R, pr * P:(pr + 1) * P],
                                                                 identity=idb[R, R]),
                              reads=[r_atb, r_idb], writes=[r_pbT])
                    tr.op("dve", lambda e: e.tensor_copy(
                        out=dst, in_=pbT[:, 0:512].rearrange("p (k c) -> p k c", k=4)[:, :, 0:n]),
                        reads=[r_pbT], writes=[r_dst])

                def ffn_pre(src, r_src, n, to):
                    R = slice(0, n)
                    c0 = to * P
                    tr.op("act", lambda e: e.activation(out=hb2[R, :], in_=src[R, :], func=AF.Square,
                                                        accum_out=st2[R, 3:4]),
                          reads=[r_src], writes=[r_hb2, r_st2])
                    tr.op("act", lambda e: e.activation(out=st2[R, 4:5], in_=st2[R, 3:4], func=AF.Sqrt,
                                                        scale=1.0 / D, bias=EPS), reads=[r_st2], writes=[r_st2])
                    tr.op("dve", lambda e: e.reciprocal(out=st2[R, 5:6], in_=st2[R, 4:5]),
                          reads=[r_st2], writes=[r_st2])
                    tr.op("dve", lambda e: e.scalar_tensor_tensor(
                        out=tmpf[R, :], in0=src[R, :], scalar=st2[R, 5:6], in1=bG2[R, :],
                        op0=ALU.mult, op1=ALU.mult), reads=[r_src, r_st2, r_bG2], writes=[r_tmpf])
                    tr.op("pool", lambda e: e.tensor_tensor(out=hb2[R, :], in0=tmpf[R, :], in1=bS2[R, :],
                                                            op=ALU.add),
                          reads=[r_tmpf, r_bS2], writes=[r_hb2])
                    for kc in range(8):
                        tr.op("pe", lambda e, kc=kc: e.transpose(out=pbT[:, kc * P:kc * P + n],
                                                                 in_=hb2[R, kc * P:(kc + 1) * P],
                                                                 identity=idb[R, R]),
                              reads=[r_hb2, r_idb], writes=[r_pbT])
                    tr.op("act", lambda e: e.activation(
                        out=h2s[:, :, 0:n], in_=pbT[:].rearrange("p (k c) -> p k c", k=8)[:, :, 0:n],
                        func=AF.Copy), reads=[r_pbT], writes=[r_h2s])
                    tr.dma("sp", h2d[:, :, c0:c0 + n], h2s[:, :, 0:n], reads=[r_h2s], writes=[r_h2d[to]])
                    for kc in range(8):
                        tr.op("pe", lambda e, kc=kc: e.matmul(
                            pb[0][R, 0:NE], lhsT=h2s[:, kc, 0:n], rhs=RWB[:, kc, :],
                            start=(kc == 0), stop=(kc == 7)), reads=[r_h2s, r_RWB], writes=[r_pb[0]])
                    tr.op("act", lambda e: e.activation(out=sc[R, :], in_=pb[0][R, 0:NE], func=AF.Sigmoid),
                          reads=[r_pb[0]], writes=[r_sc2])
                    tr.op("dve", lambda e: e.tensor_tensor(out=bi[R, :], in0=sc[R, :], in1=RB[R, :], op=ALU.add),
                          reads=[r_sc2, r_RB], writes=[r_bi])
                    for g in range(8):
                        tr.op("dve", lambda e, g=g: e.max(out=m8g[R, g, :], in_=bi[R, g * 8:(g + 1) * 8]),
                              reads=[r_bi], writes=[r_m8g])
                    tr.op("dve", lambda e: e.tensor_tensor(out=gsr[R, :].unsqueeze(2), in0=m8g[R, :, 0:1],
                                                           in1=m8g[R, :, 1:2], op=ALU.add),
                          reads=[r_m8g], writes=[r_gsr])
                    tr.op("dve", lambda e: e.max(out=m8g[R, 0, :], in_=gsr[R, :]), reads=[r_gsr, r_m8g],
                          writes=[r_m8g])
                    tr.op("dve", lambda e: e.tensor_scalar(out=gm[R, :], in0=gsr[R, :], scalar1=m8g[R, 0, 3:4],
                                                           scalar2=None, op0=ALU.is_ge),
                          reads=[r_gsr, r_m8g], writes=[r_gm])
                    tr.op("dve", lambda e: e.scalar_tensor_tensor(
                        out=msk[R, :].rearrange("p (g k) -> p g k", g=8),
                        in0=bi[R, :].rearrange("p (g k) -> p g k", g=8), scalar=2.0,
                        in1=gm[R, :].unsqueeze(2).broadcast_to([n, 8, 8]), op0=ALU.add, op1=ALU.mult),
                        reads=[r_bi, r_gm], writes=[r_msk])
                    tr.op("dve", lambda e: e.max(out=m8g[R, 1, :], in_=msk[R, :]), reads=[r_msk, r_m8g],
                          writes=[r_m8g])
                    tr.op("dve", lambda e: e.tensor_scalar(out=em[R, :], in0=msk[R, :], scalar1=m8g[R, 1, 7:8],
                                                           scalar2=None, op0=ALU.is_ge),
                          reads=[r_msk, r_m8g], writes=[r_em])
                    tr.op("dve", lambda e: e.tensor_tensor(out=wv[R, :], in0=sc[R, :], in1=em[R, :], op=ALU.mult),
                          reads=[r_sc2, r_em], writes=[r_wv])
                    tr.op("dve", lambda e: e.tensor_reduce(out=st2[R, 6:7], in_=wv[R, :], axis=AX.X, op=ALU.add),
                          reads=[r_wv], writes=[r_st2])
                    tr.op("dve", lambda e: e.reciprocal(out=st2[R, 7:8], in_=st2[R, 6:7]), reads=[r_st2],
                          writes=[r_st2])
                    tr.op("dve", lambda e: e.tensor_scalar(out=GT[R, to, :], in0=wv[R, :], scalar1=st2[R, 7:8],
                                                           scalar2=2.5, op0=ALU.mult, op1=ALU.mult),
                          reads=[r_wv, r_st2], writes=[r_GT[to]])

                def outproj(n, xsrc_dram, att_ap, r_att, yn_ap, r_yn, to):
                    R = slice(0, n)
                    tr.dma("sp", xr[R, :], xsrc_dram, writes=[r_xr])
                    for hf in range(2):
                        bk = 1 + hf
                        for kc in range(8):
                            lhs = att_ap(kc) if kc < 4 else yn_ap(kc - 4)
                            rl = r_att if kc < 4 else r_yn
                            tr.op("pe", lambda e, kc=kc, lhs=lhs, bk=bk, hf=hf: e.matmul(
                                pb[bk][R, :], lhsT=lhs, rhs=WOUT[:, kc, hf * 512:(hf + 1) * 512],
                                start=(kc == 0), stop=(kc == 7)), reads=[rl, r_WOUT], writes=[r_pb[bk]])
                        tr.op("dve", lambda e, bk=bk, hf=hf: e.tensor_tensor(
                            out=tmpf[R, hf * 512:(hf + 1) * 512], in0=pb[bk][R, :],
                            in1=bGM[R, hf * 512:(hf + 1) * 512], op=ALU.mult),
                            reads=[r_pb[bk], r_bGM], writes=[r_tmpf])
                    tr.op("pool", lambda e: e.tensor_tensor(out=xm[R, :], in0=tmpf[R, :], in1=xr[R, :], op=ALU.add),
                          reads=[r_tmpf, r_xr], writes=[r_xm])
                    tr.dma("sp", xmid[to * P:to * P + n, :], xm[R, :], reads=[r_xm], writes=[r_xmid[to]])
                    ffn_pre(xm, r_xm, n, to)

                import types
                return types.SimpleNamespace(attn_finish=attn_finish, ffn_pre=ffn_pre, outproj=outproj,
                                             AOG=AOG, r_AOG=r_AOG, AT=AT, r_AT=r_AT, st2=st2, r_st2=r_st2,
                                             hb2=hb2, r_hb2=r_hb2, atb=atb, r_atb=r_atb)

            with contextlib.ExitStack() as es:
                C = p2_common(es, "p", [(0, 0, P)])
                BL = sb(es, "BL", [P, 8, 3, 16]); r_BL = Res()
                tr.dma("sp", BL[:].rearrange("p a b c -> p (a b c)"), blkc.partition_broadcast(P), writes=[r_BL])
                TRI = sb(es, "TRI", [P, P], BF16); r_TRI = Res()
                tr.dma("pool", TRI[:], trim[:, :], writes=[r_TRI])
                OH = sb(es, "OH", [P, 16 * P], BF16); r_OH = Res()
                tr.op("pool", lambda e: e.memset(OH[:], 0.0), writes=[r_OH])
                tr.dma("pool", OH[0:16, :], ohm[:, :], reads=[r_OH], writes=[r_OH])
                QTz = sb(es, "QTz", [P, NH, 256], BF16); r_QTz = Res()
                tr.op("pool", lambda e: e.memset(QTz[:], 0.0), writes=[r_QTz])
                gbs = sb(es, "gbs", [P, NH, 16]); r_gbs = Res()
                m8 = sb(es, "m8", [P, NH, 8]); r_m8 = Res()
                mbf = sb(es, "mbf", [P, NH, 16]); r_mbf = Res()
                mbb = sb(es, "mbb", [P, NH, 16], BF16); r_mbb = Res()
                MBT = sb(es, "MBT", [P, NH, 256], BF16); r_MBT = Res()
                tr.op("pool", lambda e: e.memset(MBT[:], 0.0), writes=[r_MBT])
                PT = [sb(es, f"PT{i}", [P, 256], BF16) for i in range(4)]; r_PT = [Res() for _ in range(4)]
                OA = sb(es, "OA", [P, 2, NH, 65]); r_OA = Res()
                rc = sb(es, "rc", [P, 2, NH]); r_rc = Res()
                ATT = sb(es, "ATT", [P, 4, 256], BF16); r_ATT = [Res(), Res()]
                pcnt = [0]

                def attention(jb):
                    t0 = 16 + 2 * jb
                    qoff = jb * 256
                    rq = [r_QTz]
                    for h in range(NH):
                        pr, p0 = h // 2, 64 * (h % 2)
                        tr.op("pool", lambda e, h=h, pr=pr, p0=p0: e.tensor_copy(
                            out=QTz[p0:p0 + 64, h, :], in_=QTA[p0:p0 + 64, pr, qoff:qoff + 256]),
                            reads=[r_QTA[2 * jb], r_QTA[2 * jb + 1], r_QTz], writes=[r_QTz])
                    for i in range(2):
                        bk = 3 + i
                        for h in range(NH):
                            pr, p0 = h // 2, 64 * (h % 2)
                            tr.op("pe", lambda e, h=h, pr=pr, bk=bk, i=i: e.matmul(
                                pb[bk][:, h * 16:(h + 1) * 16],
                                lhsT=QTz[:, h, i * P:(i + 1) * P],
                                rhs=KM[:, pr, :], start=True, stop=True),
                                reads=[r_QTz, r_KM], writes=[r_pb[bk]])
                        tr.op("dve", lambda e, bk=bk: e.tensor_tensor(
                            out=gbs[:], in0=pb[bk][:, 0:128].rearrange("p (h n) -> p h n", h=NH),
                            in1=BL[:, jb, 0, :].unsqueeze(1).broadcast_to([P, NH, 16]), op=ALU.add),
                            reads=[r_pb[bk], r_BL], writes=[r_gbs])
                        for h in range(NH):
                            tr.op("dve", lambda e, h=h: e.max(out=m8[:, h, :], in_=gbs[:, h, :]),
                                  reads=[r_gbs], writes=[r_m8])
                        for h in range(NH):
                            tr.op("dve", lambda e, h=h: e.tensor_scalar(
                                out=mbf[:, h, :], in0=gbs[:, h, :], scalar1=m8[:, h, 2:3], scalar2=NEG,
                                op0=ALU.is_lt, op1=ALU.mult), reads=[r_gbs, r_m8], writes=[r_mbf])
                        tr.op("dve", lambda e: e.tensor_tensor(
                            out=mbf[:], in0=mbf[:], in1=BL[:, jb, 1, :].unsqueeze(1).broadcast_to([P, NH, 16]),
                            op=ALU.mult), reads=[r_mbf, r_BL], writes=[r_mbf])
                        tr.op("dve", lambda e: e.tensor_tensor(
                            out=mbb[:], in0=mbf[:], in1=BL[:, jb, 2, :].unsqueeze(1).broadcast_to([P, NH, 16]),
                            op=ALU.add), reads=[r_mbf, r_BL], writes=[r_mbb])
                        for h in range(NH):
                            tr.op("pe", lambda e, h=h: e.transpose(out=pbT[0:16, h * P:(h + 1) * P],
                                                                   in_=mbb[:, h, :], identity=idb[:]),
                                  reads=[r_mbb, r_idb], writes=[r_pbT])
                        tr.op("dve", lambda e, i=i: e.tensor_copy(
                            out=MBT[0:16, :, i * P:(i + 1) * P],
                            in_=pbT[0:16, :].rearrange("p (h c) -> p h c", h=NH)),
                            reads=[r_pbT, r_MBT], writes=[r_MBT])
                    nkt = t0 + 2
                    if "L1" in stages:
                        return
                    pend = []
                    for h in range(NH):
                        pr, p0 = h // 2, 64 * (h % 2)
                        reg = (h % 2) * 65
                        for kt in range(nkt):
                            c0 = 0 if kt <= t0 else P
                            bk = 2 + (pcnt[0] % 3)
                            pt = PT[pcnt[0] % 4]; rpt_ = r_PT[pcnt[0] % 4]
                            pcnt[0] += 1
                            diag = kt >= t0
                            nb = kt // 2
                            tr.op("pe", lambda e, kt=kt, c0=c0, bk=bk, pr=pr, h=h: e.matmul(
                                pb[bk][:, c0:256], lhsT=KT[:, pr, kt * P:(kt + 1) * P],
                                rhs=QTz[:, h, c0:256], start=True, stop=False),
                                reads=[r_KT[kt]] + rq, writes=[r_pb[bk]])
                            tr.op("pe", lambda e, nb=nb, c0=c0, bk=bk, h=h, diag=diag: e.matmul(
                                pb[bk][:, c0:256], lhsT=OH[:, nb * P:(nb + 1) * P],
                                rhs=MBT[:, h, c0:256], start=False, stop=(not diag)),
                                reads=[r_OH, r_MBT], writes=[r_pb[bk]])
                            if diag:
                                dc = (kt - t0) * P
                                tr.op("pe", lambda e, bk=bk, dc=dc: e.matmul(
                                    pb[bk][:, dc:dc + P], lhsT=idb[:], rhs=TRI[:], start=False, stop=True),
                                    reads=[r_idb, r_TRI], writes=[r_pb[bk]])
                            tr.op("act", lambda e, bk=bk, c0=c0, pt=pt: e.activation(
                                out=pt[:, c0:256], in_=pb[bk][:, c0:256], func=AF.Exp),
                                reads=[r_pb[bk]], writes=[rpt_])

                            def emit_pv(kt=kt, pt=pt, rpt_=rpt_, h=h, reg=reg, last=(kt == nkt - 1)):
                                for i in range(2):
                                    if kt > t0 + i:
                                        continue
                                    tr.op("pe", lambda e, i=i: e.matmul(
                                        pb[5 + i][:, reg:reg + 65], lhsT=pt[:, i * P:(i + 1) * P],
                                        rhs=VA[:, kt, h, :], start=(kt == 0), stop=(kt == t0 + i)),
                                        reads=[rpt_, r_VA[kt]], writes=[r_pb[5 + i]])
                                if last:
                                    for i in range(2):
                                        tr.op("act", lambda e, i=i: e.activation(
                                            out=OA[:, i, h, :], in_=pb[5 + i][:, reg:reg + 65], func=AF.Copy),
                                            reads=[r_pb[5 + i]], writes=[r_OA])
                            pend.append(emit_pv)
                            if len(pend) > 2:
                                pend.pop(0)()
                    while pend:
                        pend.pop(0)()
                    if "L2" in stages:
                        return
                    tr.op("dve", lambda e: e.reciprocal(out=rc[:], in_=OA[:, :, :, 64]), reads=[r_OA],
                          writes=[r_rc])
                    for i in range(2):
                        C.attn_finish(OA[:, i, :, 0:64], rc[:, i, :], P, [r_OA, r_rc],
                                    ATT[:, :, i * P:(i + 1) * P], r_ATT[i])

                if DO_ATT:
                    for jb in range(8):
                        attention(jb)
                        if "L1" in stages or "L2" in stages or "L3" in stages:
                            continue
                        for i in range(2):
                            t = 16 + 2 * jb + i
                            to = t - NCT
                            C.outproj(P, xp[t * P:(t + 1) * P, :],
                                    lambda kc, i=i: ATT[:, kc, i * P:(i + 1) * P], r_ATT[i],
                                    lambda kc, to=to: YNA[:, kc, to * P:(to + 1) * P], r_YNA[to // 4], to)
        tr.barrier()
        if DO_SMP:
            with contextlib.ExitStack() as es:
                C = p2_common(es, "s", SROWS)
                KTs = sb(es, "KTs", [P, 4, 64 * P], BF16); r_KTs = Res()
                PTs = sb(es, "PTs", [P, 64, 64], BF16); r_PTs = [Res() for _ in range(8)]
                KP = [sb(es, f"KP{i}", [P, 512]) for i in range(4)]; r_KP = [Res() for _ in range(4)]
                VP = [sb(es, f"VP{i}", [P, 512]) for i in range(4)]; r_VP = [Res() for _ in range(4)]
                VPb = [sb(es, f"VPb{i}", [P, 512], BF16) for i in range(4)]; r_VPb = [Res() for _ in range(4)]
                KS = sb(es, "KS", [P, 4, 64]); r_KS = Res()
                KSt = sb(es, "KSt", [P, 4, 32]); r_KSt = Res()
                KMs = sb(es, "KMs", [P, 4, 32], BF16); r_KMs = Res()
                QZ = sb(es, "QZ", [P, 4, 64], BF16); r_QZ = Res()
                OHS = sb(es, "OHS", [P, 32 * P], BF16); r_OHS = Res()
                tr.op("pool", lambda e: e.memset(OHS[:], 0.0), writes=[r_OHS])
                for hh_ in range(2):
                    tr.dma("pool", OHS[0:32, hh_ * 2048:(hh_ + 1) * 2048], ohs[:, hh_ * 2048:(hh_ + 1) * 2048],
                           reads=[r_OHS], writes=[r_OHS])
                TRSz = sb(es, "TRSz", [P, 4, 64], BF16); r_TRSz = Res()
                tr.op("pool", lambda e: e.memset(TRSz[:], 0.0), writes=[r_TRSz])
                tr.dma("pool", TRSz[0:32, :, :].rearrange("p a b -> p (a b)"), trs[:, :], reads=[r_TRSz],
                       writes=[r_TRSz])
                SEL = sb(es, "SEL", [P, 64]); r_SEL = Res()
                tr.dma("sp", SEL[:], selq[:, :], writes=[r_SEL])
                DM = sb(es, "DM", [64, 512]); r_DM = Res()
                tr.dma("sp", DM[:], dmask[:, :], writes=[r_DM])
                PAR = sb(es, "PAR", [64, 2]); r_PAR = Res()
                tr.dma("sp", PAR[:], par[:, :], writes=[r_PAR])
                AOG2 = sb(es, "AOG2", [64, 64]); r_AOG2 = Res()
                for h in range(NH):
                    tr.dma("sp", AOG2[h * 8:(h + 1) * 8, :], aog[h * 64:(h + 1) * 64].partition_broadcast(8),
                           writes=[r_AOG2])
                IOT = sb(es, "IOT", [P, 1]); r_IOT = Res()
                tr.dma("sp", IOT[:], iot[:, :], writes=[r_IOT])
                PTI = sb(es, "PTI", [P, 64], I32); r_PTI = Res()
                PTF = sb(es, "PTF", [P, 64]); r_PTF = Res()
                IDX = sb(es, "IDX", [P, 64], I32); r_IDX = Res()
                gbS = sb(es, "gbS", [64, 32]); r_gbS = Res()
                m8S = sb(es, "m8S", [64, 8]); r_m8S = Res()
                mbS = sb(es, "mbS", [P, 32], BF16); r_mbS = Res()
                tr.op("pool", lambda e: e.memset(mbS[:], 0.0), writes=[r_mbS])
                MBTs = sb(es, "MBTs", [P, 64], BF16); r_MBTs = Res()
                tr.op("pool", lambda e: e.memset(MBTs[:], 0.0), writes=[r_MBTs])
                PTn = sb(es, "PTn", [P, 64], BF16); r_PTn = Res()
                tr.op("pool", lambda e: e.memset(PTn[:], 0.0), writes=[r_PTn])
                ones_b = sb(es, "ones_b", [P, 2], BF16); r_onesb = Res()
                tr.op("pool", lambda e: e.memset(ones_b[:], 1.0), writes=[r_onesb])
                O2 = sb(es, "O2", [64, 512]); r_O2 = Res()
                AT2 = sb(es, "AT2", [64, 64]); r_AT2 = Res()
                FU = sb(es, "FU", [64, 64]); r_FU = Res()
                rs = sb(es, "rs", [P, 8]); r_rs = Res()
                tr.op("pool", lambda e: e.memset(rs[:], 0.0), writes=[r_rs])
                TP = sb(es, "TP", [P, P], BF16); r_TP = Res()
                tr.op("pool", lambda e: e.memset(TP[:], 0.0), writes=[r_TP])
                ATTs = sb(es, "ATTs", [P, 4, NS], BF16); r_ATTs = Res()

                for s in range(4):
                    if "S00" in stages:
                        continue
                    tr.dma("sp", PTI[:], ptab[s, :].partition_broadcast(P), writes=[r_PTI])
                    tr.op("dve", lambda e: e.tensor_copy(out=PTF[:], in_=PTI[:]), reads=[r_PTI], writes=[r_PTF])
                    tr.op("dve", lambda e: e.tensor_scalar(out=PTF[:], in0=PTF[:], scalar1=128.0,
                                                           scalar2=IOT[:, 0:1], op0=ALU.mult, op1=ALU.add),
                          reads=[r_PTF, r_IOT], writes=[r_PTF])
                    tr.op("dve", lambda e: e.tensor_copy(out=IDX[:], in_=PTF[:]), reads=[r_PTF], writes=[r_IDX])
                    tr.op("pool", lambda e: e.memset(QZ[:], 0.0), writes=[r_QZ])
                    for h in range(NH):
                        pr, p0 = h // 2, 64 * (h % 2)
                        tr.op("pool", lambda e, h=h, pr=pr, p0=p0, s=s: e.tensor_copy(
                            out=QZ[p0:p0 + 64, pr, h * 8:(h + 1) * 8], in_=QTs[p0:p0 + 64, pr, s * 8:(s + 1) * 8]),
                            reads=[r_QTs, r_QZ], writes=[r_QZ])
                    if "S0" in stages:
                        continue
                    for j in range(64):
                        kp = KP[j % 4]; rkp = r_KP[j % 4]
                        tr.gather(kp[:, :], cache_k[:, :], IDX[:, j:j + 1], reads=[r_IDX], writes=[rkp])
                        bk = j % 2
                        for pr in range(4):
                            tr.op("pe", lambda e, pr=pr, kp=kp, bk=bk: e.transpose(
                                out=pb[bk][:, pr * P:(pr + 1) * P], in_=kp[:, pr * P:(pr + 1) * P], identity=idf[:]),
                                reads=[rkp, r_idf], writes=[r_pb[bk]])
                        tr.op("act", lambda e, j=j, bk=bk: e.activation(
                            out=KTs[:, :, j * P:(j + 1) * P], in_=pb[bk][:].rearrange("p (k c) -> p k c", k=4),
                            func=AF.Copy), reads=[r_pb[bk]], writes=[r_KTs])
                        tr.op("dve", lambda e, j=j, bk=bk: e.tensor_reduce(
                            out=KS[:, :, j], in_=pb[bk][:].rearrange("p (k c) -> p k c", k=4), axis=AX.X,
                            op=ALU.add), reads=[r_pb[bk]], writes=[r_KS])
                    ks4 = KS[:].rearrange("p k (n t) -> p k n t", t=2)
                    tr.op("dve", lambda e: e.tensor_tensor(out=KSt[:].unsqueeze(3), in0=ks4[:, :, :, 0:1],
                                                           in1=ks4[:, :, :, 1:2], op=ALU.add),
                          reads=[r_KS], writes=[r_KSt])
                    tr.op("dve", lambda e: e.tensor_scalar(out=KMs[:], in0=KSt[:], scalar1=1.0 / 256.0,
                                                           scalar2=None, op0=ALU.mult),
                          reads=[r_KSt], writes=[r_KMs])
                    if "S1" in stages:
                        continue
                    for pr in range(4):
                        tr.op("pe", lambda e, pr=pr: e.matmul(pb[2][0:64, 0:32], lhsT=QZ[:, pr, :], rhs=KMs[:, pr, :],
                                                              start=(pr == 0), stop=(pr == 3)),
                              reads=[r_QZ, r_KMs], writes=[r_pb[2]])
                    tr.op("dve", lambda e: e.tensor_copy(out=gbS[:], in_=pb[2][0:64, 0:32]), reads=[r_pb[2]],
                          writes=[r_gbS])
                    tr.op("dve", lambda e: e.max(out=m8S[:], in_=gbS[:]), reads=[r_gbS], writes=[r_m8S])
                    tr.op("dve", lambda e: e.tensor_scalar(out=mbS[0:64, :], in0=gbS[:], scalar1=m8S[:, 2:3],
                                                           scalar2=NEG, op0=ALU.is_lt, op1=ALU.mult),
                          reads=[r_gbS, r_m8S, r_mbS], writes=[r_mbS])
                    tr.op("pe", lambda e: e.transpose(out=pbT[0:32, 0:P], in_=mbS[:, :], identity=idb[:]),
                          reads=[r_mbS, r_idb], writes=[r_pbT])
                    tr.op("dve", lambda e: e.tensor_copy(out=MBTs[0:32, :], in_=pbT[0:32, 0:64]),
                          reads=[r_pbT, r_MBTs], writes=[r_MBTs])
                    if "S2" in stages:
                        continue
                    for cidx in range(8):
                        bk = 3 + (cidx % 2)
                        for jj in range(8):
                            j = cidx * 8 + jj
                            reg = pb[bk][:, jj * 64:(jj + 1) * 64]
                            nb = j // 2
                            tr.op("pe", lambda e, reg=reg, nb=nb: e.matmul(
                                reg, lhsT=OHS[:, nb * P:(nb + 1) * P], rhs=MBTs[:, :], start=True, stop=False),
                                reads=[r_OHS, r_MBTs], writes=[r_pb[bk]])
                            for pr in range(4):
                                tr.op("pe", lambda e, reg=reg, pr=pr, j=j: e.matmul(
                                    reg, lhsT=KTs[:, pr, j * P:(j + 1) * P], rhs=QZ[:, pr, :],
                                    start=False, stop=(pr == 3)), reads=[r_KTs, r_QZ], writes=[r_pb[bk]])
                        tr.op("act", lambda e, cidx=cidx, bk=bk: e.activation(
                            out=PTs[:, cidx * 8:(cidx + 1) * 8, :],
                            in_=pb[bk][:].rearrange("p (a b) -> p a b", a=8), func=AF.Exp),
                            reads=[r_pb[bk]], writes=[r_PTs[cidx]])
                    tr.op("pe", lambda e, s=s: e.matmul(pb[2][0:32, 128:192], lhsT=idb[:, 0:32], rhs=TRSz[:, s, :],
                                                        start=True, stop=False),
                          reads=[r_idb, r_TRSz], writes=[r_pb[2]])
                    for pr in range(4):
                        tr.op("pe", lambda e, pr=pr: e.matmul(pb[2][0:32, 128:192], lhsT=KTn[:, pr, :], rhs=QZ[:, pr, :],
                                                              start=False, stop=(pr == 3)),
                              reads=[r_KTn, r_QZ], writes=[r_pb[2]])
                    tr.op("act", lambda e: e.activation(out=PTn[0:32, :], in_=pb[2][0:32, 128:192], func=AF.Exp),
                          reads=[r_pb[2], r_PTn], writes=[r_PTn])
                    if "S3" in stages:
                        continue
                    for j in range(64):
                        vp = VP[j % 4]; rvp = r_VP[j % 4]
                        vb = VPb[j % 4]; rvb = r_VPb[j % 4]
                        tr.gather(vp[:, :], cache_v[:, :], IDX[:, j:j + 1], reads=[r_IDX], writes=[rvp])
                        tr.op("act", lambda e, vp=vp, vb=vb: e.activation(out=vb[:], in_=vp[:], func=AF.Copy),
                              reads=[rvp], writes=[rvb])
                        tr.op("pe", lambda e, j=j, vb=vb: e.matmul(pb[5][0:64, :], lhsT=PTs[:, j, :], rhs=vb[:],
                                                                   start=(j == 0), stop=False),
                              reads=[r_PTs[j // 8], rvb], writes=[r_pb[5]])
                        tr.op("pe", lambda e, j=j: e.matmul(pb[6][0:64, 0:2], lhsT=PTs[:, j, :], rhs=ones_b[:, :],
                                                            start=(j == 0), stop=False),
                              reads=[r_PTs[j // 8], r_onesb], writes=[r_pb[6]])
                    tr.op("pe", lambda e: e.matmul(pb[5][0:64, :], lhsT=PTn[:, :], rhs=VNb[:, :],
                                                   start=False, stop=True),
                          reads=[r_PTn, r_VNb], writes=[r_pb[5]])
                    tr.op("pe", lambda e: e.matmul(pb[6][0:64, 0:2], lhsT=PTn[:, :], rhs=ones_b[:, :],
                                                   start=False, stop=True),
                          reads=[r_PTn, r_onesb], writes=[r_pb[6]])
                    if "S4" in stages:
                        continue
                    tr.op("act", lambda e: e.activation(out=O2[:], in_=pb[5][0:64, :], func=AF.Copy),
                          reads=[r_pb[5]], writes=[r_O2])
                    tr.op("dve", lambda e: e.reciprocal(out=rs[0:64, 0:1], in_=pb[6][0:64, 0:1]),
                          reads=[r_pb[6], r_rs], writes=[r_rs])
                    tr.op("dve", lambda e: e.tensor_tensor(out=O2[:], in0=O2[:], in1=DM[:], op=ALU.mult),
                          reads=[r_O2, r_DM], writes=[r_O2])
                    tr.op("dve", lambda e: e.tensor_reduce(
                        out=AT2[:], in_=O2[:].rearrange("p (h d) -> p d h", h=NH), axis=AX.X, op=ALU.add),
                        reads=[r_O2], writes=[r_AT2])
                    tr.op("dve", lambda e: e.tensor_scalar(out=AT2[:], in0=AT2[:], scalar1=rs[0:64, 0:1],
                                                           scalar2=None, op0=ALU.mult),
                          reads=[r_AT2, r_rs], writes=[r_AT2])
                    tr.op("act", lambda e: e.activation(out=FU[:], in_=AT2[:], func=AF.Square,
                                                        accum_out=rs[0:64, 1:2]),
                          reads=[r_AT2, r_rs], writes=[r_FU, r_rs])
                    tr.op("pe", lambda e: e.matmul(pb[2][0:64, 256:258], lhsT=SEL[:, :], rhs=rs[:, 1:3],
                                                   start=True, stop=True),
                          reads=[r_SEL, r_rs], writes=[r_pb[2]])
                    tr.op("act", lambda e: e.activation(out=rs[0:64, 3:4], in_=pb[2][0:64, 256:257], func=AF.Sqrt,
                                                        scale=1.0 / 512.0, bias=EPS),
                          reads=[r_pb[2], r_rs], writes=[r_rs])
                    tr.op("dve", lambda e: e.reciprocal(out=rs[0:64, 4:5], in_=rs[0:64, 3:4]), reads=[r_rs],
                          writes=[r_rs])
                    tr.op("dve", lambda e: e.scalar_tensor_tensor(
                        out=FU[:], in0=AT2[:], scalar=rs[0:64, 4:5], in1=AOG2[:], op0=ALU.mult, op1=ALU.mult),
                        reads=[r_AT2, r_rs, r_AOG2], writes=[r_FU])
                    for pp in range(2):
                        tr.op("dve", lambda e, pp=pp: e.tensor_scalar(
                            out=TP[0:64, pp * 64:(pp + 1) * 64], in0=FU[:], scalar1=PAR[:, pp:pp + 1],
                            scalar2=None, op0=ALU.mult), reads=[r_FU, r_PAR, r_TP], writes=[r_TP])
                    tr.op("pe", lambda e: e.transpose(out=pbT[:, 0:P], in_=TP[:, :], identity=idb[:]),
                          reads=[r_TP, r_idb], writes=[r_pbT])
                    tv = pbT[:, 0:64].rearrange("p (a b q) -> p a b q", a=4, b=2)
                    for pp in range(2):
                        tr.op("dve", lambda e, pp=pp, s=s: e.tensor_copy(
                            out=ATTs[pp * 64:(pp + 1) * 64, :, s * 8:(s + 1) * 8],
                            in_=tv[pp * 64:(pp + 1) * 64, :, pp, :]),
                            reads=[r_pbT, r_ATTs], writes=[r_ATTs])
                if not any(l in stages for l in ("S00", "S0", "S1", "S2", "S3", "S4", "S5")):
                    C.outproj(NS, xs[:, :], lambda kc: ATTs[:, kc, :], r_ATTs,
                              lambda kc: YNs[:, kc, :], r_YNs, NOT_)
        tr.barrier()
        if DO_MOE:
            with contextlib.ExitStack() as es:
                H2T = sb(es, "H2T", [P, 8, TOKS], BF16); r_H2T = Res()
                tr.dma("sp", H2T[:], h2d[:, :, :], reads=r_h2d, writes=[r_H2T])
                ACC = sb(es, "ACC", [P, NOT_ + 1, D]); r_ACC = [Res() for _ in range(NOT_ + 1)]
                tr.op("pool", lambda e: e.memset(ACC[:], 0.0), writes=r_ACC)
                ACTT = [sb(es, f"ACTT{i}", [P, 2, TOKS], BF16) for i in range(2)]
                r_ACTT = [[Res() for _ in range(5)] for _ in range(2)]
                WG = [sb(es, f"WG{i}", [P, 8, 512], BF16) for i in range(2)]; r_WG = [Res(), Res()]
                WD = [sb(es, f"WD{i}", [P, 2, D], BF16) for i in range(2)]; r_WD = [Res(), Res()]
                sg = [sb(es, f"sg{i}", [P, 512]) for i in range(2)]; r_sg = [Res(), Res()]
                groups = [(0, 512), (512, 512), (1024, 512), (1536, 512), (2048, 32)]
                gcnt = [0]; dcnt = [0]

                def moe_gu(e_, dgen):
                    wg = WG[e_ % 2]; rwg = r_WG[e_ % 2]
                    wd = WD[e_ % 2]; rwd = r_WD[e_ % 2]
                    tr.dma("pool", wg[:], w_gu[e_].rearrange("(kc p) n -> p kc n", p=P), writes=[rwg])
                    tr.dma("pool", wd[:], w_dn[e_].rearrange("(fc p) n -> p fc n", p=P), writes=[rwd])
                    at = ACTT[e_ % 2]
                    for gi, (c0, w) in enumerate(groups):
                        for fc in range(2):
                            b0 = 2 * (gcnt[0] % 2)
                            sgt = sg[gcnt[0] % 2]; rsg = r_sg[gcnt[0] % 2]
                            gcnt[0] += 1
                            for (bk, col) in ((b0, fc * P), (b0 + 1, 256 + fc * P)):
                                for kc in range(8):
                                    tr.op("pe", lambda e, kc=kc, bk=bk, col=col, c0=c0, w=w: e.matmul(
                                        pb[bk][:, 0:w], lhsT=wg[:, kc, col:col + P], rhs=H2T[:, kc, c0:c0 + w],
                                        start=(kc == 0), stop=(kc == 7)), reads=[rwg, r_H2T], writes=[r_pb[bk]])
                            tr.op("act", lambda e, b0=b0, w=w, sgt=sgt: e.activation(
                                out=sgt[:, 0:w], in_=pb[b0][:, 0:w], func=AF.Silu),
                                reads=[r_pb[b0]], writes=[rsg])
                            tr.op("dve", lambda e, b0=b0, w=w, sgt=sgt, fc=fc, c0=c0, at=at: e.tensor_tensor(
                                out=at[:, fc, c0:c0 + w], in0=sgt[:, 0:w], in1=pb[b0 + 1][:, 0:w], op=ALU.mult),
                                reads=[rsg, r_pb[b0 + 1]], writes=[r_ACTT[e_ % 2][gi]])
                            if dgen is not None:
                                for _ in range(4):
                                    next(dgen, None)
                    if dgen is not None:
                        for _ in dgen:
                            pass

                def moe_down(e_):
                    wd = WD[e_ % 2]; rwd = r_WD[e_ % 2]
                    at = ACTT[e_ % 2]
                    for t in range(NOT_ + 1):
                        n = P if t < NOT_ else 32
                        gi = t // 4
                        for hf in range(2):
                            bk = 4 + (dcnt[0] % 3)
                            dcnt[0] += 1
                            for fc in range(2):
                                tr.op("pe", lambda e, fc=fc, bk=bk, hf=hf, t=t, n=n: e.matmul(
                                    pb[bk][0:n, :], lhsT=at[:, fc, t * P:t * P + n],
                                    rhs=wd[:, fc, hf * 512:(hf + 1) * 512], start=(fc == 0), stop=(fc == 1)),
                                    reads=[r_ACTT[e_ % 2][gi], rwd], writes=[r_pb[bk]])
                            acc = ACC[0:n, t, hf * 512:(hf + 1) * 512]
                            if e_ < NE:
                                tr.op("dve", lambda e, bk=bk, n=n, t=t, acc=acc: e.scalar_tensor_tensor(
                                    out=acc, in0=pb[bk][0:n, :], scalar=GT[0:n, t, e_:e_ + 1], in1=acc,
                                    op0=ALU.mult, op1=ALU.add),
                                    reads=[r_pb[bk], r_GT[t], r_ACC[t]], writes=[r_ACC[t]])
                            else:
                                tr.op("dve", lambda e, bk=bk, n=n, acc=acc: e.tensor_tensor(
                                    out=acc, in0=pb[bk][0:n, :], in1=acc, op=ALU.add),
                                    reads=[r_pb[bk], r_ACC[t]], writes=[r_ACC[t]])
                            yield

                for e_ in range(NE + 1):
                    moe_gu(e_, moe_down(e_ - 1) if e_ > 0 else None)
                for _ in moe_down(NE):
                    pass

                bGF = sb(es, "bGF", [P, D]); r_bGF = Res()
                bFG = sb(es, "bFG", [P, D]); r_bFG = Res()
                xf = sb(es, "xf", [P, D]); r_xf = Res()
                yf = sb(es, "yf", [P, D]); r_yf = Res()
                jk = sb(es, "jk", [P, D], BF16); r_jk = Res()
                st3 = sb(es, "st3", [P, 4]); r_st3 = Res()
                tr.dma("sp", bFG[:], fing.partition_broadcast(P), writes=[r_bFG])
                for t in range(NOT_ + 1):
                    n = P if t < NOT_ else 32
                    R = slice(0, n)
                    if t == 0:
                        load_bc([(bGF, r_bGF, 5, None)], [(0, 0, P)], None)
                    if t == NOT_:
                        load_bc([(bGF, r_bGF, 5, None)], SROWS, None)
                    tr.dma("sp", xf[R, :], xmid[t * P:t * P + n, :], reads=[r_xmid[t]], writes=[r_xf])
                    tr.op("dve", lambda e, t=t: e.tensor_tensor(out=yf[R, :], in0=ACC[R, t, :], in1=bGF[R, :],
                                                                op=ALU.mult),
                          reads=[r_ACC[t], r_bGF], writes=[r_yf])
                    tr.op("pool", lambda e: e.tensor_tensor(out=yf[R, :], in0=yf[R, :], in1=xf[R, :], op=ALU.add),
                          reads=[r_yf, r_xf], writes=[r_yf])
                    tr.op("act", lambda e: e.activation(out=jk[R, :], in_=yf[R, :], func=AF.Square,
                                                        accum_out=st3[R, 0:1]),
                          reads=[r_yf], writes=[r_jk, r_st3])
                    tr.op("act", lambda e: e.activation(out=st3[R, 1:2], in_=st3[R, 0:1], func=AF.Sqrt,
                                                        scale=1.0 / D, bias=EPS), reads=[r_st3], writes=[r_st3])
                    tr.op("dve", lambda e: e.reciprocal(out=st3[R, 2:3], in_=st3[R, 1:2]),
                          reads=[r_st3], writes=[r_st3])
                    tr.op("dve", lambda e: e.scalar_tensor_tensor(
                        out=xf[R, :], in0=yf[R, :], scalar=st3[R, 2:3], in1=bFG[R, :],
                        op0=ALU.mult, op1=ALU.mult), reads=[r_yf, r_st3, r_bFG], writes=[r_xf])
                    ro = Res()
                    if t < NOT_:
                        tr.dma("sp", o_y[t * P:(t + 1) * P, :], xf[:, :], reads=[r_xf], writes=[ro])
                    else:
                        tr.dma("sp", o_ys[:, :], xf[R, :], reads=[r_xf], writes=[ro])
                    out_res.append(ro)
        tr.finish(out_res)
    return nc


def _rope_table(pos):
    half = 32
    inv = (10000.0 ** (-(np.arange(half, dtype=np.float32) / np.float32(half)))).astype(np.float32)
    ang = pos.astype(np.float32)[:, None] * inv[None, :]
    return np.concatenate([np.cos(ang), np.sin(ang)], axis=1).astype(np.float32)


def prep_inputs(inp):
    f = lambda a: np.ascontiguousarray(a, dtype=np.float32)
    maps = []
    S = 4096
    tri = np.where(np.arange(P)[:, None] <= np.arange(P)[None, :], 0.0, NEG).astype(np.float32)
    ohm = np.zeros((16, 16 * P), np.float32)
    for n in range(16):
        ohm[n, n * P:(n + 1) * P] = 1.0
    ohs = np.zeros((32, 32 * P), np.float32)
    for n in range(32):
        ohs[n, n * P:(n + 1) * P] = 1.0
    trs = np.full((32, 4, NH, 8), NEG, np.float32)
    for s in range(4):
        for tk in range(8):
            for tq in range(8):
                if tk <= tq:
                    trs[8 * s + tk, s, :, tq] = 0.0
    trs = trs.reshape(32, 256)
    selq = np.zeros((P, 64), np.float32)
    for h in range(NH):
        for h2 in range(NH):
            for q in range(8):
                selq[h * 8 + q, h2 * 8 + q] = 1.0
    dmask = np.zeros((64, NH, 64), np.float32)
    for h in range(NH):
        dmask[h * 8:(h + 1) * 8, h, :] = 1.0
    dmask = dmask.reshape(64, 512)
    par = np.zeros((64, 2), np.float32)
    for h in range(NH):
        par[h * 8:(h + 1) * 8, h % 2] = 1.0
    iot = np.arange(P, dtype=np.float32).reshape(P, 1)
    wab = np.zeros((P, 4, P), np.float32); wxb = np.zeros((P, 4, P), np.float32)
    ga = inp["gate_a_w"][0]; gx = inp["gate_x_w"][0]
    for ci in range(4):
        for j in range(2):
            wab[64 * j:64 * j + 64, ci, 64 * j:64 * j + 64] = ga[2 * ci + j]
            wxb[64 * j:64 * j + 64, ci, 64 * j:64 * j + 64] = gx[2 * ci + j]
    lv = np.zeros((P, 4, 12), np.float32)
    def fm(v):
        return v.reshape(4, P).T
    cw = inp["conv_w"][0]
    for jj in range(4):
        lv[:, :, jj] = fm(cw[jj])
    lv[:, :, 4] = fm(inp["conv_b"][0]); lv[:, :, 5] = fm(inp["gate_a_b"][0])
    lv[:, :, 6] = fm(inp["gate_x_b"][0]); lv[:, :, 7] = fm(inp["lru_lambda"][0])
    lv[:, :, 8] = fm(inp["lru_out_g"][0])
    w_gu = np.concatenate([inp["exp_w_gu"][0], inp["shared_w_gu"]], axis=0)
    w_dn = np.concatenate([inp["exp_w_down"][0], inp["shared_w_down"]], axis=0)
    ck = inp["cache_k"][0].reshape(2560 * P, 512)
    cv = inp["cache_v"][0].reshape(2560 * P, 512)
    ropes = _rope_table(8192 + np.arange(8))
    for c in range(8):
        b, half = c // 2, c % 2
        x = inp["x_prompt"][b]
        if half == 1:
            xpl = x
            pos = np.arange(S)
        else:
            xpl = np.concatenate([np.zeros((2048, D), np.float32), x[:2048]], axis=0)
            pos = np.concatenate([np.zeros(2048, np.int64), np.arange(2048)])
        cs = np.concatenate([inp["c_prompt"][b:b + 1], inp["c_sample"][4 * c:4 * c + 4]], axis=0)
        c5 = cs.T.reshape(8, P, 5).transpose(1, 0, 2)
        bl = np.zeros((8, 3, 16), np.float32)
        for j in range(8):
            cur = 8 + j
            for n in range(16):
                valid_past = (n < cur) and (half == 1 or n >= 8)
                bl[j, 0, n] = 0.0 if valid_past else -1e30
                bl[j, 1, n] = 0.0 if n == cur else 1.0
                bl[j, 2, n] = 0.0 if (valid_past or n == cur) else NEG
        fl = np.zeros((P, 2), np.float32)
        fl[:, 0] = float(half); fl[:, 1] = 1.0 - float(half)
        sh = inp["state_h"][0, 4 * c:4 * c + 4]
        sc = inp["state_conv"][0, 4 * c:4 * c + 4]
        sth = sh.reshape(4, 4, P).transpose(2, 1, 0)
        stc = sc.reshape(4, 3, 4, P).transpose(3, 2, 0, 1)
        m = {
            "xp": f(xpl), "xs": f(inp["x_sample"][4 * c:4 * c + 4].reshape(32, D)), "c5": f(c5),
            "ada_w": f(inp["ada_w"][0]), "ada_b": f(inp["ada_b"][0]),
            "norm_mix_g": f(inp["norm_mix_g"][0]), "norm_ffn_g": f(inp["norm_ffn_g"][0]),
            "final_g": f(inp["final_g"]), "w_in": f(inp["w_in"][0]), "w_out": f(inp["w_out"][0]),
            "rope": _rope_table(pos), "ropes": ropes, "lruv": lv, "wab": wab, "wxb": wxb,
            "attn_out_g": f(inp["attn_out_g"][0]), "blkc": f(bl.reshape(-1)), "flags": fl,
            "trim": tri, "ohm": ohm, "router_w": f(inp["router_w"][0]),
            "router_bias": f(inp["router_bias"][0]), "w_gu": w_gu, "w_dn": w_dn,
            "cache_k": ck, "cache_v": cv,
            "ptab": np.ascontiguousarray(inp["page_table"][4 * c:4 * c + 4], dtype=np.int32),
            "sth": f(sth), "stc": f(stc), "ohs": ohs, "trs": trs, "iot": iot, "selq": selq, "dmask": dmask, "par": par,
        }
        maps.append(m)
    return maps


def assemble(results):
    y_p = np.zeros((4, 4096, D), np.float32)
    k_p = np.zeros((1, 4, 4096, NH, HD), np.float32)
    v_p = np.zeros((1, 4, 4096, NH, HD), np.float32)
    h_p = np.zeros((1, 4, 512), np.float32)
    c_p = np.zeros((1, 4, 3, 512), np.float32)
    y_s = np.zeros((32, 8, D), np.float32)
    k_s = np.zeros((1, 32, 8, NH, HD), np.float32)
    v_s = np.zeros((1, 32, 8, NH, HD), np.float32)
    h_s = np.zeros((1, 32, 512), np.float32)
    c_s = np.zeros((1, 32, 3, 512), np.float32)
    for c in range(8):
        b, half = c // 2, c % 2
        r = results[c]
        sl = slice(half * 2048, half * 2048 + 2048)
        y_p[b, sl] = r["o_y"]
        k_p[0, b, sl] = r["o_k"].reshape(2048, NH, HD)
        v_p[0, b, sl] = r["o_v"].reshape(2048, NH, HD)
        if half == 1:
            h_p[0, b] = r["o_h"]
            c_p[0, b] = r["o_c"]
        y_s[4 * c:4 * c + 4] = r["o_ys"].reshape(4, 8, D)
        k_s[0, 4 * c:4 * c + 4] = r["o_ks"].reshape(4, 8, NH, HD)
        v_s[0, 4 * c:4 * c + 4] = r["o_vs"].reshape(4, 8, NH, HD)
        h_s[0, 4 * c:4 * c + 4] = r["o_hs"]
        c_s[0, 4 * c:4 * c + 4] = r["o_cs"].reshape(4, 3, 512)
    return (y_p, y_s, k_p, v_p, h_p, c_p, k_s, v_s, h_s, c_s)


_NC_CACHE = {}


def kernel(**inputs):
    inp = {k: np.asarray(v) for k, v in inputs.items()}
    maps = prep_inputs(inp)
    if "nc" not in _NC_CACHE:
        _NC_CACHE["nc"] = build()
    res = run_bass_kernel_spmd(_NC_CACHE["nc"], maps, core_ids=list(range(8)))
    return assemble(res.results)
```
